# Optimizing a Trainium2 kernel written in Bass

```python
import jax, jax.numpy as jnp
from jax import lax
import numpy as np

D_MODEL = 1024
BATCH = 2
SEQ = 8192
DEPTH = 2

HEAD_DIM = 64
MIX_DIM = D_MODEL
N_MIX_HEADS = MIX_DIM // HEAD_DIM
A_HEADS = N_MIX_HEADS // 2
B_HEADS = N_MIX_HEADS - A_HEADS
RWKV_DIM = A_HEADS * HEAD_DIM
MOBA_DIM = B_HEADS * HEAD_DIM
DECAY_LORA = max(32, int(round(1.8 * RWKV_DIM ** 0.5 / 32)) * 32)
AAA_LORA = max(32, int(round(1.8 * RWKV_DIM ** 0.5 / 32)) * 32)
GATE_LORA = max(32, int(round(0.6 * RWKV_DIM ** 0.8 / 32)) * 32)
RWKV_COLS = 3 * RWKV_DIM + DECAY_LORA + AAA_LORA + GATE_LORA
RWKV_SPLITS = (RWKV_DIM, 2 * RWKV_DIM, 3 * RWKV_DIM, 3 * RWKV_DIM + DECAY_LORA, 3 * RWKV_DIM + DECAY_LORA + AAA_LORA)
AB_IN_COLS = RWKV_COLS + 3 * MOBA_DIM
RWKV_GN_EPS = 64e-5
MOBA_BLOCK = 256
MOBA_TOPK = 3
MOBA_Q_CHUNK = 32
ROPE_THETA = 500000.0
ROPE_DIM = HEAD_DIM // 4
SB_HEADS = N_MIX_HEADS
SB_Q_BLOCK = 128
N_EXPERTS = 32
TOP_K = 4
EXPERT_FF = D_MODEL
SWIGLU_ALPHA = 1.702
SWIGLU_LIMIT = 7.0
MOE_ROW_BLOCK = 256
LN_EPS = 1e-5
DEEPNORM_ALPHA = (2 * DEPTH) ** 0.25
DEEPNORM_BETA = (8 * DEPTH) ** -0.25

kernel_name = 'hybrid_rwkv7_moba_stickbreak_moe'


def layer_norm(x, g, b):
    xf = x.astype(jnp.float32)
    mu = xf.mean(-1, keepdims=True)
    var = jnp.square(xf - mu).mean(-1, keepdims=True)
    return ((xf - mu) * lax.rsqrt(var + LN_EPS) * g + b).astype(x.dtype)


def partial_rope(t, pos):
    half = ROPE_DIM // 2
    inv_freq = ROPE_THETA ** (-jnp.arange(half, dtype=jnp.float32) / half)
    ang = pos.astype(jnp.float32)[:, None] * inv_freq[None, :]
    cos = jnp.cos(ang)[None, :, None, :]
    sin = jnp.sin(ang)[None, :, None, :]
    t1 = t[..., :half].astype(jnp.float32)
    t2 = t[..., half:ROPE_DIM].astype(jnp.float32)
    rot = jnp.concatenate([t1 * cos - t2 * sin, t2 * cos + t1 * sin], -1).astype(t.dtype)
    return jnp.concatenate([rot, t[..., ROPE_DIM:]], -1)


def rwkv7_scan(r, decay, k, v, a, b):
    B, S, H, N = r.shape
    def step(state, inp):
        r_t, w_t, k_t, v_t, a_t, b_t = inp
        sa = jnp.einsum('bhvk,bhk->bhv', state, a_t)
        state = state * w_t[:, :, None, :] + sa[..., None] * b_t[:, :, None, :] + v_t[..., None] * k_t[:, :, None, :]
        return state, jnp.einsum('bhvk,bhk->bhv', state, r_t)
    xs = tuple(jnp.moveaxis(t.astype(jnp.float32), 1, 0) for t in (r, decay, k, v, a, b))
    _, ys = lax.scan(step, jnp.zeros((B, H, N, N), jnp.float32), xs)
    return jnp.moveaxis(ys, 0, 1)


def rwkv7_mix(p, shift_mu, w0, w2, a0, a2, g2, k_k, k_a, r_k, lnx_g, lnx_b):
    B, S, _ = p.shape
    dt = p.dtype
    p_prev = jnp.pad(p, ((0, 0), (1, 0), (0, 0)))[:, :-1]
    p = p + shift_mu * (p_prev - p)
    r, k, v, w_lo, a_lo, g_lo = jnp.split(p, RWKV_SPLITS, axis=-1)
    w = -jax.nn.softplus(-(w0 + jnp.tanh(w_lo) @ w2)) - 0.5
    decay = jnp.exp(-jnp.exp(w.astype(jnp.float32)))
    a = jax.nn.sigmoid(a0 + a_lo @ a2)
    g = jax.nn.sigmoid(g_lo) @ g2
    heads = lambda t: t.reshape(B, S, A_HEADS, HEAD_DIM)
    kk = heads(k * k_k).astype(jnp.float32)
    kk = kk / jnp.maximum(jnp.sqrt(jnp.sum(kk * kk, -1, keepdims=True)), 1e-12)
    k = k * (1 + (a - 1) * k_a)
    r_h, k_h, v_h, a_h = heads(r), heads(k), heads(v), heads(a)
    y = rwkv7_scan(r_h, heads(decay), k_h, v_h, -kk, kk * a_h.astype(jnp.float32))
    mu = y.mean(-1, keepdims=True)
    var = jnp.square(y - mu).mean(-1, keepdims=True)
    y = ((y - mu) * lax.rsqrt(var + RWKV_GN_EPS)).reshape(B, S, RWKV_DIM) * lnx_g + lnx_b
    bonus = jnp.sum((r_h * k_h * r_k).astype(jnp.float32), -1, keepdims=True) * v_h.astype(jnp.float32)
    y = y + bonus.reshape(B, S, RWKV_DIM)
    return y.astype(dt) * g


def moba_attention(q, k, v):
    B, S, H, dh = q.shape
    nb = -(-S // MOBA_BLOCK)
    s_pad = nb * MOBA_BLOCK
    n_sel = max(1, min(MOBA_TOPK, nb - 1))
    pad = ((0, 0), (0, s_pad - S), (0, 0), (0, 0))
    q = jnp.pad(q, pad).transpose(0, 2, 1, 3) * (dh ** -0.5)
    kb = jnp.pad(k, pad).transpose(0, 2, 1, 3).reshape(B, H, nb, MOBA_BLOCK, dh)
    vb = jnp.pad(v, pad).transpose(0, 2, 1, 3).reshape(B, H, nb, MOBA_BLOCK, dh)
    k_mean = kb.astype(jnp.float32).mean(axis=3)
    gate = jnp.einsum('bhsd,bhnd->bhsn', q.astype(jnp.float32), k_mean)
    q_block = jnp.arange(s_pad) // MOBA_BLOCK
    fully_past = jnp.arange(nb)[None, :] < q_block[:, None]
    gate = jnp.where(fully_past, gate, -jnp.inf)
    _, sel = lax.top_k(gate, n_sel)
    sel_valid = sel < q_block[:, None]
    nq = s_pad // MOBA_Q_CHUNK
    def to_chunks(t):
        return jnp.moveaxis(t.reshape(B, H, nq, MOBA_Q_CHUNK, *t.shape[3:]), 2, 0)
    gather_blocks = jax.vmap(jax.vmap(lambda blocks, idx: blocks[idx]))
    offs = jnp.arange(MOBA_BLOCK)
    def chunk(args):
        c, q_c, sel_c, valid_c = args
        own = (c * MOBA_Q_CHUNK) // MOBA_BLOCK
        k_own = lax.dynamic_index_in_dim(kb, own, axis=2, keepdims=False)
        v_own = lax.dynamic_index_in_dim(vb, own, axis=2, keepdims=False)
        k_sel = gather_blocks(kb, sel_c)
        v_sel = gather_blocks(vb, sel_c)
        s_sel = jnp.einsum('bhqd,bhqnkd->bhqnk', q_c, k_sel).astype(jnp.float32)
        s_sel = jnp.where(valid_c[..., None], s_sel, -jnp.inf)
        q_pos = c * MOBA_Q_CHUNK + jnp.arange(MOBA_Q_CHUNK)
        k_pos = own * MOBA_BLOCK + offs
        s_own = jnp.einsum('bhqd,bhkd->bhqk', q_c, k_own).astype(jnp.float32)
        s_own = jnp.where(k_pos[None, :] <= q_pos[:, None], s_own, -jnp.inf)
        s_all = jnp.concatenate([s_sel.reshape(B, H, MOBA_Q_CHUNK, n_sel * MOBA_BLOCK), s_own], -1)
        prob = jax.nn.softmax(s_all, axis=-1).astype(vb.dtype)
        p_sel = prob[..., :n_sel * MOBA_BLOCK].reshape(B, H, MOBA_Q_CHUNK, n_sel, MOBA_BLOCK)
        p_own = prob[..., n_sel * MOBA_BLOCK:]
        return jnp.einsum('bhqnk,bhqnkd->bhqd', p_sel, v_sel) + jnp.einsum('bhqk,bhkd->bhqd', p_own, v_own)
    out = lax.map(chunk, (jnp.arange(nq), to_chunks(q), to_chunks(sel), to_chunks(sel_valid)))
    out = jnp.moveaxis(out, 0, 2).reshape(B, H, s_pad, dh)[:, :, :S]
    return out.transpose(0, 2, 1, 3)


def stick_breaking_attention(q, k, v):
    B, S, H, dh = q.shape
    q = q.transpose(0, 2, 1, 3) * (dh ** -0.5)
    k = k.transpose(0, 2, 1, 3)
    v = v.transpose(0, 2, 1, 3)
    nq = S // SB_Q_BLOCK
    q_blocks = jnp.moveaxis(q.reshape(B, H, nq, SB_Q_BLOCK, dh), 2, 0)
    k_pos = jnp.arange(S)
    def block(args):
        c, q_c = args
        z = jnp.einsum('bhqd,bhkd->bhqk', q_c, k).astype(jnp.float32)
        q_pos = c * SB_Q_BLOCK + jnp.arange(SB_Q_BLOCK)
        causal = k_pos[None, :] < q_pos[:, None]
        log_keep = jnp.where(causal, -jax.nn.softplus(z), 0.0)
        after = lax.cumsum(log_keep, axis=3, reverse=True) - log_keep
        att = jnp.where(causal, jnp.exp(jax.nn.log_sigmoid(z) + after), 0.0)
        return jnp.einsum('bhqk,bhkd->bhqd', att.astype(v.dtype), v)
    out = lax.map(block, (jnp.arange(nq), q_blocks))
    out = jnp.moveaxis(out, 0, 2).reshape(B, H, S, dh)
    return out.transpose(0, 2, 1, 3)


def clamped_swiglu(h):
    glu = jnp.minimum(h[..., :EXPERT_FF], SWIGLU_LIMIT)
    lin = jnp.clip(h[..., EXPERT_FF:], -SWIGLU_LIMIT, SWIGLU_LIMIT)
    return glu * jax.nn.sigmoid(SWIGLU_ALPHA * glu) * (lin + 1)


def moe_ffn(x, router_w, router_b, w1, b1, w2, b2):
    B, S, D = x.shape
    T = B * S
    TK = T * TOP_K
    xf = x.reshape(T, D)
    logits = (xf @ router_w + router_b).astype(jnp.float32)
    top_val, top_idx = lax.top_k(logits, TOP_K)
    gates = jax.nn.softmax(top_val, axis=-1).astype(x.dtype)
    flat_e = top_idx.reshape(-1)
    order = jnp.argsort(flat_e)
    sorted_e = flat_e[order]
    sorted_tok = (order // TOP_K).astype(jnp.int32)
    sorted_gate = gates.reshape(-1)[order]
    counts = jnp.bincount(flat_e, length=N_EXPERTS)
    padded = (counts + MOE_ROW_BLOCK - 1) // MOE_ROW_BLOCK * MOE_ROW_BLOCK
    start = jnp.cumsum(counts) - counts
    pend = jnp.cumsum(padded)
    pstart = pend - padded
    dest = pstart[sorted_e] + jnp.arange(TK) - start[sorted_e]
    n_blocks = -(-(TK + N_EXPERTS * (MOE_ROW_BLOCK - 1)) // MOE_ROW_BLOCK)
    n_rows = n_blocks * MOE_ROW_BLOCK
    row_tok = jnp.full((n_rows,), T, jnp.int32).at[dest].set(sorted_tok)
    row_gate = jnp.zeros((n_rows,), x.dtype).at[dest].set(sorted_gate)
    block_e = jnp.minimum(jnp.searchsorted(pend, jnp.arange(n_blocks) * MOE_ROW_BLOCK, side='right'), N_EXPERTS - 1)
    x_pad = jnp.concatenate([xf, jnp.zeros((1, D), x.dtype)], 0)
    def expert_block(args):
        toks, e = args
        h = x_pad[toks] @ w1[e] + b1[e]
        return clamped_swiglu(h) @ w2[e] + b2[e]
    y_rows = lax.map(expert_block, (row_tok.reshape(n_blocks, MOE_ROW_BLOCK), block_e))
    y_rows = y_rows.reshape(n_rows, D) * row_gate[:, None]
    y = jnp.zeros((T + 1, D), x.dtype).at[row_tok].add(y_rows)[:T]
    return y.reshape(B, S, D)


def mix_rwkv_moba(x, w_in, shift_mu, w0, w2, a0, a2, g2, k_k, k_a, r_k, lnx_g, lnx_b, w_out):
    B, S, _ = x.shape
    p = x @ w_in
    y_a = rwkv7_mix(p[..., :RWKV_COLS], shift_mu, w0, w2, a0, a2, g2, k_k, k_a, r_k, lnx_g, lnx_b)
    q, k, v = jnp.split(p[..., RWKV_COLS:].reshape(B, S, 3 * B_HEADS, HEAD_DIM), 3, axis=2)
    pos = jnp.arange(S, dtype=jnp.int32)
    y_b = moba_attention(partial_rope(q, pos), partial_rope(k, pos), v).reshape(B, S, MOBA_DIM)
    return jnp.concatenate([y_a, y_b], -1) @ w_out


def mix_stick_breaking(x, w_in, w_out):
    B, S, _ = x.shape
    q, k, v = jnp.split((x @ w_in).reshape(B, S, 3 * SB_HEADS, HEAD_DIM), 3, axis=2)
    return stick_breaking_attention(q, k, v).reshape(B, S, MIX_DIM) @ w_out


def setup_inputs(seed: int = 0) -> dict:
    key = jax.random.key(seed)
    ks = iter(jax.random.split(key, 40))
    def nrm(shape, scale):
        return scale * jax.random.normal(next(ks), shape, jnp.float32)
    def unif(shape, lo, hi):
        return jax.random.uniform(next(ks), shape, jnp.float32, lo, hi)
    n_ab = (DEPTH + 1) // 2
    n_sb = DEPTH // 2
    out_scale = DEEPNORM_BETA * MIX_DIM ** -0.5
    return {
        'x': nrm((BATCH, SEQ, D_MODEL), 1.0),
        'ab_w_in': nrm((n_ab, D_MODEL, AB_IN_COLS), D_MODEL ** -0.5),
        'ab_shift_mu': unif((n_ab, RWKV_COLS), 0.0, 1.0),
        'ab_w0': unif((n_ab, RWKV_DIM), -6.0, 1.0),
        'ab_w2': nrm((n_ab, DECAY_LORA, RWKV_DIM), 0.1 * DECAY_LORA ** -0.5),
        'ab_a0': nrm((n_ab, RWKV_DIM), 0.5),
        'ab_a2': nrm((n_ab, AAA_LORA, RWKV_DIM), 0.5 * AAA_LORA ** -0.5),
        'ab_g2': nrm((n_ab, GATE_LORA, RWKV_DIM), GATE_LORA ** -0.5),
        'ab_k_k': 0.85 + nrm((n_ab, RWKV_DIM), 0.05),
        'ab_k_a': 1.0 + nrm((n_ab, RWKV_DIM), 0.05),
        'ab_r_k': nrm((n_ab, A_HEADS, HEAD_DIM), 0.1),
        'ab_lnx_g': 1.0 + nrm((n_ab, RWKV_DIM), 0.05),
        'ab_lnx_b': nrm((n_ab, RWKV_DIM), 0.02),
        'ab_w_out': nrm((n_ab, MIX_DIM, D_MODEL), out_scale),
        'sb_w_in': nrm((n_sb, D_MODEL, 3 * MIX_DIM), D_MODEL ** -0.5),
        'sb_w_out': nrm((n_sb, MIX_DIM, D_MODEL), out_scale),
        'ln1_g': 1.0 + nrm((DEPTH, D_MODEL), 0.05),
        'ln1_b': nrm((DEPTH, D_MODEL), 0.02),
        'router_w': nrm((DEPTH, D_MODEL, N_EXPERTS), D_MODEL ** -0.5),
        'router_b': nrm((DEPTH, N_EXPERTS), 0.01),
        'exp_w1': nrm((DEPTH, N_EXPERTS, D_MODEL, 2 * EXPERT_FF), D_MODEL ** -0.5),
        'exp_b1': nrm((DEPTH, N_EXPERTS, 2 * EXPERT_FF), 0.01),
        'exp_w2': nrm((DEPTH, N_EXPERTS, EXPERT_FF, D_MODEL), DEEPNORM_BETA * EXPERT_FF ** -0.5),
        'exp_b2': nrm((DEPTH, N_EXPERTS, D_MODEL), 0.01),
        'ln2_g': 1.0 + nrm((DEPTH, D_MODEL), 0.05),
        'ln2_b': nrm((DEPTH, D_MODEL), 0.02),
    }


def reference(x, ab_w_in, ab_shift_mu, ab_w0, ab_w2, ab_a0, ab_a2, ab_g2, ab_k_k, ab_k_a, ab_r_k,
              ab_lnx_g, ab_lnx_b, ab_w_out, sb_w_in, sb_w_out, ln1_g, ln1_b, router_w, router_b,
              exp_w1, exp_b1, exp_w2, exp_b2, ln2_g, ln2_b):
    for i in range(DEPTH):
        j = i // 2
        if i % 2 == 0:
            h = mix_rwkv_moba(x, ab_w_in[j], ab_shift_mu[j], ab_w0[j], ab_w2[j], ab_a0[j], ab_a2[j],
                              ab_g2[j], ab_k_k[j], ab_k_a[j], ab_r_k[j], ab_lnx_g[j], ab_lnx_b[j], ab_w_out[j])
        else:
            h = mix_stick_breaking(x, sb_w_in[j], sb_w_out[j])
        x = layer_norm(DEEPNORM_ALPHA * x + h, ln1_g[i], ln1_b[i])
        f = moe_ffn(x, router_w[i], router_b[i], exp_w1[i], exp_b1[i], exp_w2[i], exp_b2[i])
        x = layer_norm(DEEPNORM_ALPHA * x + f, ln2_g[i], ln2_b[i])
    return x
```

```python
import numpy as np
from contextlib import ExitStack
import concourse.bass as bass
import concourse.mybir as mybir
from concourse.bass_utils import run_bass_kernel_spmd

F32 = mybir.dt.float32
BF16 = mybir.dt.bfloat16
AF = mybir.ActivationFunctionType
ALU = mybir.AluOpType
AX = mybir.AxisListType

D = 1024
SEQ = 8192
NB = 2
NCORES = 8
ALPHA = float((2 * 2) ** 0.25)
LN_EPS = 1e-5
N_EXP = 32


class KB:
    def __init__(self, nc, es, n_dma_sems=24):
        self.nc = nc
        self.es = es
        self.E = {'pe': nc.tensor, 'dve': nc.vector, 'act': nc.scalar, 'pool': nc.gpsimd, 'sp': nc.sync}
        self.sem = {}
        self.cnt = {}
        for k in ['pe', 'dve', 'act', 'pool']:
            self.sem[k] = es.enter_context(nc.semaphore('s_' + k))
            self.cnt[k] = 0
        self.dsem = []
        for i in range(n_dma_sems):
            self.dsem.append(es.enter_context(nc.semaphore('d%d' % i)))
        self.dcnt = [0] * n_dma_sems
        self.dnext = 0
        self.waited = {k: {} for k in self.E}
        self.lastw = {}
        self.readers = {}
        self.ninst = 0

    def sb(self, name, shape, dt):
        return self.es.enter_context(self.nc.sbuf_tensor("sb_" + name, shape, dt))

    def ps(self, name, shape, dt=F32):
        return self.es.enter_context(self.nc.psum_tensor("ps_" + name, shape, dt))

    def _deps(self, reads, writes):
        deps = {}

        def add(t):
            if t is None:
                return
            s, v = t
            if deps.get(s, 0) < v:
                deps[s] = v
        for r in reads:
            add(self.lastw.get(r))
            if isinstance(r, str) and r.startswith('pb'):
                for t in self.readers.get(r, ()):
                    add(t)
        for w in writes:
            add(self.lastw.get(w))
            for t in self.readers.get(w, ()):
                add(t)
        return deps

    def _wait(self, eng, deps, skip=None):
        for s, v in deps.items():
            if skip is not None and s == skip:
                continue
            if self.waited[eng].get(s, 0) < v:
                semobj = self.sem[s] if isinstance(s, str) else self.dsem[s]
                self.E[eng].wait_ge(semobj, v)
                self.waited[eng][s] = v

    def _commit(self, tok, reads, writes):
        for r in reads:
            self.readers.setdefault(r, []).append(tok)
        for w in writes:
            self.lastw[w] = tok
            self.readers[w] = []

    def op(self, eng, reads, writes, emit):
        deps = self._deps(reads, writes)
        self._wait(eng, deps, skip='pe' if eng == 'pe' else None)
        inst = emit(self.E[eng])
        self.cnt[eng] += 1
        inst.then_inc(self.sem[eng], 1)
        self._commit((eng, self.cnt[eng]), reads, writes)
        self.ninst += 1
        return inst

    def dma(self, q, reads, writes, out, in_, **kw):
        deps = self._deps(reads, writes)
        i = self.dnext
        self.dnext = (self.dnext + 1) % len(self.dsem)
        if self.dcnt[i] > 0:
            deps[i] = max(deps.get(i, 0), self.dcnt[i])
        self._wait(q, deps)
        inst = self.E[q].dma_start(out=out, in_=in_, **kw)
        self.dcnt[i] += 16
        inst.then_inc(self.dsem[i], 16)
        self._commit((i, self.dcnt[i]), reads, writes)
        self.ninst += 1
        return inst

    def finish(self, eng='sp'):
        deps = {}
        for t in self.lastw.values():
            s, v = t
            if deps.get(s, 0) < v:
                deps[s] = v
        self._wait(eng, deps)


class _Stop(Exception):
    pass


import os as _os
_STAGE = int(_os.environ.get("KSTAGE", "0"))


def stage(k):
    if _STAGE == k:
        raise _Stop()


def _din(nc, name, shape, dt=F32):
    return nc.dram_tensor(name, list(shape), dt, kind="ExternalInput").ap()


def _dout(nc, name, shape, dt=F32):
    return nc.dram_tensor(name, list(shape), dt, kind="ExternalOutput").ap()


def layer_norm_inplace(kb, t, tkey, gB, bB, stats, mv, rs, sfx):
    kb.op('dve', [tkey], ['stats' + sfx], lambda e: e.bn_stats(out=stats[:, 0, :], in_=t[:, 0:512]))
    kb.op('dve', [tkey], ['stats' + sfx], lambda e: e.bn_stats(out=stats[:, 1, :], in_=t[:, 512:1024]))
    kb.op('dve', ['stats' + sfx], ['mv' + sfx],
          lambda e: e.bn_aggr(out=mv[:, :], in_=stats[:, :, :].rearrange("p a b -> p (a b)")))
    kb.op('act', ['mv' + sfx], ['rs' + sfx],
          lambda e: e.activation(out=rs[:, :], in_=mv[:, 1:2], func=AF.Sqrt, bias=kb.eps_t[:, 0:1], scale=1.0))
    kb.op('dve', ['rs' + sfx], ['rs' + sfx], lambda e: e.reciprocal(out=rs[:, :], in_=rs[:, :]))
    kb.op('dve', [tkey, 'mv' + sfx, 'rs' + sfx], [tkey],
          lambda e: e.tensor_scalar(out=t, in0=t, scalar1=mv[:, 0:1], scalar2=rs[:, 0:1],
                                    op0=ALU.subtract, op1=ALU.mult))
    kb.op('dve', [tkey, gB[1]], [tkey], lambda e: e.tensor_tensor(out=t, in0=t, in1=gB[0][:, :], op=ALU.mult))
    kb.op('dve', [tkey, bB[1]], [tkey], lambda e: e.tensor_tensor(out=t, in0=t, in1=bB[0][:, :], op=ALU.add))


def build_post(n_exp=N_EXP, npass=None):
    nc = bass.Bass("TRN2", target_bir_lowering=False)
    NTOK = 2048
    PT = 1024
    TT = PT // 128
    NPASS = npass or NTOK // PT
    yT = _din(nc, "yT", [D, NTOK])
    xr = _din(nc, "xr", [NTOK, D])
    w_out = _din(nc, "w_out", [D, D])
    ln1g = _din(nc, "ln1g", [D])
    ln1b = _din(nc, "ln1b", [D])
    rw = _din(nc, "rw", [D, N_EXP])
    rb = _din(nc, "rb", [N_EXP])
    w1 = _din(nc, "w1", [N_EXP, D, 2 * D])
    b1T = _din(nc, "b1T", [128, N_EXP, 16])
    w2 = _din(nc, "w2", [N_EXP, D, D])
    b2 = _din(nc, "b2", [N_EXP, D])
    ln2g = _din(nc, "ln2g", [D])
    ln2b = _din(nc, "ln2b", [D])
    ident_d = _din(nc, "ident", [128, 128])
    out = _dout(nc, "out", [NTOK, D])

    with ExitStack() as es:
        kb = KB(nc, es)
        yacc = kb.sb("yacc", [128, TT, D], F32)
        x1T = kb.sb("x1T", [128, 8, PT], BF16)
        gate = kb.sb("gate", [128, TT, N_EXP], F32)
        NSLOT = 6
        slots = [kb.sb("wslot%d" % i, [128, 8, 512], BF16) for i in range(NSLOT)]
        actT = kb.sb("actT", [128, 8, PT], BF16)
        g32 = [kb.sb("g32_%d" % i, [128, 512], F32) for i in range(2)]
        s32 = [kb.sb("s32_%d" % i, [128, 512], F32) for i in range(2)]
        l32 = [kb.sb("l32_%d" % i, [128, 512], F32) for i in range(2)]
        gB = kb.sb("gB", [128, D], F32)
        bB = kb.sb("bB", [128, D], F32)
        yTb = [kb.sb("yTb%d" % i, [128, 8, 128], BF16) for i in range(2)]
        xrt = kb.sb("xrt", [128, D], F32)
        x1T32 = kb.sb("x1T32", [128, 8, 128], F32)
        rw32 = kb.sb("rw32", [128, 8, N_EXP], F32)
        rbB = kb.sb("rbB", [128, N_EXP], F32)
        b1s = kb.sb("b1s", [128, N_EXP, 16], F32)
        b2t = kb.sb("b2t", [1, D], F32)
        ones32 = kb.sb("ones32", [1, 128], F32)
        ident = kb.sb("ident", [128, 128], F32)
        woutb = kb.sb("woutb", [128, 8, D], BF16)
        stats = kb.sb("stats", [128, 2, 6], F32)
        mv = kb.sb("mv", [128, 2], F32)
        rs = kb.sb("rs", [128, 1], F32)
        lg = kb.sb("lg", [128, N_EXP], F32)
        m8 = kb.sb("m8", [128, 8], F32)
        msk = kb.sb("msk", [128, N_EXP], F32)
        nm = kb.sb("nm", [128, 1], F32)
        ex = kb.sb("ex", [128, N_EXP], F32)
        den = kb.sb("den", [128, 1], F32)
        eps_t = kb.sb("eps_t", [128, 1], F32)
        kb.eps_t = eps_t
        pb = [kb.ps("pb%d" % i, [128, 512]) for i in range(8)]

        kb.op('dve', [], ['eps'], lambda e: e.memset(eps_t[:, :], LN_EPS))
        kb.op('dve', [], ['ones32'], lambda e: e.memset(ones32[:, :], 1.0))
        kb.dma('sp', [], ['ident'], ident[:, :], ident_d[:, :])
        kb.dma('sp', [], ['rw32'], rw32[:, :, :], rw.rearrange("(c p) e -> p c e", p=128))
        kb.dma('sp', [], ['rbB'], rbB[:, :], rb.partition_broadcast(128))
        kb.dma('sp', [], ['b1s'], b1s[:, :, :], b1T[:, :, :])
        kb.dma('pool', [], ['woutb'], woutb[:, :, :], w_out.rearrange("(c p) n -> p c n", p=128))

        blocks = []
        for p_ in range(NPASS):
            for e_ in range(n_exp):
                for b_ in range(6):
                    blocks.append((e_, b_))
        nload = [0]

        def load_next_block():
            n = nload[0]
            if n >= len(blocks):
                return
            e_, b_ = blocks[n]
            s = n % NSLOT
            if b_ < 4:
                c0 = [0, 1024, 512, 1536][b_]
                src = w1[e_, :, c0:c0 + 512].rearrange("(c p) n -> p c n", p=128)
            else:
                c0 = (b_ - 4) * 512
                src = w2[e_, :, c0:c0 + 512].rearrange("(c p) n -> p c n", p=128)
            kb.dma('pool', [], ['slot%d' % s], slots[s][:, :, :], src)
            nload[0] += 1

        for _ in range(NSLOT):
            load_next_block()
        nuse = [0]

        try:
            stage(1)
            for ps_ in range(NPASS):
                tok0 = ps_ * PT
                kb.dma('sp', ['eps'], ['gB'], gB[:, :], ln1g.partition_broadcast(128))
                kb.dma('sp', ['eps'], ['bB'], bB[:, :], ln1b.partition_broadcast(128))
                for i in range(TT):
                    t0 = tok0 + i * 128
                    yb = yTb[i % 2]
                    ybk = 'yTb%d' % (i % 2)
                    kb.dma('pool', [], [ybk], yb[:, :, :], yT[:, t0:t0 + 128].rearrange("(c p) t -> p c t", p=128))
                    kb.dma('sp', [], ['xrt'], xrt[:, :], xr[t0:t0 + 128, :])
                    for h in range(2):
                        for c in range(8):
                            kb.op('pe', [ybk, 'woutb'], ['pb%d' % h],
                                  lambda e, c=c, h=h: e.matmul(pb[h][:, :], yb[:, c, :], woutb[:, c, h * 512:(h + 1) * 512],
                                                               start=(c == 0), stop=(c == 7)))
                    yk = 'yacc%d' % i
                    for h in range(2):
                        kb.op('dve', ['xrt', 'pb%d' % h], [yk],
                              lambda e, h=h: e.scalar_tensor_tensor(out=yacc[:, i, h * 512:(h + 1) * 512],
                                                                    in0=xrt[:, h * 512:(h + 1) * 512], scalar=ALPHA,
                                                                    in1=pb[h][:, :], op0=ALU.mult, op1=ALU.add))
                    stage(2)
                    layer_norm_inplace(kb, yacc[:, i, :], yk, (gB, 'gB'), (bB, 'bB'), stats, mv, rs, '')
                    stage(3)
                    for c in range(8):
                        bk = 2 + c // 4
                        kb.op('pe', [yk, 'ident'], ['pb%d' % bk],
                              lambda e, c=c, bk=bk: e.transpose(pb[bk][:, (c % 4) * 128:(c % 4 + 1) * 128],
                                                                yacc[:, i, c * 128:(c + 1) * 128], ident[:, :]))
                    for hb in range(2):
                        bk = 2 + hb
                        kb.op('act', ['pb%d' % bk], ['x1T'],
                              lambda e, hb=hb, bk=bk: e.activation(
                                  out=x1T[:, hb * 4:(hb + 1) * 4, i * 128:(i + 1) * 128],
                                  in_=pb[bk][:, :].rearrange("p (c t) -> p c t", c=4), func=AF.Copy))
                        kb.op('dve', ['pb%d' % bk], ['x1T32'],
                              lambda e, hb=hb, bk=bk: e.tensor_copy(
                                  out=x1T32[:, hb * 4:(hb + 1) * 4, :],
                                  in_=pb[bk][:, :].rearrange("p (c t) -> p c t", c=4)))
                    stage(4)
                    kb.op('act', [yk], [yk], lambda e: e.mul(yacc[:, i, :], yacc[:, i, :], ALPHA))
                    stage(5)
                    for c in range(8):
                        kb.op('pe', ['x1T32', 'rw32'], ['pb4'],
                              lambda e, c=c: e.matmul(pb[4][:, 0:N_EXP], x1T32[:, c, :], rw32[:, c, :],
                                                      start=(c == 0), stop=(c == 7)))
                    kb.op('dve', ['pb4', 'rbB'], ['lg'],
                          lambda e: e.tensor_tensor(out=lg[:, :], in0=pb[4][:, 0:N_EXP], in1=rbB[:, :], op=ALU.add))
                    kb.op('dve', ['lg'], ['m8'], lambda e: e.max(out=m8[:, :], in_=lg[:, :]))
                    kb.op('dve', ['lg', 'm8'], ['msk'],
                          lambda e: e.tensor_scalar(out=msk[:, :], in0=lg[:, :], scalar1=m8[:, 3:4], scalar2=None,
                                                    op0=ALU.is_ge))
                    kb.op('dve', ['m8'], ['nm'],
                          lambda e: e.tensor_scalar(out=nm[:, :], in0=m8[:, 0:1], scalar1=-1.0, scalar2=None,
                                                    op0=ALU.mult))
                    kb.op('act', ['lg', 'nm'], ['ex'],
                          lambda e: e.activation(out=ex[:, :], in_=lg[:, :], func=AF.Exp, bias=nm[:, 0:1], scale=1.0))
                    kb.op('dve', ['ex', 'msk'], ['ex'],
                          lambda e: e.tensor_tensor(out=ex[:, :], in0=ex[:, :], in1=msk[:, :], op=ALU.mult))
                    kb.op('dve', ['ex'], ['den'], lambda e: e.reduce_sum(out=den[:, :], in_=ex[:, :], axis=AX.X))
                    kb.op('dve', ['den'], ['den'], lambda e: e.reciprocal(out=den[:, :], in_=den[:, :]))
                    kb.op('dve', ['ex', 'den'], ['gate'],
                          lambda e: e.tensor_scalar(out=gate[:, i, :], in0=ex[:, :], scalar1=den[:, 0:1], scalar2=None,
                                                    op0=ALU.mult))

                    stage(6)
                stage(7)
                pair = 0
                obn = 0
                for e_ in range(n_exp):
                    kb.dma('sp', [], ['b2t'], b2t[:, :], b2[e_:e_ + 1, :])
                    for fcb in range(2):
                        sa = nuse[0] % NSLOT
                        sl = (nuse[0] + 1) % NSLOT
                        for tg in range(PT // 512):
                            for f4 in range(4):
                                fc = fcb * 4 + f4
                                hg = pair % 2
                                hl = 2 + pair % 2
                                tb = pair % 2
                                pair += 1
                                for c in range(8):
                                    kb.op('pe', ['slot%d' % sa, 'x1T'], ['pb%d' % hg],
                                          lambda e, c=c, hg=hg, sa=sa, f4=f4, tg=tg: e.matmul(
                                              pb[hg][:, :], slots[sa][:, c, f4 * 128:(f4 + 1) * 128],
                                              x1T[:, c, tg * 512:(tg + 1) * 512], start=(c == 0), stop=(c == 7)))
                                for c in range(8):
                                    kb.op('pe', ['slot%d' % sl, 'x1T'], ['pb%d' % hl],
                                          lambda e, c=c, hl=hl, sl=sl, f4=f4, tg=tg: e.matmul(
                                              pb[hl][:, :], slots[sl][:, c, f4 * 128:(f4 + 1) * 128],
                                              x1T[:, c, tg * 512:(tg + 1) * 512], start=(c == 0), stop=(c == 7)))
                                G, S, L = g32[tb], s32[tb], l32[tb]
                                gk, sk, lk = 'g32_%d' % tb, 's32_%d' % tb, 'l32_%d' % tb
                                kb.op('dve', ['pb%d' % hg, 'b1s'], [gk],
                                      lambda e, G=G, hg=hg, fc=fc: e.tensor_scalar(
                                          out=G[:, :], in0=pb[hg][:, :], scalar1=b1s[:, e_, fc:fc + 1], scalar2=7.0,
                                          op0=ALU.add, op1=ALU.min))
                                kb.op('act', [gk], [sk],
                                      lambda e, G=G, S=S: e.activation(out=S[:, :], in_=G[:, :], func=AF.Sigmoid, scale=1.702))
                                kb.op('dve', ['pb%d' % hl, 'b1s'], [lk],
                                      lambda e, L=L, hl=hl, fc=fc: e.tensor_scalar(
                                          out=L[:, :], in0=pb[hl][:, :], scalar1=b1s[:, e_, 8 + fc:9 + fc], scalar2=-7.0,
                                          op0=ALU.add, op1=ALU.max))
                                kb.op('dve', [lk], [lk],
                                      lambda e, L=L: e.tensor_scalar(out=L[:, :], in0=L[:, :], scalar1=7.0, scalar2=1.0,
                                                                     op0=ALU.min, op1=ALU.add))
                                kb.op('dve', [gk, sk], [sk],
                                      lambda e, G=G, S=S: e.tensor_tensor(out=S[:, :], in0=G[:, :], in1=S[:, :], op=ALU.mult))
                                kb.op('dve', [sk, lk], ['actT'],
                                      lambda e, S=S, L=L, fc=fc, tg=tg: e.tensor_tensor(
                                          out=actT[:, fc, tg * 512:(tg + 1) * 512], in0=S[:, :], in1=L[:, :], op=ALU.mult))
                        nuse[0] += 2
                        load_next_block()
                        load_next_block()
                    stage(8)
                    for h in range(2):
                        s2 = nuse[0] % NSLOT
                        for tt in range(TT):
                            ob = 4 + obn % 3
                            obn += 1
                            kb.op('pe', ['ones32', 'b2t'], ['pb%d' % ob],
                                  lambda e, ob=ob, h=h: e.matmul(pb[ob][:, :], ones32[0:1, :], b2t[0:1, h * 512:(h + 1) * 512],
                                                                 start=True, stop=False))
                            for fc in range(8):
                                kb.op('pe', ['actT', 'slot%d' % s2], ['pb%d' % ob],
                                      lambda e, ob=ob, fc=fc, tt=tt, s2=s2: e.matmul(
                                          pb[ob][:, :], actT[:, fc, tt * 128:(tt + 1) * 128], slots[s2][:, fc, :],
                                          start=False, stop=(fc == 7)))
                            yk = 'yacc%d' % tt
                            kb.op('dve', ['pb%d' % ob, 'gate', yk], [yk],
                                  lambda e, ob=ob, tt=tt, h=h: e.scalar_tensor_tensor(
                                      out=yacc[:, tt, h * 512:(h + 1) * 512], in0=pb[ob][:, :],
                                      scalar=gate[:, tt, e_:e_ + 1], in1=yacc[:, tt, h * 512:(h + 1) * 512],
                                      op0=ALU.mult, op1=ALU.add))
                        nuse[0] += 1
                        load_next_block()

                stage(9)
                kb.dma('sp', [], ['gB'], gB[:, :], ln2g.partition_broadcast(128))
                kb.dma('sp', [], ['bB'], bB[:, :], ln2b.partition_broadcast(128))
                for i in range(TT):
                    yk = 'yacc%d' % i
                    layer_norm_inplace(kb, yacc[:, i, :], yk, (gB, 'gB'), (bB, 'bB'), stats, mv, rs, '')
                    kb.dma('sp', [yk], ['out'], out[tok0 + i * 128:tok0 + (i + 1) * 128, :], yacc[:, i, :])
        except _Stop:
            pass
        kb.finish('sp')
    return nc


def build_sb(n_pairs=2, n_qg=16):
    nc = bass.Bass("TRN2", target_bir_lowering=False)
    S = SEQ
    xT = _din(nc, "xT", [D, S])
    wq = _din(nc, "wq", [D, 256])
    wk = _din(nc, "wk", [D, 256])
    wv = _din(nc, "wv", [D, 256])
    maskd = _din(nc, "mask", [128, 4, 512])
    negtri_d = _din(nc, "negtri", [128, 128])
    yT = _dout(nc, "yT", [256, S])
    with ExitStack() as es:
        kb = KB(nc, es)
        qT = [kb.sb("qT%d" % i, [64, S], BF16) for i in range(2)]
        kT = [kb.sb("kT%d" % i, [64, S], BF16) for i in range(2)]
        v = kb.sb("v", [128, S // 128, 128], BF16)
        xTb = [kb.sb("xTb%d" % i, [128, 8, 512], BF16) for i in range(2)]
        wqb = kb.sb("wqb", [128, 8, 128], BF16)
        wkb = kb.sb("wkb", [128, 8, 128], BF16)
        wvb = kb.sb("wvb", [128, 8, 128], BF16)
        e_t = [kb.sb("e_t%d" % i, [128, 512], F32) for i in range(2)]
        sp_t = [kb.sb("sp_t%d" % i, [128, 512], F32) for i in range(3)]
        att_t = [kb.sb("att_t%d" % i, [128, 512], BF16) for i in range(2)]
        sacc = [kb.sb("sacc%d" % i, [128, 512], F32) for i in range(2)]
        mask = kb.sb("mask", [128, 4, 512], F32)
        negtri = kb.sb("negtri", [128, 128], F32)
        negones = kb.sb("negones", [128, 128], F32)
        osb = [kb.sb("osb%d" % i, [64, 512], F32) for i in range(2)]
        pb = [kb.ps("pb%d" % i, [128, 512]) for i in range(8)]

        kb.dma('sp', [], ['mask'], mask[:, :, :], maskd[:, :, :])
        kb.dma('sp', [], ['negtri'], negtri[:, :], negtri_d[:, :])
        kb.op('dve', [], ['negones'], lambda e: e.memset(negones[:, :], -1.0))

        for hp in range(n_pairs):
            c0 = hp * 128
            kb.dma('pool', [], ['wqb'], wqb[:, :, :], wq[:, c0:c0 + 128].rearrange("(c p) n -> p c n", p=128))
            kb.dma('pool', [], ['wkb'], wkb[:, :, :], wk[:, c0:c0 + 128].rearrange("(c p) n -> p c n", p=128))
            kb.dma('pool', [], ['wvb'], wvb[:, :, :], wv[:, c0:c0 + 128].rearrange("(c p) n -> p c n", p=128))
            for tg in range(S // 512):
                xb = xTb[tg % 2]
                xk = 'xTb%d' % (tg % 2)
                kb.dma('pool', [], [xk], xb[:, :, :],
                       xT[:, tg * 512:(tg + 1) * 512].rearrange("(c p) t -> p c t", p=128))
                for hl in range(2):
                    for which in range(2):
                        wb, wkey = (wqb, 'wqb') if which == 0 else (wkb, 'wkb')
                        bk = 6 + which
                        for c in range(8):
                            kb.op('pe', [wkey, xk], ['pb%d' % bk],
                                  lambda e, c=c: e.matmul(pb[bk][0:64, :], wb[:, c, hl * 64:(hl + 1) * 64], xb[:, c, :],
                                                          start=(c == 0), stop=(c == 7)))
                        if which == 0:
                            kb.op('act', ['pb%d' % bk], ['qT%d' % hl],
                                  lambda e: e.activation(out=qT[hl][:, tg * 512:(tg + 1) * 512], in_=pb[bk][0:64, :],
                                                         func=AF.Copy, scale=0.125))
                        else:
                            kb.op('dve', ['pb%d' % bk], ['kT%d' % hl],
                                  lambda e: e.tensor_copy(out=kT[hl][:, tg * 512:(tg + 1) * 512], in_=pb[bk][0:64, :]))
                for tt in range(4):
                    tile_i = tg * 4 + tt
                    bk = 4 + tt % 2
                    for c in range(8):
                        kb.op('pe', ['wvb', xk], ['pb%d' % bk],
                              lambda e, c=c: e.matmul(pb[bk][:, 0:128], xb[:, c, tt * 128:(tt + 1) * 128], wvb[:, c, :],
                                                      start=(c == 0), stop=(c == 7)))
                    if tt % 2 == 0:
                        kb.op('act', ['pb%d' % bk], ['v'],
                              lambda e: e.activation(out=v[:, tile_i, :], in_=pb[bk][:, 0:128], func=AF.Copy))
                    else:
                        kb.op('dve', ['pb%d' % bk], ['v'],
                              lambda e: e.tensor_copy(out=v[:, tile_i, :], in_=pb[bk][:, 0:128]))

            tiles = []
            g = 0
            for hl in range(2):
                for qg in range(n_qg):
                    for kt in range(4 * qg + 3, -1, -1):
                        tiles.append(dict(hl=hl, qg=qg, kt=kt, j=(kt - 4 * qg if kt >= 4 * qg else None),
                                          first=(kt == 4 * qg + 3), last=(kt == 0), g=g, i=len(tiles)))
                    g += 1
            n = len(tiles)

            def S1(t):
                i = t['i']
                kb.op('pe', ['kT%d' % t['hl'], 'qT%d' % t['hl']], ['pb%d' % (i % 2)],
                      lambda e: e.matmul(pb[i % 2][:, :], kT[t['hl']][:, t['kt'] * 128:(t['kt'] + 1) * 128],
                                         qT[t['hl']][:, t['qg'] * 512:(t['qg'] + 1) * 512], start=True, stop=True))

            def S2(t):
                i = t['i']
                ek, sk = 'e_t%d' % (i % 2), 'sp_t%d' % (i % 3)
                kb.op('act', ['pb%d' % (i % 2)], [ek],
                      lambda e: e.activation(out=e_t[i % 2][:, :], in_=pb[i % 2][:, :], func=AF.Exp))
                kb.op('act', [ek], [sk],
                      lambda e: e.activation(out=sp_t[i % 3][:, :], in_=e_t[i % 2][:, :], func=AF.Ln, bias=1.0, scale=1.0))
                if t['j'] is not None:
                    kb.op('dve', [sk, 'mask'], [sk],
                          lambda e: e.tensor_tensor(out=sp_t[i % 3][:, :], in0=sp_t[i % 3][:, :],
                                                    in1=mask[:, t['j'], :], op=ALU.mult))

            def S3(t):
                i = t['i']
                zb = 2 + i % 2
                sk = 'sp_t%d' % (i % 3)
                sa = t['g'] % 2
                kb.op('pe', ['kT%d' % t['hl'], 'qT%d' % t['hl']], ['pb%d' % zb],
                      lambda e: e.matmul(pb[zb][:, :], kT[t['hl']][:, t['kt'] * 128:(t['kt'] + 1) * 128],
                                         qT[t['hl']][:, t['qg'] * 512:(t['qg'] + 1) * 512], start=True, stop=False))
                kb.op('pe', ['negtri', sk], ['pb%d' % zb],
                      lambda e: e.matmul(pb[zb][:, :], negtri[:, :], sp_t[i % 3][:, :], start=False, stop=t['first']))
                if not t['first']:
                    kb.op('pe', ['negones', 'sacc%d' % sa], ['pb%d' % zb],
                          lambda e: e.matmul(pb[zb][:, :], negones[:, :], sacc[sa][:, :], start=False, stop=True))
                if not t['last']:
                    if t['first']:
                        kb.op('dve', [sk], ['sacc%d' % sa],
                              lambda e: e.tensor_copy(out=sacc[sa][:, :], in_=sp_t[i % 3][:, :]))
                    else:
                        kb.op('dve', [sk, 'sacc%d' % sa], ['sacc%d' % sa],
                              lambda e: e.tensor_tensor(out=sacc[sa][:, :], in0=sacc[sa][:, :], in1=sp_t[i % 3][:, :],
                                                        op=ALU.add))

            def S4(t):
                i = t['i']
                zb = 2 + i % 2
                ak = 'att_t%d' % (i % 2)
                kb.op('act', ['pb%d' % zb], [ak],
                      lambda e: e.activation(out=att_t[i % 2][:, :], in_=pb[zb][:, :], func=AF.Exp))
                if t['j'] is not None:
                    kb.op('dve', [ak, 'mask'], [ak],
                          lambda e: e.tensor_tensor(out=att_t[i % 2][:, :], in0=att_t[i % 2][:, :],
                                                    in1=mask[:, t['j'], :], op=ALU.mult))

            def S5(t):
                i = t['i']
                ob = 4 + t['g'] % 2
                ak = 'att_t%d' % (i % 2)
                kb.op('pe', ['v', ak], ['pb%d' % ob],
                      lambda e: e.matmul(pb[ob][0:64, :], v[:, t['kt'], t['hl'] * 64:(t['hl'] + 1) * 64], att_t[i % 2][:, :],
                                         start=t['first'], stop=t['last']))
                if t['last']:
                    ok = 'osb%d' % (t['g'] % 2)
                    kb.op('dve', ['pb%d' % ob], [ok],
                          lambda e: e.tensor_copy(out=osb[t['g'] % 2][:, :], in_=pb[ob][0:64, :]))
                    r0 = (hp * 2 + t['hl']) * 64
                    kb.dma('sp', [ok], ['yT'], yT[r0:r0 + 64, t['qg'] * 512:(t['qg'] + 1) * 512], osb[t['g'] % 2][:, :])

            for s in range(n + 4):
                if s < n:
                    S1(tiles[s])
                if 0 <= s - 1 < n:
                    S2(tiles[s - 1])
                if 0 <= s - 2 < n:
                    S3(tiles[s - 2])
                if 0 <= s - 3 < n:
                    S4(tiles[s - 3])
                if 0 <= s - 4 < n:
                    S5(tiles[s - 4])
        kb.finish('sp')
    return nc


def sb_mask():
    s = np.arange(128)[:, None, None]
    j = np.arange(4)[None, :, None]
    t = np.arange(512)[None, None, :]
    return np.ascontiguousarray((128 * j + s < t).astype(np.float32))


def sb_negtri():
    j = np.arange(128)[:, None]
    s = np.arange(128)[None, :]
    return np.ascontiguousarray(-(j >= s).astype(np.float32))


RW_EPS = 64e-5


def build_rwkv(n_groups=16):
    nc = bass.Bass("TRN2", target_bir_lowering=False)
    S = SEQ
    xT = _din(nc, "xT", [D, S])
    wr = _din(nc, "wr", [D, 128])
    wk = _din(nc, "wk", [D, 128])
    wv = _din(nc, "wv", [D, 128])
    wlo = _din(nc, "wlo", [D, 160])
    pch = _din(nc, "pch", [64, 2, 12])
    plo = _din(nc, "plo", [96, 4])
    w2h = _din(nc, "w2h", [32, 128])
    a2h = _din(nc, "a2h", [32, 128])
    g2h = _din(nc, "g2h", [96, 128])
    cst = _din(nc, "cst", [64, 6, 512])
    yaT = _dout(nc, "yaT", [128, S])
    with ExitStack() as es:
        kb = KB(nc, es)
        xTb = [kb.sb("xTb%d" % i, [128, 8, 512], BF16) for i in range(2)]
        wrb = kb.sb("wrb", [128, 8, 128], BF16)
        wkb = kb.sb("wkb", [128, 8, 128], BF16)
        wvb = kb.sb("wvb", [128, 8, 128], BF16)
        wlob = kb.sb("wlob", [128, 8, 160], BF16)
        pc = kb.sb("pc", [64, 2, 12], F32)
        pl = kb.sb("pl", [96, 4], F32)
        omk = kb.sb("omk", [64, 2], F32)
        w2s = kb.sb("w2s", [32, 128], F32)
        a2s = kb.sb("a2s", [32, 128], F32)
        g2s = kb.sb("g2s", [96, 128], F32)
        cs_ = kb.sb("cst", [64, 6, 512], F32)
        epst = kb.sb("epst", [64, 1], F32)
        praw = {}
        for sig in ['r', 'k', 'v']:
            for hl in range(2):
                for b in range(2):
                    praw[(sig, hl, b)] = kb.sb("praw_%s%d%d" % (sig, hl, b), [64, 513], F32)
        for sig, npart in [('w', 32), ('a', 32), ('g', 96)]:
            for b in range(2):
                praw[(sig, 0, b)] = kb.sb("praw_%s%d" % (sig, b), [npart, 513], F32)
        wlo_s = kb.sb("wlo_s", [32, 512], F32)
        alo_s = kb.sb("alo_s", [32, 512], F32)
        glo_s = kb.sb("glo_s", [96, 512], F32)
        tmp96 = kb.sb("tmp96", [96, 512], F32)
        names = ['R', 'K', 'V', 'A', 'G', 'LW', 'CW', 'E1', 'T1', 'T2', 'T3', 'BON', 'P0', 'P0T', 'Pa', 'PaT', 'Pb',
                 'PbT', 'TT', 'AakT', 'ArbT', 'ArkT', 'Btok', 'Ktok', 'Vtok', 'ysb']
        W = {nm: kb.sb("w_" + nm, [64, 512], F32) for nm in names}
        Xs = kb.sb("Xs", [64, 64], F32)
        Us = kb.sb("Us", [64, 64], F32)
        H = [[kb.sb("H%d%d" % (hl, i), [64, 64], F32) for i in range(2)] for hl in range(2)]
        osb = kb.sb("osb", [64, 512], F32)
        pb = [kb.ps("pb%d" % i, [128, 512]) for i in range(8)]

        def dve(r, w, fn):
            kb.op('dve', r, w, fn)

        def act(r, w, fn):
            kb.op('act', r, w, fn)

        def pe(r, w, fn):
            kb.op('pe', r, w, fn)

        kb.dma('sp', [], ['pc'], pc[:, :, :], pch[:, :, :])
        kb.dma('sp', [], ['pl'], pl[:, :], plo[:, :])
        kb.dma('sp', [], ['w2s'], w2s[:, :], w2h[:, :])
        kb.dma('sp', [], ['a2s'], a2s[:, :], a2h[:, :])
        kb.dma('sp', [], ['g2s'], g2s[:, :], g2h[:, :])
        kb.dma('sp', [], ['cst'], cs_[:, :, :], cst[:, :, :])
        kb.dma('pool', [], ['wrb'], wrb[:, :, :], wr.rearrange("(c p) n -> p c n", p=128))
        kb.dma('pool', [], ['wkb'], wkb[:, :, :], wk.rearrange("(c p) n -> p c n", p=128))
        kb.dma('pool', [], ['wvb'], wvb[:, :, :], wv.rearrange("(c p) n -> p c n", p=128))
        kb.dma('pool', [], ['wlob'], wlob[:, :, :], wlo.rearrange("(c p) n -> p c n", p=128))
        dve([], ['epst'], lambda e: e.memset(epst[:, :], RW_EPS))
        dve(['pc'], ['omk'], lambda e: e.tensor_scalar(out=omk[:, :], in0=pc[:, :, 6], scalar1=-1.0, scalar2=1.0,
                                                       op0=ALU.mult, op1=ALU.add))
        for hl in range(2):
            dve([], ['H%d0' % hl], lambda e: e.memset(H[hl][0][:, :], 0.0))
        hcur = [0, 0]
        MK0, MK1, MK2, IDR, ONE, ONEM = (cs_[:, i, :] for i in range(6))
        ident64 = cs_[:, 3, 0:64]
        ones64 = cs_[:, 4, 0:64]
        onesm64 = cs_[:, 5, 0:64]
        nbk = [0]

        def bank():
            nbk[0] += 1
            return nbk[0] % 2

        for tg in range(n_groups):
            b = tg % 2
            xb = xTb[b]
            xk = 'xTb%d' % b
            kb.dma('pool', [], [xk], xb[:, :, :], xT[:, tg * 512:(tg + 1) * 512].rearrange("(c p) t -> p c t", p=128))

            def proj(wb, wkey, c0, m, dst, dkey, prev):
                bk = bank()
                for c in range(8):
                    pe([wkey, xk], ['pb%d' % bk],
                       lambda e, c=c: e.matmul(pb[bk][0:m, :], wb[:, c, c0:c0 + m], xb[:, c, :], start=(c == 0), stop=(c == 7)))
                act(['pb%d' % bk], [dkey], lambda e: e.activation(out=dst[:, 1:513], in_=pb[bk][0:m, :], func=AF.Copy))
                if tg == 0:
                    dve([dkey], [dkey], lambda e: e.memset(dst[:, 0:1], 0.0))
                else:
                    dve([dkey, prev[1]], [dkey], lambda e: e.tensor_copy(out=dst[:, 0:1], in_=prev[0][:, 512:513]))

            def shift(src, skey, mu, out, okey, tmp, tkey):
                dve([skey], [tkey], lambda e: e.tensor_tensor(out=tmp, in0=src[:, 0:512], in1=src[:, 1:513], op=ALU.subtract))
                dve([skey, tkey, 'pc', 'pl'], [okey],
                    lambda e: e.scalar_tensor_tensor(out=out, in0=tmp, scalar=mu, in1=src[:, 1:513], op0=ALU.mult, op1=ALU.add))

            for hl in range(2):
                for si, (sig, wb, wkey) in enumerate([('r', wrb, 'wrb'), ('k', wkb, 'wkb'), ('v', wvb, 'wvb')]):
                    proj(wb, wkey, hl * 64, 64, praw[(sig, hl, b)], 'praw_%s%d%d' % (sig, hl, b),
                         (praw[(sig, hl, 1 - b)], 'praw_%s%d%d' % (sig, hl, 1 - b)))
            for sig, c0, m in [('w', 0, 32), ('a', 32, 32), ('g', 64, 96)]:
                proj(wlob, 'wlob', c0, m, praw[(sig, 0, b)], 'praw_%s%d' % (sig, b),
                     (praw[(sig, 0, 1 - b)], 'praw_%s%d' % (sig, 1 - b)))
            shift(praw[('w', 0, b)], 'praw_w%d' % b, pl[0:32, 0:1], wlo_s[:, :], 'wlo_s', tmp96[0:32, :], 'tmp96')
            shift(praw[('a', 0, b)], 'praw_a%d' % b, pl[0:32, 1:2], alo_s[:, :], 'alo_s', tmp96[0:32, :], 'tmp96')
            shift(praw[('g', 0, b)], 'praw_g%d' % b, pl[0:96, 2:3], glo_s[:, :], 'glo_s', tmp96[0:96, :], 'tmp96')
            act(['wlo_s'], ['wlo_s'], lambda e: e.activation(out=wlo_s[:, :], in_=wlo_s[:, :], func=AF.Tanh))
            act(['glo_s'], ['glo_s'], lambda e: e.activation(out=glo_s[:, :], in_=glo_s[:, :], func=AF.Sigmoid))

            for hl in range(2):
                P = lambda i: pc[:, hl, i:i + 1]
                R, K, V, A, G, LW, CW, E1, T1, T2, T3, BON = (W[n][:, :] for n in
                                                                ['R', 'K', 'V', 'A', 'G', 'LW', 'CW', 'E1', 'T1', 'T2', 'T3', 'BON'])
                for si, (sig, dst, dk) in enumerate([('r', R, 'R'), ('k', K, 'K'), ('v', V, 'V')]):
                    shift(praw[(sig, hl, b)], 'praw_%s%d%d' % (sig, hl, b), P(si), dst, dk, T3, 'T3')
                hc = slice(hl * 64, (hl + 1) * 64)
                bk = bank()
                pe(['w2s', 'wlo_s'], ['pb%d' % bk], lambda e: e.matmul(pb[bk][0:64, :], w2s[:, hc], wlo_s[:, :], start=True, stop=True))
                act(['pb%d' % bk, 'pc'], ['LW'], lambda e: e.activation(out=LW, in_=pb[bk][0:64, :], func=AF.Sigmoid, bias=P(3), scale=1.0))
                dve(['LW'], ['LW'], lambda e: e.tensor_scalar(out=LW, in0=LW, scalar1=-0.6065306597126334, scalar2=None, op0=ALU.mult))
                bk = bank()
                pe(['a2s', 'alo_s'], ['pb%d' % bk], lambda e: e.matmul(pb[bk][0:64, :], a2s[:, hc], alo_s[:, :], start=True, stop=True))
                act(['pb%d' % bk, 'pc'], ['A'], lambda e: e.activation(out=A, in_=pb[bk][0:64, :], func=AF.Sigmoid, bias=P(4), scale=1.0))
                bk = bank()
                pe(['g2s', 'glo_s'], ['pb%d' % bk], lambda e: e.matmul(pb[bk][0:64, :], g2s[:, hc], glo_s[:, :], start=True, stop=True))
                act(['pb%d' % bk], ['G'], lambda e: e.activation(out=G, in_=pb[bk][0:64, :], func=AF.Copy))
                dve(['K', 'pc'], ['T1'], lambda e: e.tensor_scalar(out=T1, in0=K, scalar1=P(5), scalar2=None, op0=ALU.mult))
                dve(['T1'], ['T2'], lambda e: e.tensor_tensor(out=T2, in0=T1, in1=T1, op=ALU.mult))
                bk = bank()
                pe(['cst', 'T2'], ['pb%d' % bk], lambda e: e.matmul(pb[bk][0:64, :], ones64, T2, start=True, stop=True))
                act(['pb%d' % bk], ['T2'], lambda e: e.activation(out=T2, in_=pb[bk][0:64, :], func=AF.Sqrt))
                dve(['T2'], ['T2'], lambda e: e.tensor_scalar(out=T2, in0=T2, scalar1=1e-12, scalar2=None, op0=ALU.max))
                dve(['T2'], ['T2'], lambda e: e.reciprocal(out=T2, in_=T2))
                dve(['T1', 'T2'], ['T1'], lambda e: e.tensor_tensor(out=T1, in0=T1, in1=T2, op=ALU.mult))
                dve(['A', 'pc', 'omk'], ['T2'], lambda e: e.tensor_scalar(out=T2, in0=A, scalar1=P(6), scalar2=omk[:, hl:hl + 1],
                                                                         op0=ALU.mult, op1=ALU.add))
                dve(['K', 'T2'], ['K'], lambda e: e.tensor_tensor(out=K, in0=K, in1=T2, op=ALU.mult))
                dve(['T1', 'A'], ['A'], lambda e: e.tensor_tensor(out=A, in0=T1, in1=A, op=ALU.mult))
                dve(['T1'], ['T1'], lambda e: e.tensor_scalar(out=T1, in0=T1, scalar1=-1.0, scalar2=None, op0=ALU.mult))
                dve(['R', 'K', 'pc'], ['T2'], lambda e: e.scalar_tensor_tensor(out=T2, in0=R, scalar=P(8), in1=K, op0=ALU.mult, op1=ALU.mult))
                bk = bank()
                pe(['cst', 'T2'], ['pb%d' % bk], lambda e: e.matmul(pb[bk][0:64, :], ones64, T2, start=True, stop=True))
                dve(['V', 'pb%d' % bk], ['BON'], lambda e: e.tensor_tensor(out=BON, in0=V, in1=pb[bk][0:64, :], op=ALU.mult))
                for c in range(8):
                    cs = slice(c * 64, (c + 1) * 64)
                    dve(['LW', 'cst'], ['CW'], lambda e, cs=cs: e.tensor_tensor_scan(out=CW[:, cs], data0=ONE[:, 0:64], data1=LW[:, cs],
                                                                                    initial=0.0, op0=ALU.mult, op1=ALU.add))
                act(['CW'], ['E1'], lambda e: e.activation(out=E1, in_=CW, func=AF.Exp))
                act(['CW'], ['T3'], lambda e: e.activation(out=T3, in_=CW, func=AF.Exp, scale=-1.0))
                dve(['R', 'E1'], ['R'], lambda e: e.tensor_tensor(out=R, in0=R, in1=E1, op=ALU.mult))
                dve(['K', 'T3'], ['K'], lambda e: e.tensor_tensor(out=K, in0=K, in1=T3, op=ALU.mult))
                dve(['A', 'T3'], ['A'], lambda e: e.tensor_tensor(out=A, in0=A, in1=T3, op=ALU.mult))
                dve(['CW', 'LW'], ['T3'], lambda e: e.tensor_tensor(out=T3, in0=CW, in1=LW, op=ALU.subtract))
                act(['T3'], ['T3'], lambda e: e.activation(out=T3, in_=T3, func=AF.Exp))
                dve(['T1', 'T3'], ['T1'], lambda e: e.tensor_tensor(out=T1, in0=T1, in1=T3, op=ALU.mult))
                CH = [slice(c * 64, (c + 1) * 64) for c in range(8)]

                def batched(bk, lk, L_, rk, R_, transpose=False):
                    for cs in CH:
                        if transpose:
                            pe([lk, 'cst'], ['pb%d' % bk], lambda e, cs=cs: e.transpose(pb[bk][0:64, cs], L_[:, cs], ident64))
                        else:
                            pe([lk, rk], ['pb%d' % bk],
                               lambda e, cs=cs: e.matmul(pb[bk][0:64, cs], L_[:, cs], R_[:, cs], start=True, stop=True))

                def evac_mask(bk, dst, dkey, m):
                    dve(['pb%d' % bk, 'cst'], [dkey], lambda e: e.tensor_tensor(out=dst, in0=pb[bk][0:64, :], in1=m, op=ALU.mult))

                P0, P0T, TT, AakT, ArbT, ArkT, Btok, Ktok, Vtok, ysb = (W[n][:, :] for n in
                                                                         ['P0', 'P0T', 'TT', 'AakT', 'ArbT', 'ArkT', 'Btok', 'Ktok', 'Vtok', 'ysb'])
                batched(2, 'A', A, 'T1', T1)
                evac_mask(2, P0T, 'P0T', MK0)
                batched(3, 'T1', T1, 'A', A)
                evac_mask(3, P0, 'P0', MK1)
                batched(2, 'K', K, 'T1', T1)
                evac_mask(2, AakT, 'AakT', MK0)
                batched(3, 'A', A, 'R', R)
                evac_mask(3, ArbT, 'ArbT', MK2)
                batched(2, 'K', K, 'R', R)
                evac_mask(2, ArkT, 'ArkT', MK2)
                batched(3, 'A', A, None, None, transpose=True)
                act(['pb3'], ['Btok'], lambda e: e.activation(out=Btok, in_=pb[3][0:64, :], func=AF.Copy))
                batched(2, 'K', K, None, None, transpose=True)
                act(['pb2'], ['Ktok'], lambda e: e.activation(out=Ktok, in_=pb[2][0:64, :], func=AF.Copy))
                batched(3, 'V', V, None, None, transpose=True)
                act(['pb3'], ['Vtok'], lambda e: e.activation(out=Vtok, in_=pb[3][0:64, :], func=AF.Copy))
                dve(['P0T', 'cst'], ['TT'], lambda e: e.tensor_tensor(out=TT, in0=P0T, in1=IDR, op=ALU.add))
                cur, curk, curT, curTk = P0, 'P0', P0T, 'P0T'
                for lvl in range(1, 6):
                    nn, nnT = ('Pa', 'PaT') if lvl % 2 == 1 else ('Pb', 'PbT')
                    Pn, PnT = W[nn][:, :], W[nnT][:, :]
                    batched(2, curTk, curT, curk, cur)
                    act(['pb2'], [nn], lambda e: e.activation(out=Pn, in_=pb[2][0:64, :], func=AF.Copy))
                    if lvl < 5:
                        batched(3, curk, cur, curTk, curT)
                        dve(['pb3'], [nnT], lambda e: e.tensor_copy(out=PnT, in_=pb[3][0:64, :]))
                    batched(2, nn, Pn, 'TT', TT)
                    dve(['pb2', 'TT'], ['TT'], lambda e: e.tensor_tensor(out=TT, in0=TT, in1=pb[2][0:64, :], op=ALU.add))
                    cur, curk, curT, curTk = Pn, nn, PnT, nnT
                sbk = 4 + hl
                ybk = 6 + hl
                for c in range(8):
                    cs = CH[c]
                    Hc = H[hl][hcur[hl]]
                    Hn = H[hl][1 - hcur[hl]]
                    hk, hnk = 'H%d%d' % (hl, hcur[hl]), 'H%d%d' % (hl, 1 - hcur[hl])
                    sk, yk = 'pb%d' % sbk, 'pb%d' % ybk
                    pe(['T1', hk], [sk], lambda e: e.matmul(pb[sbk][0:64, 0:64], T1[:, cs], Hc[:, :], start=True, stop=False))
                    pe(['AakT', 'Vtok'], [sk], lambda e: e.matmul(pb[sbk][0:64, 0:64], AakT[:, cs], Vtok[:, cs], start=False, stop=True))
                    act([sk], ['Xs'], lambda e: e.activation(out=Xs[:, :], in_=pb[sbk][0:64, 0:64], func=AF.Copy))
                    pe(['TT', 'Xs'], [sk], lambda e: e.matmul(pb[sbk][0:64, 64:128], TT[:, cs], Xs[:, :], start=True, stop=True))
                    dve([sk], ['Us'], lambda e: e.tensor_copy(out=Us[:, :], in_=pb[sbk][0:64, 64:128]))
                    pe([hk, 'R'], [yk], lambda e: e.matmul(pb[ybk][0:64, cs], Hc[:, :], R[:, cs], start=True, stop=False))
                    pe(['Us', 'ArbT'], [yk], lambda e: e.matmul(pb[ybk][0:64, cs], Us[:, :], ArbT[:, cs], start=False, stop=False))
                    pe(['Vtok', 'ArkT'], [yk], lambda e: e.matmul(pb[ybk][0:64, cs], Vtok[:, cs], ArkT[:, cs], start=False, stop=True))
                    pe(['cst', hk], [sk], lambda e: e.matmul(pb[sbk][0:64, 128:192], ident64, Hc[:, :], start=True, stop=False))
                    pe(['Btok', 'Us'], [sk], lambda e: e.matmul(pb[sbk][0:64, 128:192], Btok[:, cs], Us[:, :], start=False, stop=False))
                    pe(['Ktok', 'Vtok'], [sk], lambda e: e.matmul(pb[sbk][0:64, 128:192], Ktok[:, cs], Vtok[:, cs], start=False, stop=True))
                    dve([sk, 'E1'], [hnk], lambda e: e.tensor_scalar(out=Hn[:, :], in0=pb[sbk][0:64, 128:192],
                                                                    scalar1=E1[:, c * 64 + 63:c * 64 + 64], scalar2=None, op0=ALU.mult))
                    hcur[hl] = 1 - hcur[hl]
                yk = 'pb%d' % ybk
                act([yk], ['ysb'], lambda e: e.activation(out=ysb, in_=pb[ybk][0:64, :], func=AF.Copy))
                bk = bank()
                pe(['cst', 'ysb'], ['pb%d' % bk], lambda e: e.matmul(pb[bk][0:64, :], onesm64, ysb, start=True, stop=True))
                dve(['ysb', 'pb%d' % bk], ['ysb'], lambda e: e.tensor_tensor(out=ysb, in0=ysb, in1=pb[bk][0:64, :], op=ALU.subtract))
                dve(['ysb'], ['T2'], lambda e: e.tensor_tensor(out=T2, in0=ysb, in1=ysb, op=ALU.mult))
                bk = bank()
                pe(['cst', 'T2'], ['pb%d' % bk], lambda e: e.matmul(pb[bk][0:64, :], onesm64, T2, start=True, stop=True))
                act(['pb%d' % bk, 'epst'], ['T2'], lambda e: e.activation(out=T2, in_=pb[bk][0:64, :], func=AF.Sqrt, bias=epst[:, 0:1], scale=1.0))
                dve(['T2'], ['T2'], lambda e: e.reciprocal(out=T2, in_=T2))
                dve(['ysb', 'T2'], ['ysb'], lambda e: e.tensor_tensor(out=ysb, in0=ysb, in1=T2, op=ALU.mult))
                dve(['ysb', 'pc'], ['ysb'], lambda e: e.tensor_scalar(out=ysb, in0=ysb, scalar1=P(9), scalar2=P(10), op0=ALU.mult, op1=ALU.add))
                dve(['ysb', 'BON'], ['ysb'], lambda e: e.tensor_tensor(out=ysb, in0=ysb, in1=BON, op=ALU.add))
                dve(['ysb', 'G'], ['osb'], lambda e: e.tensor_tensor(out=osb[:, :], in0=ysb, in1=G, op=ALU.mult))
                kb.dma('sp', ['osb'], ['yaT'], yaT[hl * 64:(hl + 1) * 64, tg * 512:(tg + 1) * 512], osb[:, :])
        kb.finish('sp')
    return nc


def rwkv_consts():
    j = np.arange(64)[:, None]
    t = np.arange(64)[None, :]
    c = np.zeros((64, 6, 512), np.float32)
    for i in range(8):
        sl = slice(i * 64, (i + 1) * 64)
        c[:, 0, sl] = (j < t)
        c[:, 1, sl] = (t < j)
        c[:, 2, sl] = (j <= t)
        c[:, 3, sl] = (j == t)
    c[:, 4, :] = 1.0
    c[:, 5, :] = 1.0 / 64.0
    return c


def rwkv_inputs(xT, ab, hg):
    w_in = ab['ab_w_in'][0]
    mu = ab['ab_shift_mu'][0]
    cols = np.arange(hg * 128, (hg + 1) * 128)
    pch = np.zeros((64, 2, 12), np.float32)
    for hl in range(2):
        h = 2 * hg + hl
        c = np.arange(h * 64, (h + 1) * 64)
        pch[:, hl, 0] = mu[c]
        pch[:, hl, 1] = mu[512 + c]
        pch[:, hl, 2] = mu[1024 + c]
        pch[:, hl, 3] = ab['ab_w0'][0][c]
        pch[:, hl, 4] = ab['ab_a0'][0][c]
        pch[:, hl, 5] = ab['ab_k_k'][0][c]
        pch[:, hl, 6] = ab['ab_k_a'][0][c]
        pch[:, hl, 8] = ab['ab_r_k'][0][h]
        pch[:, hl, 9] = ab['ab_lnx_g'][0][c]
        pch[:, hl, 10] = ab['ab_lnx_b'][0][c]
    plo = np.zeros((96, 4), np.float32)
    plo[:32, 0] = mu[1536:1568]
    plo[:32, 1] = mu[1568:1600]
    plo[:96, 2] = mu[1600:1696]
    return {
        "xT": xT,
        "wr": np.ascontiguousarray(w_in[:, cols]),
        "wk": np.ascontiguousarray(w_in[:, 512 + cols]),
        "wv": np.ascontiguousarray(w_in[:, 1024 + cols]),
        "wlo": np.ascontiguousarray(w_in[:, 1536:1696]),
        "pch": pch, "plo": plo,
        "w2h": np.ascontiguousarray(ab['ab_w2'][0][:, cols]),
        "a2h": np.ascontiguousarray(ab['ab_a2'][0][:, cols]),
        "g2h": np.ascontiguousarray(ab['ab_g2'][0][:, cols]),
        "cst": rwkv_consts(),
    }


def build_moba(n_blocks=32):
    nc = bass.Bass("TRN2", target_bir_lowering=False)
    S = SEQ
    xT = _din(nc, "xT", [D, S])
    wq = _din(nc, "wq", [D, 128])
    wk = _din(nc, "wk", [D, 128])
    wv = _din(nc, "wv", [D, 128])
    cosd = _din(nc, "cosf", [64, S])
    sind = _din(nc, "sinf", [64, S])
    protd = _din(nc, "prot", [64, 64])
    ebd = _din(nc, "eb", [33, 32, 128])
    cmd = _din(nc, "cmask", [128, 2, 256])
    identd = _din(nc, "ident", [128, 128])
    ybT = _dout(nc, "ybT", [128, S])
    with ExitStack() as es:
        kb = KB(nc, es)
        qTm = [kb.sb("qTm%d" % i, [64, S], BF16) for i in range(2)]
        kTm = [kb.sb("kTm%d" % i, [64, S], BF16) for i in range(2)]
        vm = kb.sb("vm", [128, S // 128, 128], BF16)
        biasT = [kb.sb("biasT%d" % i, [33, S], BF16) for i in range(2)]
        xTb = [kb.sb("xTb%d" % i, [128, 8, 256], BF16) for i in range(2)]
        wqb = kb.sb("wqb", [128, 8, 128], BF16)
        wkb = kb.sb("wkb", [128, 8, 128], BF16)
        wvb = kb.sb("wvb", [128, 8, 128], BF16)
        Eb = kb.sb("Eb", [33, 32, 128], BF16)
        cmask = kb.sb("cmask", [128, 2, 256], F32)
        ident = kb.sb("ident", [128, 128], F32)
        prot = kb.sb("prot", [64, 64], F32)
        ones32 = kb.sb("ones32", [64, 128], F32)
        onesb = kb.sb("onesb", [128, 64], BF16)
        ct = kb.sb("ct", [64, 256], F32)
        st = kb.sb("st", [64, 256], F32)
        qs = kb.sb("qs", [64, 256], F32)
        t1 = kb.sb("t1", [64, 256], F32)
        t2 = kb.sb("t2", [64, 256], F32)
        qf = kb.sb("qf", [64, 256], F32)
        kf = kb.sb("kf", [64, 256], F32)
        sq = kb.sb("sq", [64, 256], F32)
        kmean = [kb.sb("kmean%d" % i, [64, 32], F32) for i in range(2)]
        kmax2 = [kb.sb("kmax2_%d" % i, [128, 1], F32) for i in range(2)]
        kmx = kb.sb("kmx", [128, 1], F32)
        gsb = kb.sb("gsb", [128, 32], F32)
        m8 = kb.sb("m8", [128, 8], F32)
        selt = kb.sb("selt", [128, 33], F32)
        mt = kb.sb("mt", [128, 1], F32)
        att = [kb.sb("att%d" % i, [128, 256], BF16) for i in range(3)]
        rec = kb.sb("rec", [64, 256], F32)
        yo = [kb.sb("yo%d" % i, [64, 256], F32) for i in range(2)]
        pb = [kb.ps("pb%d" % i, [128, 512]) for i in range(8)]

        def dve(r, w, fn):
            kb.op('dve', r, w, fn)

        def act(r, w, fn):
            kb.op('act', r, w, fn)

        def pe(r, w, fn):
            kb.op('pe', r, w, fn)

        kb.dma('sp', [], ['cmask'], cmask[:, :, :], cmd[:, :, :])
        kb.dma('sp', [], ['ident'], ident[:, :], identd[:, :])
        kb.dma('sp', [], ['prot'], prot[:, :], protd[:, :])
        kb.dma('pool', [], ['Eb'], Eb[:, :, :], ebd[:, :, :])
        kb.dma('pool', [], ['wqb'], wqb[:, :, :], wq.rearrange("(c p) n -> p c n", p=128))
        kb.dma('pool', [], ['wkb'], wkb[:, :, :], wk.rearrange("(c p) n -> p c n", p=128))
        kb.dma('pool', [], ['wvb'], wvb[:, :, :], wv.rearrange("(c p) n -> p c n", p=128))
        dve([], ['ones32'], lambda e: e.memset(ones32[:, :], 1.0))
        dve([], ['onesb'], lambda e: e.memset(onesb[:, :], 1.0))
        for hl in range(2):
            dve([], ['kmean%d' % hl], lambda e: e.memset(kmean[hl][:, :], 0.0))
            dve([], ['kmax2_%d' % hl], lambda e: e.memset(kmax2[hl][:, :], 0.0))
        nbk = [0]

        def bank():
            nbk[0] += 1
            return nbk[0] % 8

        for n in range(n_blocks):
            b = n % 2
            xb, xk = xTb[b], 'xTb%d' % b
            cols = slice(n * 256, (n + 1) * 256)
            kb.dma('pool', [], [xk], xb[:, :, :], xT[:, cols].rearrange("(c p) t -> p c t", p=128))
            kb.dma('sp', [], ['ct'], ct[:, :], cosd[:, cols])
            kb.dma('sp', [], ['st'], st[:, :], sind[:, cols])

            def rope_proj(wb, wkey, hl, outf, okey):
                bk = bank()
                for c in range(8):
                    pe([wkey, xk], ['pb%d' % bk],
                       lambda e, c=c: e.matmul(pb[bk][0:64, 0:256], wb[:, c, hl * 64:(hl + 1) * 64], xb[:, c, :],
                                               start=(c == 0), stop=(c == 7)))
                act(['pb%d' % bk], ['qs'], lambda e: e.activation(out=qs[:, :], in_=pb[bk][0:64, 0:256], func=AF.Copy))
                bk2 = bank()
                pe(['prot', 'qs'], ['pb%d' % bk2], lambda e: e.matmul(pb[bk2][0:64, 0:256], prot[:, :], qs[:, :], start=True, stop=True))
                dve(['qs', 'ct'], ['t1'], lambda e: e.tensor_tensor(out=t1[:, :], in0=qs[:, :], in1=ct[:, :], op=ALU.mult))
                dve(['pb%d' % bk2, 'st'], ['t2'], lambda e: e.tensor_tensor(out=t2[:, :], in0=pb[bk2][0:64, 0:256], in1=st[:, :], op=ALU.mult))
                dve(['t1', 't2'], [okey], lambda e: e.tensor_tensor(out=outf[:, :], in0=t1[:, :], in1=t2[:, :], op=ALU.add))

            for hl in range(2):
                rope_proj(wkb, 'wkb', hl, kf, 'kf')
                act(['kf'], ['kTm%d' % hl], lambda e: e.activation(out=kTm[hl][:, cols], in_=kf[:, :], func=AF.Copy))
                dve(['kf'], ['kmean%d' % hl], lambda e: e.reduce_sum(out=kmean[hl][:, n:n + 1], in_=kf[:, :], axis=AX.X))
                dve(['kf'], ['sq'], lambda e: e.tensor_tensor(out=sq[:, :], in0=kf[:, :], in1=kf[:, :], op=ALU.mult))
                bk = bank()
                pe(['ones32', 'sq'], ['pb%d' % bk], lambda e: e.matmul(pb[bk][:, 0:256], ones32[:, :], sq[:, :], start=True, stop=True))
                dve(['pb%d' % bk], ['kmx'], lambda e: e.reduce_max(out=kmx[:, :], in_=pb[bk][:, 0:256], axis=AX.X))
                dve(['kmx', 'kmax2_%d' % hl], ['kmax2_%d' % hl],
                    lambda e: e.tensor_tensor(out=kmax2[hl][:, :], in0=kmax2[hl][:, :], in1=kmx[:, :], op=ALU.max))
                rope_proj(wqb, 'wqb', hl, qf, 'qf')
                act(['qf'], ['qTm%d' % hl], lambda e: e.activation(out=qTm[hl][:, cols], in_=qf[:, :], func=AF.Copy, scale=0.125))
                dve(['qf'], ['sq'], lambda e: e.tensor_tensor(out=sq[:, :], in0=qf[:, :], in1=qf[:, :], op=ALU.mult))
                for tt in range(2):
                    ts_ = slice(tt * 128, (tt + 1) * 128)
                    bk = bank()
                    pk = 'pb%d' % bk
                    pe(['qf', 'kmean%d' % hl], [pk], lambda e: e.matmul(pb[bk][:, 0:32], qf[:, ts_], kmean[hl][:, :], start=True, stop=True))
                    pe(['sq', 'ones32'], [pk], lambda e: e.matmul(pb[bk][:, 32:33], sq[:, ts_], ones32[:, 0:1], start=True, stop=True))
                    if n == 0:
                        dve([], ['selt'], lambda e: e.memset(selt[:, 0:32], 0.0))
                    else:
                        dve([pk], ['gsb'], lambda e: e.tensor_copy(out=gsb[:, :], in_=pb[bk][:, 0:32]))
                        dve(['gsb'], ['gsb'], lambda e: e.memset(gsb[:, n:32], -1e30))
                        dve(['gsb'], ['m8'], lambda e: e.max(out=m8[:, :], in_=gsb[:, :]))
                        dve(['gsb', 'm8'], ['selt'], lambda e: e.tensor_scalar(out=selt[:, 0:32], in0=gsb[:, :], scalar1=m8[:, 2:3],
                                                                              scalar2=None, op0=ALU.is_ge))
                        dve(['selt'], ['selt'], lambda e: e.memset(selt[:, n:32], 0.0))
                    dve(['selt'], ['selt'], lambda e: e.memset(selt[:, n:n + 1], 1.0))
                    dve(['selt'], ['selt'], lambda e: e.tensor_scalar(out=selt[:, 0:32], in0=selt[:, 0:32], scalar1=-1.0, scalar2=30000.0,
                                                                     op0=ALU.add, op1=ALU.mult))
                    dve([pk, 'kmax2_%d' % hl], ['mt'], lambda e: e.tensor_tensor(out=mt[:, :], in0=pb[bk][:, 32:33], in1=kmax2[hl][:, :], op=ALU.mult))
                    act(['mt'], ['mt'], lambda e: e.activation(out=mt[:, :], in_=mt[:, :], func=AF.Sqrt))
                    dve(['mt', 'selt'], ['selt'], lambda e: e.tensor_scalar(out=selt[:, 32:33], in0=mt[:, :], scalar1=-0.125, scalar2=None, op0=ALU.mult))
                    bk2 = bank()
                    pe(['selt', 'ident'], ['pb%d' % bk2], lambda e: e.transpose(pb[bk2][0:33, 0:128], selt[:, :], ident[:, :]))
                    act(['pb%d' % bk2], ['biasT%d' % hl],
                        lambda e: e.activation(out=biasT[hl][:, n * 256 + tt * 128:n * 256 + (tt + 1) * 128], in_=pb[bk2][0:33, 0:128], func=AF.Copy))
            for tt in range(2):
                bk = bank()
                for c in range(8):
                    pe(['wvb', xk], ['pb%d' % bk],
                       lambda e, c=c: e.matmul(pb[bk][:, 0:128], xb[:, c, tt * 128:(tt + 1) * 128], wvb[:, c, :], start=(c == 0), stop=(c == 7)))
                act(['pb%d' % bk], ['vm'], lambda e: e.activation(out=vm[:, 2 * n + tt, :], in_=pb[bk][:, 0:128], func=AF.Copy))

        tiles = []
        g = 0
        for hl in range(2):
            for n in range(n_blocks):
                for kt in range(2 * n + 2):
                    tiles.append(dict(hl=hl, n=n, kt=kt, first=(kt == 0), last=(kt == 2 * n + 1),
                                      j=(kt - 2 * n if kt >= 2 * n else None), g=g, i=len(tiles)))
                g += 1
        nt = len(tiles)

        def M1(t):
            i = t['i']
            sb_ = i % 2
            qc = slice(t['n'] * 256, (t['n'] + 1) * 256)
            pe(['kTm%d' % t['hl'], 'qTm%d' % t['hl']], ['pb%d' % sb_],
               lambda e: e.matmul(pb[sb_][:, 0:256], kTm[t['hl']][:, t['kt'] * 128:(t['kt'] + 1) * 128], qTm[t['hl']][:, qc],
                                  start=True, stop=False))
            pe(['Eb', 'biasT%d' % t['hl']], ['pb%d' % sb_],
               lambda e: e.matmul(pb[sb_][:, 0:256], Eb[:, t['kt'] // 2, :], biasT[t['hl']][:, qc], start=False, stop=True))

        def M2(t):
            i = t['i']
            ak = 'att%d' % (i % 3)
            act(['pb%d' % (i % 2)], [ak], lambda e: e.activation(out=att[i % 3][:, :], in_=pb[i % 2][:, 0:256], func=AF.Exp))
            if t['j'] is not None:
                dve([ak, 'cmask'], [ak], lambda e: e.tensor_tensor(out=att[i % 3][:, :], in0=att[i % 3][:, :], in1=cmask[:, t['j'], :], op=ALU.mult))

        def M3(t):
            i = t['i']
            ak = 'att%d' % (i % 3)
            ob, db = 2 + t['g'] % 2, 4 + t['g'] % 2
            pe(['vm', ak], ['pb%d' % ob],
               lambda e: e.matmul(pb[ob][0:64, 0:256], vm[:, t['kt'], t['hl'] * 64:(t['hl'] + 1) * 64], att[i % 3][:, :],
                                  start=t['first'], stop=t['last']))
            pe(['onesb', ak], ['pb%d' % db],
               lambda e: e.matmul(pb[db][0:64, 0:256], onesb[:, :], att[i % 3][:, :], start=t['first'], stop=t['last']))
            if t['last']:
                yk = 'yo%d' % (t['g'] % 2)
                dve(['pb%d' % db], ['rec'], lambda e: e.reciprocal(out=rec[:, :], in_=pb[db][0:64, 0:256]))
                dve(['pb%d' % ob, 'rec'], [yk], lambda e: e.tensor_tensor(out=yo[t['g'] % 2][:, :], in0=pb[ob][0:64, 0:256], in1=rec[:, :], op=ALU.mult))
                kb.dma('sp', [yk], ['ybT'], ybT[t['hl'] * 64:(t['hl'] + 1) * 64, t['n'] * 256:(t['n'] + 1) * 256], yo[t['g'] % 2][:, :])

        for s in range(nt + 2):
            if s < nt:
                M1(tiles[s])
            if 0 <= s - 1 < nt:
                M2(tiles[s - 1])
            if 0 <= s - 2 < nt:
                M3(tiles[s - 2])
        kb.finish('sp')
    return nc


def moba_consts():
    half = 8
    inv_freq = (np.float32(500000.0) ** (-np.arange(half, dtype=np.float32) / np.float32(half))).astype(np.float32)
    ang = (np.arange(SEQ, dtype=np.float32)[:, None] * inv_freq[None, :]).astype(np.float32)
    cos = np.cos(ang).astype(np.float32).T
    sin = np.sin(ang).astype(np.float32).T
    cosf = np.ones((64, SEQ), np.float32)
    sinf = np.zeros((64, SEQ), np.float32)
    cosf[0:8] = cos
    cosf[8:16] = cos
    sinf[0:8] = sin
    sinf[8:16] = sin
    prot = np.zeros((64, 64), np.float32)
    for m in range(8):
        prot[m + 8, m] = -1.0
        prot[m, m + 8] = 1.0
    eb = np.zeros((33, 32, 128), np.float32)
    for n in range(32):
        eb[n, n, :] = 1.0
    eb[32, :, :] = 1.0
    s = np.arange(128)[:, None, None]
    j = np.arange(2)[None, :, None]
    t = np.arange(256)[None, None, :]
    cmask = (128 * j + s <= t).astype(np.float32)
    return {"cosf": cosf, "sinf": sinf, "prot": prot, "eb": eb, "cmask": np.ascontiguousarray(cmask),
            "ident": np.eye(128, dtype=np.float32)}


def moba_inputs(xT, w_in, hg, consts):
    cols = 1696 + np.arange(hg * 128, (hg + 1) * 128)
    m = {"xT": xT, "wq": np.ascontiguousarray(w_in[:, cols]), "wk": np.ascontiguousarray(w_in[:, 512 + cols]),
         "wv": np.ascontiguousarray(w_in[:, 1024 + cols])}
    m.update(consts)
    return m


_PROGS = {}


def _prog(name, fn):
    if name not in _PROGS:
        _PROGS[name] = fn()
    return _PROGS[name]


def _run(nc, in_maps):
    res = run_bass_kernel_spmd(nc, in_maps, core_ids=list(range(NCORES)))
    return res.results


def _post_launch(yT_b, xres, w_out, i, inputs):
    ident = np.eye(128, dtype=np.float32)
    b1T = np.ascontiguousarray(inputs['exp_b1'][i].reshape(32, 16, 128).transpose(2, 0, 1))
    shared = {
        "w_out": np.ascontiguousarray(w_out), "ln1g": inputs['ln1_g'][i], "ln1b": inputs['ln1_b'][i],
        "rw": inputs['router_w'][i], "rb": inputs['router_b'][i], "w1": inputs['exp_w1'][i], "b1T": b1T,
        "w2": inputs['exp_w2'][i], "b2": inputs['exp_b2'][i], "ln2g": inputs['ln2_g'][i], "ln2b": inputs['ln2_b'][i],
        "ident": ident,
    }
    maps = []
    for c in range(NCORES):
        b, s0 = c // 4, (c % 4) * 2048
        m = dict(shared)
        m["yT"] = np.ascontiguousarray(yT_b[b][:, s0:s0 + 2048])
        m["xr"] = np.ascontiguousarray(xres[b, s0:s0 + 2048, :])
        maps.append(m)
    res = _run(_prog("post", build_post), maps)
    out = np.empty((NB, SEQ, D), np.float32)
    for c in range(NCORES):
        b, s0 = c // 4, (c % 4) * 2048
        out[b, s0:s0 + 2048, :] = res[c]["out"]
    return out


def kernel(**inputs):
    inputs = {k: np.asarray(v) for k, v in inputs.items()}
    x = inputs['x'].astype(np.float32, copy=False)
    xT = [np.ascontiguousarray(x[b].T) for b in range(NB)]
    ab = {k: v for k, v in inputs.items() if k.startswith('ab_')}
    maps = [rwkv_inputs(xT[c // 4], ab, c % 4) for c in range(NCORES)]
    res = _run(_prog("rwkv", build_rwkv), maps)
    yT0 = [np.empty((D, SEQ), np.float32) for _ in range(NB)]
    for c in range(NCORES):
        b, hg = c // 4, c % 4
        yT0[b][hg * 128:(hg + 1) * 128, :] = res[c]["yaT"]
    mc = moba_consts()
    maps = [moba_inputs(xT[c // 4], ab['ab_w_in'][0], c % 4, mc) for c in range(NCORES)]
    res = _run(_prog("moba", build_moba), maps)
    for c in range(NCORES):
        b, hg = c // 4, c % 4
        yT0[b][512 + hg * 128:512 + (hg + 1) * 128, :] = res[c]["ybT"]
    x2 = _post_launch(yT0, x, ab['ab_w_out'][0], 0, inputs)
    xT2 = [np.ascontiguousarray(x2[b].T) for b in range(NB)]
    w_in = inputs['sb_w_in'][0]
    mask, negtri = sb_mask(), sb_negtri()
    maps = []
    for c in range(NCORES):
        b, hq = c // 4, c % 4
        cols = hq * 256 + np.arange(256)
        maps.append({"xT": xT2[b], "wq": np.ascontiguousarray(w_in[:, cols]), "wk": np.ascontiguousarray(w_in[:, 1024 + cols]),
                     "wv": np.ascontiguousarray(w_in[:, 2048 + cols]), "mask": mask, "negtri": negtri})
    res = _run(_prog("sb", build_sb), maps)
    yT1 = [np.empty((D, SEQ), np.float32) for _ in range(NB)]
    for c in range(NCORES):
        b, hq = c // 4, c % 4
        yT1[b][hq * 256:(hq + 1) * 256, :] = res[c]["yT"]
    out = _post_launch(yT1, x2, inputs['sb_w_out'][0], 1, inputs)
    return out
```

```python
import numpy as np
from contextlib import ExitStack
import concourse.bass as bass
import concourse.mybir as mybir
from concourse.bass_utils import run_bass_kernel_spmd

F32 = mybir.dt.float32
BF16 = mybir.dt.bfloat16
AF = mybir.ActivationFunctionType
ALU = mybir.AluOpType
AX = mybir.AxisListType

D = 1024
SEQ = 8192
NB = 2
NCORES = 8
ALPHA = float((2 * 2) ** 0.25)
LN_EPS = 1e-5
N_EXP = 32


class KB:
    def __init__(self, nc, es, n_dma_sems=24):
        self.nc = nc
        self.es = es
        self.E = {'pe': nc.tensor, 'dve': nc.vector, 'act': nc.scalar, 'pool': nc.gpsimd, 'sp': nc.sync}
        self.sem = {}
        self.cnt = {}
        for k in ['pe', 'dve', 'act', 'pool']:
            self.sem[k] = es.enter_context(nc.semaphore('s_' + k))
            self.cnt[k] = 0
        self.dsem = []
        for i in range(n_dma_sems):
            self.dsem.append(es.enter_context(nc.semaphore('d%d' % i)))
        self.dcnt = [0] * n_dma_sems
        self.dnext = 0
        self.waited = {k: {} for k in self.E}
        self.lastw = {}
        self.readers = {}
        self.ninst = 0

    def sb(self, name, shape, dt):
        return self.es.enter_context(self.nc.sbuf_tensor("sb_" + name, shape, dt))

    def ps(self, name, shape, dt=F32):
        return self.es.enter_context(self.nc.psum_tensor("ps_" + name, shape, dt))

    def _deps(self, reads, writes):
        deps = {}

        def add(t):
            if t is None:
                return
            s, v = t
            if deps.get(s, 0) < v:
                deps[s] = v
        for r in reads:
            add(self.lastw.get(r))
            if isinstance(r, str) and r.startswith('pb'):
                for t in self.readers.get(r, ()):
                    add(t)
        for w in writes:
            add(self.lastw.get(w))
            for t in self.readers.get(w, ()):
                add(t)
        return deps

    def _wait(self, eng, deps, skip=None):
        for s, v in deps.items():
            if skip is not None and s == skip:
                continue
            if self.waited[eng].get(s, 0) < v:
                semobj = self.sem[s] if isinstance(s, str) else self.dsem[s]
                self.E[eng].wait_ge(semobj, v)
                self.waited[eng][s] = v

    def _commit(self, tok, reads, writes):
        for r in reads:
            self.readers.setdefault(r, []).append(tok)
        for w in writes:
            self.lastw[w] = tok
            self.readers[w] = []

    def op(self, eng, reads, writes, emit):
        deps = self._deps(reads, writes)
        self._wait(eng, deps, skip='pe' if eng == 'pe' else None)
        inst = emit(self.E[eng])
        self.cnt[eng] += 1
        inst.then_inc(self.sem[eng], 1)
        self._commit((eng, self.cnt[eng]), reads, writes)
        self.ninst += 1
        return inst

    def dma(self, q, reads, writes, out, in_, **kw):
        deps = self._deps(reads, writes)
        i = self.dnext
        self.dnext = (self.dnext + 1) % len(self.dsem)
        if self.dcnt[i] > 0:
            deps[i] = max(deps.get(i, 0), self.dcnt[i])
        self._wait(q, deps)
        inst = self.E[q].dma_start(out=out, in_=in_, **kw)
        self.dcnt[i] += 16
        inst.then_inc(self.dsem[i], 16)
        self._commit((i, self.dcnt[i]), reads, writes)
        self.ninst += 1
        return inst

    def finish(self, eng='sp'):
        deps = {}
        for t in self.lastw.values():
            s, v = t
            if deps.get(s, 0) < v:
                deps[s] = v
        self._wait(eng, deps)


class _Stop(Exception):
    pass


import os as _os
_STAGE = int(_os.environ.get("KSTAGE", "0"))


def stage(k):
    if _STAGE == k:
        raise _Stop()


def _din(nc, name, shape, dt=F32):
    return nc.dram_tensor(name, list(shape), dt, kind="ExternalInput").ap()


def _dout(nc, name, shape, dt=F32):
    return nc.dram_tensor(name, list(shape), dt, kind="ExternalOutput").ap()


def layer_norm_inplace(kb, t, tkey, gB, bB, stats, mv, rs, sfx):
    kb.op('dve', [tkey], ['stats' + sfx], lambda e: e.bn_stats(out=stats[:, 0, :], in_=t[:, 0:512]))
    kb.op('dve', [tkey], ['stats' + sfx], lambda e: e.bn_stats(out=stats[:, 1, :], in_=t[:, 512:1024]))
    kb.op('dve', ['stats' + sfx], ['mv' + sfx],
          lambda e: e.bn_aggr(out=mv[:, :], in_=stats[:, :, :].rearrange("p a b -> p (a b)")))
    kb.op('act', ['mv' + sfx], ['rs' + sfx],
          lambda e: e.activation(out=rs[:, :], in_=mv[:, 1:2], func=AF.Sqrt, bias=kb.eps_t[:, 0:1], scale=1.0))
    kb.op('dve', ['rs' + sfx], ['rs' + sfx], lambda e: e.reciprocal(out=rs[:, :], in_=rs[:, :]))
    kb.op('dve', [tkey, 'mv' + sfx, 'rs' + sfx], [tkey],
          lambda e: e.tensor_scalar(out=t, in0=t, scalar1=mv[:, 0:1], scalar2=rs[:, 0:1],
                                    op0=ALU.subtract, op1=ALU.mult))
    kb.op('dve', [tkey, gB[1]], [tkey], lambda e: e.tensor_tensor(out=t, in0=t, in1=gB[0][:, :], op=ALU.mult))
    kb.op('dve', [tkey, bB[1]], [tkey], lambda e: e.tensor_tensor(out=t, in0=t, in1=bB[0][:, :], op=ALU.add))


def build_post(n_exp=N_EXP, npass=None):
    nc = bass.Bass("TRN2", target_bir_lowering=False)
    NTOK = 2048
    PT = 1024
    TT = PT // 128
    NPASS = npass or NTOK // PT
    yT = _din(nc, "yT", [D, NTOK])
    xr = _din(nc, "xr", [NTOK, D])
    w_out = _din(nc, "w_out", [D, D])
    ln1g = _din(nc, "ln1g", [D])
    ln1b = _din(nc, "ln1b", [D])
    rw = _din(nc, "rw", [D, N_EXP])
    rb = _din(nc, "rb", [N_EXP])
    w1 = _din(nc, "w1", [N_EXP, D, 2 * D])
    b1T = _din(nc, "b1T", [128, N_EXP, 16])
    w2 = _din(nc, "w2", [N_EXP, D, D])
    b2 = _din(nc, "b2", [N_EXP, D])
    ln2g = _din(nc, "ln2g", [D])
    ln2b = _din(nc, "ln2b", [D])
    ident_d = _din(nc, "ident", [128, 128])
    out = _dout(nc, "out", [NTOK, D])

    with ExitStack() as es:
        kb = KB(nc, es)
        yacc = kb.sb("yacc", [128, TT, D], F32)
        x1T = kb.sb("x1T", [128, 8, PT], BF16)
        gate = kb.sb("gate", [128, TT, N_EXP], F32)
        NSLOT = 6
        slots = [kb.sb("wslot%d" % i, [128, 8, 512], BF16) for i in range(NSLOT)]
        actT = kb.sb("actT", [128, 8, PT], BF16)
        g32 = [kb.sb("g32_%d" % i, [128, 512], F32) for i in range(2)]
        s32 = [kb.sb("s32_%d" % i, [128, 512], F32) for i in range(2)]
        l32 = [kb.sb("l32_%d" % i, [128, 512], F32) for i in range(2)]
        gB = kb.sb("gB", [128, D], F32)
        bB = kb.sb("bB", [128, D], F32)
        yTb = [kb.sb("yTb%d" % i, [128, 8, 128], BF16) for i in range(2)]
        xrt = kb.sb("xrt", [128, D], F32)
        x1T32 = kb.sb("x1T32", [128, 8, 128], F32)
        rw32 = kb.sb("rw32", [128, 8, N_EXP], F32)
        rbB = kb.sb("rbB", [128, N_EXP], F32)
        b1s = kb.sb("b1s", [128, N_EXP, 16], F32)
        b2t = kb.sb("b2t", [1, D], F32)
        ones32 = kb.sb("ones32", [1, 128], F32)
        ident = kb.sb("ident", [128, 128], F32)
        woutb = kb.sb("woutb", [128, 8, D], BF16)
        stats = kb.sb("stats", [128, 2, 6], F32)
        mv = kb.sb("mv", [128, 2], F32)
        rs = kb.sb("rs", [128, 1], F32)
        lg = kb.sb("lg", [128, N_EXP], F32)
        m8 = kb.sb("m8", [128, 8], F32)
        msk = kb.sb("msk", [128, N_EXP], F32)
        nm = kb.sb("nm", [128, 1], F32)
        ex = kb.sb("ex", [128, N_EXP], F32)
        den = kb.sb("den", [128, 1], F32)
        eps_t = kb.sb("eps_t", [128, 1], F32)
        kb.eps_t = eps_t
        pb = [kb.ps("pb%d" % i, [128, 512]) for i in range(8)]

        kb.op('dve', [], ['eps'], lambda e: e.memset(eps_t[:, :], LN_EPS))
        kb.op('dve', [], ['ones32'], lambda e: e.memset(ones32[:, :], 1.0))
        kb.dma('sp', [], ['ident'], ident[:, :], ident_d[:, :])
        kb.dma('sp', [], ['rw32'], rw32[:, :, :], rw.rearrange("(c p) e -> p c e", p=128))
        kb.dma('sp', [], ['rbB'], rbB[:, :], rb.partition_broadcast(128))
        kb.dma('sp', [], ['b1s'], b1s[:, :, :], b1T[:, :, :])
        kb.dma('pool', [], ['woutb'], woutb[:, :, :], w_out.rearrange("(c p) n -> p c n", p=128))

        blocks = []
        for p_ in range(NPASS):
            for e_ in range(n_exp):
                for b_ in range(6):
                    blocks.append((e_, b_))
        nload = [0]

        def load_next_block():
            n = nload[0]
            if n >= len(blocks):
                return
            e_, b_ = blocks[n]
            s = n % NSLOT
            if b_ < 4:
                c0 = [0, 1024, 512, 1536][b_]
                src = w1[e_, :, c0:c0 + 512].rearrange("(c p) n -> p c n", p=128)
            else:
                c0 = (b_ - 4) * 512
                src = w2[e_, :, c0:c0 + 512].rearrange("(c p) n -> p c n", p=128)
            kb.dma('pool', [], ['slot%d' % s], slots[s][:, :, :], src)
            nload[0] += 1

        for _ in range(NSLOT):
            load_next_block()
        nuse = [0]

        try:
            stage(1)
            for ps_ in range(NPASS):
                tok0 = ps_ * PT
                kb.dma('sp', ['eps'], ['gB'], gB[:, :], ln1g.partition_broadcast(128))
                kb.dma('sp', ['eps'], ['bB'], bB[:, :], ln1b.partition_broadcast(128))
                for i in range(TT):
                    t0 = tok0 + i * 128
                    yb = yTb[i % 2]
                    ybk = 'yTb%d' % (i % 2)
                    kb.dma('pool', [], [ybk], yb[:, :, :], yT[:, t0:t0 + 128].rearrange("(c p) t -> p c t", p=128))
                    kb.dma('sp', [], ['xrt'], xrt[:, :], xr[t0:t0 + 128, :])
                    for h in range(2):
                        for c in range(8):
                            kb.op('pe', [ybk, 'woutb'], ['pb%d' % h],
                                  lambda e, c=c, h=h: e.matmul(pb[h][:, :], yb[:, c, :], woutb[:, c, h * 512:(h + 1) * 512],
                                                               start=(c == 0), stop=(c == 7)))
                    yk = 'yacc%d' % i
                    for h in range(2):
                        kb.op('dve', ['xrt', 'pb%d' % h], [yk],
                              lambda e, h=h: e.scalar_tensor_tensor(out=yacc[:, i, h * 512:(h + 1) * 512],
                                                                    in0=xrt[:, h * 512:(h + 1) * 512], scalar=ALPHA,
                                                                    in1=pb[h][:, :], op0=ALU.mult, op1=ALU.add))
                    stage(2)
                    layer_norm_inplace(kb, yacc[:, i, :], yk, (gB, 'gB'), (bB, 'bB'), stats, mv, rs, '')
                    stage(3)
                    for c in range(8):
                        bk = 2 + c // 4
                        kb.op('pe', [yk, 'ident'], ['pb%d' % bk],
                              lambda e, c=c, bk=bk: e.transpose(pb[bk][:, (c % 4) * 128:(c % 4 + 1) * 128],
                                                                yacc[:, i, c * 128:(c + 1) * 128], ident[:, :]))
                    for hb in range(2):
                        bk = 2 + hb
                        kb.op('act', ['pb%d' % bk], ['x1T'],
                              lambda e, hb=hb, bk=bk: e.activation(
                                  out=x1T[:, hb * 4:(hb + 1) * 4, i * 128:(i + 1) * 128],
                                  in_=pb[bk][:, :].rearrange("p (c t) -> p c t", c=4), func=AF.Copy))
                        kb.op('dve', ['pb%d' % bk], ['x1T32'],
                              lambda e, hb=hb, bk=bk: e.tensor_copy(
                                  out=x1T32[:, hb * 4:(hb + 1) * 4, :],
                                  in_=pb[bk][:, :].rearrange("p (c t) -> p c t", c=4)))
                    stage(4)
                    kb.op('act', [yk], [yk], lambda e: e.mul(yacc[:, i, :], yacc[:, i, :], ALPHA))
                    stage(5)
                    for c in range(8):
                        kb.op('pe', ['x1T32', 'rw32'], ['pb4'],
                              lambda e, c=c: e.matmul(pb[4][:, 0:N_EXP], x1T32[:, c, :], rw32[:, c, :],
                                                      start=(c == 0), stop=(c == 7)))
                    kb.op('dve', ['pb4', 'rbB'], ['lg'],
                          lambda e: e.tensor_tensor(out=lg[:, :], in0=pb[4][:, 0:N_EXP], in1=rbB[:, :], op=ALU.add))
                    kb.op('dve', ['lg'], ['m8'], lambda e: e.max(out=m8[:, :], in_=lg[:, :]))
                    kb.op('dve', ['lg', 'm8'], ['msk'],
                          lambda e: e.tensor_scalar(out=msk[:, :], in0=lg[:, :], scalar1=m8[:, 3:4], scalar2=None,
                                                    op0=ALU.is_ge))
                    kb.op('dve', ['m8'], ['nm'],
                          lambda e: e.tensor_scalar(out=nm[:, :], in0=m8[:, 0:1], scalar1=-1.0, scalar2=None,
                                                    op0=ALU.mult))
                    kb.op('act', ['lg', 'nm'], ['ex'],
                          lambda e: e.activation(out=ex[:, :], in_=lg[:, :], func=AF.Exp, bias=nm[:, 0:1], scale=1.0))
                    kb.op('dve', ['ex', 'msk'], ['ex'],
                          lambda e: e.tensor_tensor(out=ex[:, :], in0=ex[:, :], in1=msk[:, :], op=ALU.mult))
                    kb.op('dve', ['ex'], ['den'], lambda e: e.reduce_sum(out=den[:, :], in_=ex[:, :], axis=AX.X))
                    kb.op('dve', ['den'], ['den'], lambda e: e.reciprocal(out=den[:, :], in_=den[:, :]))
                    kb.op('dve', ['ex', 'den'], ['gate'],
                          lambda e: e.tensor_scalar(out=gate[:, i, :], in0=ex[:, :], scalar1=den[:, 0:1], scalar2=None,
                                                    op0=ALU.mult))

                    stage(6)
                stage(7)
                pair = 0
                obn = 0
                for e_ in range(n_exp):
                    kb.dma('sp', [], ['b2t'], b2t[:, :], b2[e_:e_ + 1, :])
                    for fcb in range(2):
                        sa = nuse[0] % NSLOT
                        sl = (nuse[0] + 1) % NSLOT
                        for tg in range(PT // 512):
                            for f4 in range(4):
                                fc = fcb * 4 + f4
                                hg = pair % 2
                                hl = 2 + pair % 2
                                tb = pair % 2
                                pair += 1
                                for c in range(8):
                                    kb.op('pe', ['slot%d' % sa, 'x1T'], ['pb%d' % hg],
                                          lambda e, c=c, hg=hg, sa=sa, f4=f4, tg=tg: e.matmul(
                                              pb[hg][:, :], slots[sa][:, c, f4 * 128:(f4 + 1) * 128],
                                              x1T[:, c, tg * 512:(tg + 1) * 512], start=(c == 0), stop=(c == 7)))
                                for c in range(8):
                                    kb.op('pe', ['slot%d' % sl, 'x1T'], ['pb%d' % hl],
                                          lambda e, c=c, hl=hl, sl=sl, f4=f4, tg=tg: e.matmul(
                                              pb[hl][:, :], slots[sl][:, c, f4 * 128:(f4 + 1) * 128],
                                              x1T[:, c, tg * 512:(tg + 1) * 512], start=(c == 0), stop=(c == 7)))
                                G, S, L = g32[tb], s32[tb], l32[tb]
                                gk, sk, lk = 'g32_%d' % tb, 's32_%d' % tb, 'l32_%d' % tb
                                kb.op('dve', ['pb%d' % hg, 'b1s'], [gk],
                                      lambda e, G=G, hg=hg, fc=fc: e.tensor_scalar(
                                          out=G[:, :], in0=pb[hg][:, :], scalar1=b1s[:, e_, fc:fc + 1], scalar2=7.0,
                                          op0=ALU.add, op1=ALU.min))
                                kb.op('act', [gk], [sk],
                                      lambda e, G=G, S=S: e.activation(out=S[:, :], in_=G[:, :], func=AF.Sigmoid, scale=1.702))
                                kb.op('dve', ['pb%d' % hl, 'b1s'], [lk],
                                      lambda e, L=L, hl=hl, fc=fc: e.tensor_scalar(
                                          out=L[:, :], in0=pb[hl][:, :], scalar1=b1s[:, e_, 8 + fc:9 + fc], scalar2=-7.0,
                                          op0=ALU.add, op1=ALU.max))
                                kb.op('dve', [lk], [lk],
                                      lambda e, L=L: e.tensor_scalar(out=L[:, :], in0=L[:, :], scalar1=7.0, scalar2=1.0,
                                                                     op0=ALU.min, op1=ALU.add))
                                kb.op('dve', [gk, sk], [sk],
                                      lambda e, G=G, S=S: e.tensor_tensor(out=S[:, :], in0=G[:, :], in1=S[:, :], op=ALU.mult))
                                kb.op('dve', [sk, lk], ['actT'],
                                      lambda e, S=S, L=L, fc=fc, tg=tg: e.tensor_tensor(
                                          out=actT[:, fc, tg * 512:(tg + 1) * 512], in0=S[:, :], in1=L[:, :], op=ALU.mult))
                        nuse[0] += 2
                        load_next_block()
                        load_next_block()
                    stage(8)
                    for h in range(2):
                        s2 = nuse[0] % NSLOT
                        for tt in range(TT):
                            ob = 4 + obn % 3
                            obn += 1
                            kb.op('pe', ['ones32', 'b2t'], ['pb%d' % ob],
                                  lambda e, ob=ob, h=h: e.matmul(pb[ob][:, :], ones32[0:1, :], b2t[0:1, h * 512:(h + 1) * 512],
                                                                 start=True, stop=False))
                            for fc in range(8):
                                kb.op('pe', ['actT', 'slot%d' % s2], ['pb%d' % ob],
                                      lambda e, ob=ob, fc=fc, tt=tt, s2=s2: e.matmul(
                                          pb[ob][:, :], actT[:, fc, tt * 128:(tt + 1) * 128], slots[s2][:, fc, :],
                                          start=False, stop=(fc == 7)))
                            yk = 'yacc%d' % tt
                            kb.op('dve', ['pb%d' % ob, 'gate', yk], [yk],
                                  lambda e, ob=ob, tt=tt, h=h: e.scalar_tensor_tensor(
                                      out=yacc[:, tt, h * 512:(h + 1) * 512], in0=pb[ob][:, :],
                                      scalar=gate[:, tt, e_:e_ + 1], in1=yacc[:, tt, h * 512:(h + 1) * 512],
                                      op0=ALU.mult, op1=ALU.add))
                        nuse[0] += 1
                        load_next_block()

                stage(9)
                kb.dma('sp', [], ['gB'], gB[:, :], ln2g.partition_broadcast(128))
                kb.dma('sp', [], ['bB'], bB[:, :], ln2b.partition_broadcast(128))
                for i in range(TT):
                    yk = 'yacc%d' % i
                    layer_norm_inplace(kb, yacc[:, i, :], yk, (gB, 'gB'), (bB, 'bB'), stats, mv, rs, '')
                    kb.dma('sp', [yk], ['out'], out[tok0 + i * 128:tok0 + (i + 1) * 128, :], yacc[:, i, :])
        except _Stop:
            pass
        kb.finish('sp')
    return nc


def build_sb(n_pairs=2, n_qg=16):
    nc = bass.Bass("TRN2", target_bir_lowering=False)
    S = SEQ
    xT = _din(nc, "xT", [D, S])
    wq = _din(nc, "wq", [D, 256])
    wk = _din(nc, "wk", [D, 256])
    wv = _din(nc, "wv", [D, 256])
    maskd = _din(nc, "mask", [128, 4, 512])
    negtri_d = _din(nc, "negtri", [128, 128])
    yT = _dout(nc, "yT", [256, S])
    with ExitStack() as es:
        kb = KB(nc, es)
        qT = [kb.sb("qT%d" % i, [64, S], BF16) for i in range(2)]
        kT = [kb.sb("kT%d" % i, [64, S], BF16) for i in range(2)]
        v = kb.sb("v", [128, S // 128, 128], BF16)
        xTb = [kb.sb("xTb%d" % i, [128, 8, 512], BF16) for i in range(2)]
        wqb = kb.sb("wqb", [128, 8, 128], BF16)
        wkb = kb.sb("wkb", [128, 8, 128], BF16)
        wvb = kb.sb("wvb", [128, 8, 128], BF16)
        e_t = [kb.sb("e_t%d" % i, [128, 512], F32) for i in range(2)]
        sp_t = [kb.sb("sp_t%d" % i, [128, 512], F32) for i in range(3)]
        att_t = [kb.sb("att_t%d" % i, [128, 512], BF16) for i in range(2)]
        sacc = [kb.sb("sacc%d" % i, [128, 512], F32) for i in range(2)]
        mask = kb.sb("mask", [128, 4, 512], F32)
        negtri = kb.sb("negtri", [128, 128], F32)
        negones = kb.sb("negones", [128, 128], F32)
        osb = [kb.sb("osb%d" % i, [64, 512], F32) for i in range(2)]
        pb = [kb.ps("pb%d" % i, [128, 512]) for i in range(8)]

        kb.dma('sp', [], ['mask'], mask[:, :, :], maskd[:, :, :])
        kb.dma('sp', [], ['negtri'], negtri[:, :], negtri_d[:, :])
        kb.op('dve', [], ['negones'], lambda e: e.memset(negones[:, :], -1.0))

        for hp in range(n_pairs):
            c0 = hp * 128
            kb.dma('pool', [], ['wqb'], wqb[:, :, :], wq[:, c0:c0 + 128].rearrange("(c p) n -> p c n", p=128))
            kb.dma('pool', [], ['wkb'], wkb[:, :, :], wk[:, c0:c0 + 128].rearrange("(c p) n -> p c n", p=128))
            kb.dma('pool', [], ['wvb'], wvb[:, :, :], wv[:, c0:c0 + 128].rearrange("(c p) n -> p c n", p=128))
            for tg in range(S // 512):
                xb = xTb[tg % 2]
                xk = 'xTb%d' % (tg % 2)
                kb.dma('pool', [], [xk], xb[:, :, :],
                       xT[:, tg * 512:(tg + 1) * 512].rearrange("(c p) t -> p c t", p=128))
                for hl in range(2):
                    for which in range(2):
                        wb, wkey = (wqb, 'wqb') if which == 0 else (wkb, 'wkb')
                        bk = 6 + which
                        for c in range(8):
                            kb.op('pe', [wkey, xk], ['pb%d' % bk],
                                  lambda e, c=c: e.matmul(pb[bk][0:64, :], wb[:, c, hl * 64:(hl + 1) * 64], xb[:, c, :],
                                                          start=(c == 0), stop=(c == 7)))
                        if which == 0:
                            kb.op('act', ['pb%d' % bk], ['qT%d' % hl],
                                  lambda e: e.activation(out=qT[hl][:, tg * 512:(tg + 1) * 512], in_=pb[bk][0:64, :],
                                                         func=AF.Copy, scale=0.125))
                        else:
                            kb.op('dve', ['pb%d' % bk], ['kT%d' % hl],
                                  lambda e: e.tensor_copy(out=kT[hl][:, tg * 512:(tg + 1) * 512], in_=pb[bk][0:64, :]))
                for tt in range(4):
                    tile_i = tg * 4 + tt
                    bk = 4 + tt % 2
                    for c in range(8):
                        kb.op('pe', ['wvb', xk], ['pb%d' % bk],
                              lambda e, c=c: e.matmul(pb[bk][:, 0:128], xb[:, c, tt * 128:(tt + 1) * 128], wvb[:, c, :],
                                                      start=(c == 0), stop=(c == 7)))
                    if tt % 2 == 0:
                        kb.op('act', ['pb%d' % bk], ['v'],
                              lambda e: e.activation(out=v[:, tile_i, :], in_=pb[bk][:, 0:128], func=AF.Copy))
                    else:
                        kb.op('dve', ['pb%d' % bk], ['v'],
                              lambda e: e.tensor_copy(out=v[:, tile_i, :], in_=pb[bk][:, 0:128]))

            tiles = []
            g = 0
            for hl in range(2):
                for qg in range(n_qg):
                    for kt in range(4 * qg + 3, -1, -1):
                        tiles.append(dict(hl=hl, qg=qg, kt=kt, j=(kt - 4 * qg if kt >= 4 * qg else None),
                                          first=(kt == 4 * qg + 3), last=(kt == 0), g=g, i=len(tiles)))
                    g += 1
            n = len(tiles)

            def S1(t):
                i = t['i']
                kb.op('pe', ['kT%d' % t['hl'], 'qT%d' % t['hl']], ['pb%d' % (i % 2)],
                      lambda e: e.matmul(pb[i % 2][:, :], kT[t['hl']][:, t['kt'] * 128:(t['kt'] + 1) * 128],
                                         qT[t['hl']][:, t['qg'] * 512:(t['qg'] + 1) * 512], start=True, stop=True))

            def S2(t):
                i = t['i']
                ek, sk = 'e_t%d' % (i % 2), 'sp_t%d' % (i % 3)
                kb.op('act', ['pb%d' % (i % 2)], [ek],
                      lambda e: e.activation(out=e_t[i % 2][:, :], in_=pb[i % 2][:, :], func=AF.Exp))
                kb.op('act', [ek], [sk],
                      lambda e: e.activation(out=sp_t[i % 3][:, :], in_=e_t[i % 2][:, :], func=AF.Ln, bias=1.0, scale=1.0))
                if t['j'] is not None:
                    kb.op('dve', [sk, 'mask'], [sk],
                          lambda e: e.tensor_tensor(out=sp_t[i % 3][:, :], in0=sp_t[i % 3][:, :],
                                                    in1=mask[:, t['j'], :], op=ALU.mult))

            def S3(t):
                i = t['i']
                zb = 2 + i % 2
                sk = 'sp_t%d' % (i % 3)
                sa = t['g'] % 2
                kb.op('pe', ['kT%d' % t['hl'], 'qT%d' % t['hl']], ['pb%d' % zb],
                      lambda e: e.matmul(pb[zb][:, :], kT[t['hl']][:, t['kt'] * 128:(t['kt'] + 1) * 128],
                                         qT[t['hl']][:, t['qg'] * 512:(t['qg'] + 1) * 512], start=True, stop=False))
                kb.op('pe', ['negtri', sk], ['pb%d' % zb],
                      lambda e: e.matmul(pb[zb][:, :], negtri[:, :], sp_t[i % 3][:, :], start=False, stop=t['first']))
                if not t['first']:
                    kb.op('pe', ['negones', 'sacc%d' % sa], ['pb%d' % zb],
                          lambda e: e.matmul(pb[zb][:, :], negones[:, :], sacc[sa][:, :], start=False, stop=True))
                if not t['last']:
                    if t['first']:
                        kb.op('dve', [sk], ['sacc%d' % sa],
                              lambda e: e.tensor_copy(out=sacc[sa][:, :], in_=sp_t[i % 3][:, :]))
                    else:
                        kb.op('dve', [sk, 'sacc%d' % sa], ['sacc%d' % sa],
                              lambda e: e.tensor_tensor(out=sacc[sa][:, :], in0=sacc[sa][:, :], in1=sp_t[i % 3][:, :],
                                                        op=ALU.add))

            def S4(t):
                i = t['i']
                zb = 2 + i % 2
                ak = 'att_t%d' % (i % 2)
                kb.op('act', ['pb%d' % zb], [ak],
                      lambda e: e.activation(out=att_t[i % 2][:, :], in_=pb[zb][:, :], func=AF.Exp))
                if t['j'] is not None:
                    kb.op('dve', [ak, 'mask'], [ak],
                          lambda e: e.tensor_tensor(out=att_t[i % 2][:, :], in0=att_t[i % 2][:, :],
                                                    in1=mask[:, t['j'], :], op=ALU.mult))

            def S5(t):
                i = t['i']
                ob = 4 + t['g'] % 2
                ak = 'att_t%d' % (i % 2)
                kb.op('pe', ['v', ak], ['pb%d' % ob],
                      lambda e: e.matmul(pb[ob][0:64, :], v[:, t['kt'], t['hl'] * 64:(t['hl'] + 1) * 64], att_t[i % 2][:, :],
                                         start=t['first'], stop=t['last']))
                if t['last']:
                    ok = 'osb%d' % (t['g'] % 2)
                    kb.op('dve', ['pb%d' % ob], [ok],
                          lambda e: e.tensor_copy(out=osb[t['g'] % 2][:, :], in_=pb[ob][0:64, :]))
                    r0 = (hp * 2 + t['hl']) * 64
                    kb.dma('sp', [ok], ['yT'], yT[r0:r0 + 64, t['qg'] * 512:(t['qg'] + 1) * 512], osb[t['g'] % 2][:, :])

            for s in range(n + 4):
                if s < n:
                    S1(tiles[s])
                if 0 <= s - 1 < n:
                    S2(tiles[s - 1])
                if 0 <= s - 2 < n:
                    S3(tiles[s - 2])
                if 0 <= s - 3 < n:
                    S4(tiles[s - 3])
                if 0 <= s - 4 < n:
                    S5(tiles[s - 4])
        kb.finish('sp')
    return nc


def sb_mask():
    s = np.arange(128)[:, None, None]
    j = np.arange(4)[None, :, None]
    t = np.arange(512)[None, None, :]
    return np.ascontiguousarray((128 * j + s < t).astype(np.float32))


def sb_negtri():
    j = np.arange(128)[:, None]
    s = np.arange(128)[None, :]
    return np.ascontiguousarray(-(j >= s).astype(np.float32))


RW_EPS = 64e-5


def build_rwkv(n_groups=16):
    nc = bass.Bass("TRN2", target_bir_lowering=False)
    S = SEQ
    xT = _din(nc, "xT", [D, S])
    wr = _din(nc, "wr", [D, 128])
    wk = _din(nc, "wk", [D, 128])
    wv = _din(nc, "wv", [D, 128])
    wlo = _din(nc, "wlo", [D, 160])
    pch = _din(nc, "pch", [64, 2, 12])
    plo = _din(nc, "plo", [96, 4])
    w2h = _din(nc, "w2h", [32, 128])
    a2h = _din(nc, "a2h", [32, 128])
    g2h = _din(nc, "g2h", [96, 128])
    cst = _din(nc, "cst", [64, 6, 512])
    yaT = _dout(nc, "yaT", [128, S])
    with ExitStack() as es:
        kb = KB(nc, es)
        xTb = [kb.sb("xTb%d" % i, [128, 8, 512], BF16) for i in range(2)]
        wrb = kb.sb("wrb", [128, 8, 128], BF16)
        wkb = kb.sb("wkb", [128, 8, 128], BF16)
        wvb = kb.sb("wvb", [128, 8, 128], BF16)
        wlob = kb.sb("wlob", [128, 8, 160], BF16)
        pc = kb.sb("pc", [64, 2, 12], F32)
        pl = kb.sb("pl", [96, 4], F32)
        omk = kb.sb("omk", [64, 2], F32)
        w2s = kb.sb("w2s", [32, 128], F32)
        a2s = kb.sb("a2s", [32, 128], F32)
        g2s = kb.sb("g2s", [96, 128], F32)
        cs_ = kb.sb("cst", [64, 6, 512], F32)
        epst = kb.sb("epst", [64, 1], F32)
        praw = {}
        for sig in ['r', 'k', 'v']:
            for hl in range(2):
                for b in range(2):
                    praw[(sig, hl, b)] = kb.sb("praw_%s%d%d" % (sig, hl, b), [64, 513], F32)
        for sig, npart in [('w', 32), ('a', 32), ('g', 96)]:
            for b in range(2):
                praw[(sig, 0, b)] = kb.sb("praw_%s%d" % (sig, b), [npart, 513], F32)
        wlo_s = kb.sb("wlo_s", [32, 512], F32)
        alo_s = kb.sb("alo_s", [32, 512], F32)
        glo_s = kb.sb("glo_s", [96, 512], F32)
        tmp96 = kb.sb("tmp96", [96, 512], F32)
        names = ['R', 'K', 'V', 'A', 'G', 'LW', 'CW', 'E1', 'T1', 'T2', 'T3', 'BON', 'P0', 'P0T', 'Pa', 'PaT', 'Pb',
                 'PbT', 'TT', 'AakT', 'ArbT', 'ArkT', 'Btok', 'Ktok', 'Vtok', 'ysb']
        W = {nm: kb.sb("w_" + nm, [64, 512], F32) for nm in names}
        Xs = kb.sb("Xs", [64, 64], F32)
        Us = kb.sb("Us", [64, 64], F32)
        H = [[kb.sb("H%d%d" % (hl, i), [64, 64], F32) for i in range(2)] for hl in range(2)]
        osb = kb.sb("osb", [64, 512], F32)
        pb = [kb.ps("pb%d" % i, [128, 512]) for i in range(8)]

        def dve(r, w, fn):
            kb.op('dve', r, w, fn)

        def act(r, w, fn):
            kb.op('act', r, w, fn)

        def pe(r, w, fn):
            kb.op('pe', r, w, fn)

        kb.dma('sp', [], ['pc'], pc[:, :, :], pch[:, :, :])
        kb.dma('sp', [], ['pl'], pl[:, :], plo[:, :])
        kb.dma('sp', [], ['w2s'], w2s[:, :], w2h[:, :])
        kb.dma('sp', [], ['a2s'], a2s[:, :], a2h[:, :])
        kb.dma('sp', [], ['g2s'], g2s[:, :], g2h[:, :])
        kb.dma('sp', [], ['cst'], cs_[:, :, :], cst[:, :, :])
        kb.dma('pool', [], ['wrb'], wrb[:, :, :], wr.rearrange("(c p) n -> p c n", p=128))
        kb.dma('pool', [], ['wkb'], wkb[:, :, :], wk.rearrange("(c p) n -> p c n", p=128))
        kb.dma('pool', [], ['wvb'], wvb[:, :, :], wv.rearrange("(c p) n -> p c n", p=128))
        kb.dma('pool', [], ['wlob'], wlob[:, :, :], wlo.rearrange("(c p) n -> p c n", p=128))
        dve([], ['epst'], lambda e: e.memset(epst[:, :], RW_EPS))
        dve(['pc'], ['omk'], lambda e: e.tensor_scalar(out=omk[:, :], in0=pc[:, :, 6], scalar1=-1.0, scalar2=1.0,
                                                       op0=ALU.mult, op1=ALU.add))
        for hl in range(2):
            dve([], ['H%d0' % hl], lambda e: e.memset(H[hl][0][:, :], 0.0))
        hcur = [0, 0]
        MK0, MK1, MK2, IDR, ONE, ONEM = (cs_[:, i, :] for i in range(6))
        ident64 = cs_[:, 3, 0:64]
        ones64 = cs_[:, 4, 0:64]
        onesm64 = cs_[:, 5, 0:64]
        nbk = [0]

        def bank():
            nbk[0] += 1
            return nbk[0] % 2

        for tg in range(n_groups):
            b = tg % 2
            xb = xTb[b]
            xk = 'xTb%d' % b
            kb.dma('pool', [], [xk], xb[:, :, :], xT[:, tg * 512:(tg + 1) * 512].rearrange("(c p) t -> p c t", p=128))

            def proj(wb, wkey, c0, m, dst, dkey, prev):
                bk = bank()
                for c in range(8):
                    pe([wkey, xk], ['pb%d' % bk],
                       lambda e, c=c: e.matmul(pb[bk][0:m, :], wb[:, c, c0:c0 + m], xb[:, c, :], start=(c == 0), stop=(c == 7)))
                act(['pb%d' % bk], [dkey], lambda e: e.activation(out=dst[:, 1:513], in_=pb[bk][0:m, :], func=AF.Copy))
                if tg == 0:
                    dve([dkey], [dkey], lambda e: e.memset(dst[:, 0:1], 0.0))
                else:
                    dve([dkey, prev[1]], [dkey], lambda e: e.tensor_copy(out=dst[:, 0:1], in_=prev[0][:, 512:513]))

            def shift(src, skey, mu, out, okey, tmp, tkey):
                dve([skey], [tkey], lambda e: e.tensor_tensor(out=tmp, in0=src[:, 0:512], in1=src[:, 1:513], op=ALU.subtract))
                dve([skey, tkey, 'pc', 'pl'], [okey],
                    lambda e: e.scalar_tensor_tensor(out=out, in0=tmp, scalar=mu, in1=src[:, 1:513], op0=ALU.mult, op1=ALU.add))

            for hl in range(2):
                for si, (sig, wb, wkey) in enumerate([('r', wrb, 'wrb'), ('k', wkb, 'wkb'), ('v', wvb, 'wvb')]):
                    proj(wb, wkey, hl * 64, 64, praw[(sig, hl, b)], 'praw_%s%d%d' % (sig, hl, b),
                         (praw[(sig, hl, 1 - b)], 'praw_%s%d%d' % (sig, hl, 1 - b)))
            for sig, c0, m in [('w', 0, 32), ('a', 32, 32), ('g', 64, 96)]:
                proj(wlob, 'wlob', c0, m, praw[(sig, 0, b)], 'praw_%s%d' % (sig, b),
                     (praw[(sig, 0, 1 - b)], 'praw_%s%d' % (sig, 1 - b)))
            shift(praw[('w', 0, b)], 'praw_w%d' % b, pl[0:32, 0:1], wlo_s[:, :], 'wlo_s', tmp96[0:32, :], 'tmp96')
            shift(praw[('a', 0, b)], 'praw_a%d' % b, pl[0:32, 1:2], alo_s[:, :], 'alo_s', tmp96[0:32, :], 'tmp96')
            shift(praw[('g', 0, b)], 'praw_g%d' % b, pl[0:96, 2:3], glo_s[:, :], 'glo_s', tmp96[0:96, :], 'tmp96')
            act(['wlo_s'], ['wlo_s'], lambda e: e.activation(out=wlo_s[:, :], in_=wlo_s[:, :], func=AF.Tanh))
            act(['glo_s'], ['glo_s'], lambda e: e.activation(out=glo_s[:, :], in_=glo_s[:, :], func=AF.Sigmoid))

            for hl in range(2):
                P = lambda i: pc[:, hl, i:i + 1]
                R, K, V, A, G, LW, CW, E1, T1, T2, T3, BON = (W[n][:, :] for n in
                                                                ['R', 'K', 'V', 'A', 'G', 'LW', 'CW', 'E1', 'T1', 'T2', 'T3', 'BON'])
                for si, (sig, dst, dk) in enumerate([('r', R, 'R'), ('k', K, 'K'), ('v', V, 'V')]):
                    shift(praw[(sig, hl, b)], 'praw_%s%d%d' % (sig, hl, b), P(si), dst, dk, T3, 'T3')
                hc = slice(hl * 64, (hl + 1) * 64)
                bk = bank()
                pe(['w2s', 'wlo_s'], ['pb%d' % bk], lambda e: e.matmul(pb[bk][0:64, :], w2s[:, hc], wlo_s[:, :], start=True, stop=True))
                act(['pb%d' % bk, 'pc'], ['LW'], lambda e: e.activation(out=LW, in_=pb[bk][0:64, :], func=AF.Sigmoid, bias=P(3), scale=1.0))
                dve(['LW'], ['LW'], lambda e: e.tensor_scalar(out=LW, in0=LW, scalar1=-0.6065306597126334, scalar2=None, op0=ALU.mult))
                bk = bank()
                pe(['a2s', 'alo_s'], ['pb%d' % bk], lambda e: e.matmul(pb[bk][0:64, :], a2s[:, hc], alo_s[:, :], start=True, stop=True))
                act(['pb%d' % bk, 'pc'], ['A'], lambda e: e.activation(out=A, in_=pb[bk][0:64, :], func=AF.Sigmoid, bias=P(4), scale=1.0))
                bk = bank()
                pe(['g2s', 'glo_s'], ['pb%d' % bk], lambda e: e.matmul(pb[bk][0:64, :], g2s[:, hc], glo_s[:, :], start=True, stop=True))
                act(['pb%d' % bk], ['G'], lambda e: e.activation(out=G, in_=pb[bk][0:64, :], func=AF.Copy))
                dve(['K', 'pc'], ['T1'], lambda e: e.tensor_scalar(out=T1, in0=K, scalar1=P(5), scalar2=None, op0=ALU.mult))
                dve(['T1'], ['T2'], lambda e: e.tensor_tensor(out=T2, in0=T1, in1=T1, op=ALU.mult))
                bk = bank()
                pe(['cst', 'T2'], ['pb%d' % bk], lambda e: e.matmul(pb[bk][0:64, :], ones64, T2, start=True, stop=True))
                act(['pb%d' % bk], ['T2'], lambda e: e.activation(out=T2, in_=pb[bk][0:64, :], func=AF.Sqrt))
                dve(['T2'], ['T2'], lambda e: e.tensor_scalar(out=T2, in0=T2, scalar1=1e-12, scalar2=None, op0=ALU.max))
                dve(['T2'], ['T2'], lambda e: e.reciprocal(out=T2, in_=T2))
                dve(['T1', 'T2'], ['T1'], lambda e: e.tensor_tensor(out=T1, in0=T1, in1=T2, op=ALU.mult))
                dve(['A', 'pc', 'omk'], ['T2'], lambda e: e.tensor_scalar(out=T2, in0=A, scalar1=P(6), scalar2=omk[:, hl:hl + 1],
                                                                         op0=ALU.mult, op1=ALU.add))
                dve(['K', 'T2'], ['K'], lambda e: e.tensor_tensor(out=K, in0=K, in1=T2, op=ALU.mult))
                dve(['T1', 'A'], ['A'], lambda e: e.tensor_tensor(out=A, in0=T1, in1=A, op=ALU.mult))
                dve(['T1'], ['T1'], lambda e: e.tensor_scalar(out=T1, in0=T1, scalar1=-1.0, scalar2=None, op0=ALU.mult))
                dve(['R', 'K', 'pc'], ['T2'], lambda e: e.scalar_tensor_tensor(out=T2, in0=R, scalar=P(8), in1=K, op0=ALU.mult, op1=ALU.mult))
                bk = bank()
                pe(['cst', 'T2'], ['pb%d' % bk], lambda e: e.matmul(pb[bk][0:64, :], ones64, T2, start=True, stop=True))
                dve(['V', 'pb%d' % bk], ['BON'], lambda e: e.tensor_tensor(out=BON, in0=V, in1=pb[bk][0:64, :], op=ALU.mult))
                for c in range(8):
                    cs = slice(c * 64, (c + 1) * 64)
                    dve(['LW', 'cst'], ['CW'], lambda e, cs=cs: e.tensor_tensor_scan(out=CW[:, cs], data0=ONE[:, 0:64], data1=LW[:, cs],
                                                                                    initial=0.0, op0=ALU.mult, op1=ALU.add))
                act(['CW'], ['E1'], lambda e: e.activation(out=E1, in_=CW, func=AF.Exp))
                act(['CW'], ['T3'], lambda e: e.activation(out=T3, in_=CW, func=AF.Exp, scale=-1.0))
                dve(['R', 'E1'], ['R'], lambda e: e.tensor_tensor(out=R, in0=R, in1=E1, op=ALU.mult))
                dve(['K', 'T3'], ['K'], lambda e: e.tensor_tensor(out=K, in0=K, in1=T3, op=ALU.mult))
                dve(['A', 'T3'], ['A'], lambda e: e.tensor_tensor(out=A, in0=A, in1=T3, op=ALU.mult))
                dve(['CW', 'LW'], ['T3'], lambda e: e.tensor_tensor(out=T3, in0=CW, in1=LW, op=ALU.subtract))
                act(['T3'], ['T3'], lambda e: e.activation(out=T3, in_=T3, func=AF.Exp))
                dve(['T1', 'T3'], ['T1'], lambda e: e.tensor_tensor(out=T1, in0=T1, in1=T3, op=ALU.mult))
                CH = [slice(c * 64, (c + 1) * 64) for c in range(8)]

                def batched(bk, lk, L_, rk, R_, transpose=False):
                    for cs in CH:
                        if transpose:
                            pe([lk, 'cst'], ['pb%d' % bk], lambda e, cs=cs: e.transpose(pb[bk][0:64, cs], L_[:, cs], ident64))
                        else:
                            pe([lk, rk], ['pb%d' % bk],
                               lambda e, cs=cs: e.matmul(pb[bk][0:64, cs], L_[:, cs], R_[:, cs], start=True, stop=True))

                def evac_mask(bk, dst, dkey, m):
                    dve(['pb%d' % bk, 'cst'], [dkey], lambda e: e.tensor_tensor(out=dst, in0=pb[bk][0:64, :], in1=m, op=ALU.mult))

                P0, P0T, TT, AakT, ArbT, ArkT, Btok, Ktok, Vtok, ysb = (W[n][:, :] for n in
                                                                         ['P0', 'P0T', 'TT', 'AakT', 'ArbT', 'ArkT', 'Btok', 'Ktok', 'Vtok', 'ysb'])
                batched(2, 'A', A, 'T1', T1)
                evac_mask(2, P0T, 'P0T', MK0)
                batched(3, 'T1', T1, 'A', A)
                evac_mask(3, P0, 'P0', MK1)
                batched(2, 'K', K, 'T1', T1)
                evac_mask(2, AakT, 'AakT', MK0)
                batched(3, 'A', A, 'R', R)
                evac_mask(3, ArbT, 'ArbT', MK2)
                batched(2, 'K', K, 'R', R)
                evac_mask(2, ArkT, 'ArkT', MK2)
                batched(3, 'A', A, None, None, transpose=True)
                act(['pb3'], ['Btok'], lambda e: e.activation(out=Btok, in_=pb[3][0:64, :], func=AF.Copy))
                batched(2, 'K', K, None, None, transpose=True)
                act(['pb2'], ['Ktok'], lambda e: e.activation(out=Ktok, in_=pb[2][0:64, :], func=AF.Copy))
                batched(3, 'V', V, None, None, transpose=True)
                act(['pb3'], ['Vtok'], lambda e: e.activation(out=Vtok, in_=pb[3][0:64, :], func=AF.Copy))
                dve(['P0T', 'cst'], ['TT'], lambda e: e.tensor_tensor(out=TT, in0=P0T, in1=IDR, op=ALU.add))
                cur, curk, curT, curTk = P0, 'P0', P0T, 'P0T'
                for lvl in range(1, 6):
                    nn, nnT = ('Pa', 'PaT') if lvl % 2 == 1 else ('Pb', 'PbT')
                    Pn, PnT = W[nn][:, :], W[nnT][:, :]
                    batched(2, curTk, curT, curk, cur)
                    act(['pb2'], [nn], lambda e: e.activation(out=Pn, in_=pb[2][0:64, :], func=AF.Copy))
                    if lvl < 5:
                        batched(3, curk, cur, curTk, curT)
                        dve(['pb3'], [nnT], lambda e: e.tensor_copy(out=PnT, in_=pb[3][0:64, :]))
                    batched(2, nn, Pn, 'TT', TT)
                    dve(['pb2', 'TT'], ['TT'], lambda e: e.tensor_tensor(out=TT, in0=TT, in1=pb[2][0:64, :], op=ALU.add))
                    cur, curk, curT, curTk = Pn, nn, PnT, nnT
                sbk = 4 + hl
                ybk = 6 + hl
                for c in range(8):
                    cs = CH[c]
                    Hc = H[hl][hcur[hl]]
                    Hn = H[hl][1 - hcur[hl]]
                    hk, hnk = 'H%d%d' % (hl, hcur[hl]), 'H%d%d' % (hl, 1 - hcur[hl])
                    sk, yk = 'pb%d' % sbk, 'pb%d' % ybk
                    pe(['T1', hk], [sk], lambda e: e.matmul(pb[sbk][0:64, 0:64], T1[:, cs], Hc[:, :], start=True, stop=False))
                    pe(['AakT', 'Vtok'], [sk], lambda e: e.matmul(pb[sbk][0:64, 0:64], AakT[:, cs], Vtok[:, cs], start=False, stop=True))
                    act([sk], ['Xs'], lambda e: e.activation(out=Xs[:, :], in_=pb[sbk][0:64, 0:64], func=AF.Copy))
                    pe(['TT', 'Xs'], [sk], lambda e: e.matmul(pb[sbk][0:64, 64:128], TT[:, cs], Xs[:, :], start=True, stop=True))
                    dve([sk], ['Us'], lambda e: e.tensor_copy(out=Us[:, :], in_=pb[sbk][0:64, 64:128]))
                    pe([hk, 'R'], [yk], lambda e: e.matmul(pb[ybk][0:64, cs], Hc[:, :], R[:, cs], start=True, stop=False))
                    pe(['Us', 'ArbT'], [yk], lambda e: e.matmul(pb[ybk][0:64, cs], Us[:, :], ArbT[:, cs], start=False, stop=False))
                    pe(['Vtok', 'ArkT'], [yk], lambda e: e.matmul(pb[ybk][0:64, cs], Vtok[:, cs], ArkT[:, cs], start=False, stop=True))
                    pe(['cst', hk], [sk], lambda e: e.matmul(pb[sbk][0:64, 128:192], ident64, Hc[:, :], start=True, stop=False))
                    pe(['Btok', 'Us'], [sk], lambda e: e.matmul(pb[sbk][0:64, 128:192], Btok[:, cs], Us[:, :], start=False, stop=False))
                    pe(['Ktok', 'Vtok'], [sk], lambda e: e.matmul(pb[sbk][0:64, 128:192], Ktok[:, cs], Vtok[:, cs], start=False, stop=True))
                    dve([sk, 'E1'], [hnk], lambda e: e.tensor_scalar(out=Hn[:, :], in0=pb[sbk][0:64, 128:192],
                                                                    scalar1=E1[:, c * 64 + 63:c * 64 + 64], scalar2=None, op0=ALU.mult))
                    hcur[hl] = 1 - hcur[hl]
                yk = 'pb%d' % ybk
                act([yk], ['ysb'], lambda e: e.activation(out=ysb, in_=pb[ybk][0:64, :], func=AF.Copy))
                bk = bank()
                pe(['cst', 'ysb'], ['pb%d' % bk], lambda e: e.matmul(pb[bk][0:64, :], onesm64, ysb, start=True, stop=True))
                dve(['ysb', 'pb%d' % bk], ['ysb'], lambda e: e.tensor_tensor(out=ysb, in0=ysb, in1=pb[bk][0:64, :], op=ALU.subtract))
                dve(['ysb'], ['T2'], lambda e: e.tensor_tensor(out=T2, in0=ysb, in1=ysb, op=ALU.mult))
                bk = bank()
                pe(['cst', 'T2'], ['pb%d' % bk], lambda e: e.matmul(pb[bk][0:64, :], onesm64, T2, start=True, stop=True))
                act(['pb%d' % bk, 'epst'], ['T2'], lambda e: e.activation(out=T2, in_=pb[bk][0:64, :], func=AF.Sqrt, bias=epst[:, 0:1], scale=1.0))
                dve(['T2'], ['T2'], lambda e: e.reciprocal(out=T2, in_=T2))
                dve(['ysb', 'T2'], ['ysb'], lambda e: e.tensor_tensor(out=ysb, in0=ysb, in1=T2, op=ALU.mult))
                dve(['ysb', 'pc'], ['ysb'], lambda e: e.tensor_scalar(out=ysb, in0=ysb, scalar1=P(9), scalar2=P(10), op0=ALU.mult, op1=ALU.add))
                dve(['ysb', 'BON'], ['ysb'], lambda e: e.tensor_tensor(out=ysb, in0=ysb, in1=BON, op=ALU.add))
                dve(['ysb', 'G'], ['osb'], lambda e: e.tensor_tensor(out=osb[:, :], in0=ysb, in1=G, op=ALU.mult))
                kb.dma('sp', ['osb'], ['yaT'], yaT[hl * 64:(hl + 1) * 64, tg * 512:(tg + 1) * 512], osb[:, :])
        kb.finish('sp')
    return nc


def rwkv_consts():
    j = np.arange(64)[:, None]
    t = np.arange(64)[None, :]
    c = np.zeros((64, 6, 512), np.float32)
    for i in range(8):
        sl = slice(i * 64, (i + 1) * 64)
        c[:, 0, sl] = (j < t)
        c[:, 1, sl] = (t < j)
        c[:, 2, sl] = (j <= t)
        c[:, 3, sl] = (j == t)
    c[:, 4, :] = 1.0
    c[:, 5, :] = 1.0 / 64.0
    return c


def rwkv_inputs(xT, ab, hg):
    w_in = ab['ab_w_in'][0]
    mu = ab['ab_shift_mu'][0]
    cols = np.arange(hg * 128, (hg + 1) * 128)
    pch = np.zeros((64, 2, 12), np.float32)
    for hl in range(2):
        h = 2 * hg + hl
        c = np.arange(h * 64, (h + 1) * 64)
        pch[:, hl, 0] = mu[c]
        pch[:, hl, 1] = mu[512 + c]
        pch[:, hl, 2] = mu[1024 + c]
        pch[:, hl, 3] = ab['ab_w0'][0][c]
        pch[:, hl, 4] = ab['ab_a0'][0][c]
        pch[:, hl, 5] = ab['ab_k_k'][0][c]
        pch[:, hl, 6] = ab['ab_k_a'][0][c]
        pch[:, hl, 8] = ab['ab_r_k'][0][h]
        pch[:, hl, 9] = ab['ab_lnx_g'][0][c]
        pch[:, hl, 10] = ab['ab_lnx_b'][0][c]
    plo = np.zeros((96, 4), np.float32)
    plo[:32, 0] = mu[1536:1568]
    plo[:32, 1] = mu[1568:1600]
    plo[:96, 2] = mu[1600:1696]
    return {
        "xT": xT,
        "wr": np.ascontiguousarray(w_in[:, cols]),
        "wk": np.ascontiguousarray(w_in[:, 512 + cols]),
        "wv": np.ascontiguousarray(w_in[:, 1024 + cols]),
        "wlo": np.ascontiguousarray(w_in[:, 1536:1696]),
        "pch": pch, "plo": plo,
        "w2h": np.ascontiguousarray(ab['ab_w2'][0][:, cols]),
        "a2h": np.ascontiguousarray(ab['ab_a2'][0][:, cols]),
        "g2h": np.ascontiguousarray(ab['ab_g2'][0][:, cols]),
        "cst": rwkv_consts(),
    }


def build_moba(n_blocks=32):
    nc = bass.Bass("TRN2", target_bir_lowering=False)
    S = SEQ
    xT = _din(nc, "xT", [D, S])
    wq = _din(nc, "wq", [D, 128])
    wk = _din(nc, "wk", [D, 128])
    wv = _din(nc, "wv", [D, 128])
    cosd = _din(nc, "cosf", [64, S])
    sind = _din(nc, "sinf", [64, S])
    protd = _din(nc, "prot", [64, 64])
    ebd = _din(nc, "eb", [33, 32, 128])
    cmd = _din(nc, "cmask", [128, 2, 256])
    identd = _din(nc, "ident", [128, 128])
    ybT = _dout(nc, "ybT", [128, S])
    with ExitStack() as es:
        kb = KB(nc, es)
        qTm = [kb.sb("qTm%d" % i, [64, S], BF16) for i in range(2)]
        kTm = [kb.sb("kTm%d" % i, [64, S], BF16) for i in range(2)]
        vm = kb.sb("vm", [128, S // 128, 128], BF16)
        biasT = [kb.sb("biasT%d" % i, [33, S], BF16) for i in range(2)]
        xTb = [kb.sb("xTb%d" % i, [128, 8, 256], BF16) for i in range(2)]
        wqb = kb.sb("wqb", [128, 8, 128], BF16)
        wkb = kb.sb("wkb", [128, 8, 128], BF16)
        wvb = kb.sb("wvb", [128, 8, 128], BF16)
        Eb = kb.sb("Eb", [33, 32, 128], BF16)
        cmask = kb.sb("cmask", [128, 2, 256], F32)
        ident = kb.sb("ident", [128, 128], F32)
        prot = kb.sb("prot", [64, 64], F32)
        ones32 = kb.sb("ones32", [64, 128], F32)
        onesb = kb.sb("onesb", [128, 64], BF16)
        ct = kb.sb("ct", [64, 256], F32)
        st = kb.sb("st", [64, 256], F32)
        qs = kb.sb("qs", [64, 256], F32)
        t1 = kb.sb("t1", [64, 256], F32)
        t2 = kb.sb("t2", [64, 256], F32)
        qf = kb.sb("qf", [64, 256], F32)
        kf = kb.sb("kf", [64, 256], F32)
        sq = kb.sb("sq", [64, 256], F32)
        kmean = [kb.sb("kmean%d" % i, [64, 32], F32) for i in range(2)]
        kmax2 = [kb.sb("kmax2_%d" % i, [128, 1], F32) for i in range(2)]
        kmx = kb.sb("kmx", [128, 1], F32)
        gsb = kb.sb("gsb", [128, 32], F32)
        m8 = kb.sb("m8", [128, 8], F32)
        selt = kb.sb("selt", [128, 33], F32)
        mt = kb.sb("mt", [128, 1], F32)
        att = [kb.sb("att%d" % i, [128, 256], BF16) for i in range(3)]
        rec = kb.sb("rec", [64, 256], F32)
        yo = [kb.sb("yo%d" % i, [64, 256], F32) for i in range(2)]
        pb = [kb.ps("pb%d" % i, [128, 512]) for i in range(8)]

        def dve(r, w, fn):
            kb.op('dve', r, w, fn)

        def act(r, w, fn):
            kb.op('act', r, w, fn)

        def pe(r, w, fn):
            kb.op('pe', r, w, fn)

        kb.dma('sp', [], ['cmask'], cmask[:, :, :], cmd[:, :, :])
        kb.dma('sp', [], ['ident'], ident[:, :], identd[:, :])
        kb.dma('sp', [], ['prot'], prot[:, :], protd[:, :])
        kb.dma('pool', [], ['Eb'], Eb[:, :, :], ebd[:, :, :])
        kb.dma('pool', [], ['wqb'], wqb[:, :, :], wq.rearrange("(c p) n -> p c n", p=128))
        kb.dma('pool', [], ['wkb'], wkb[:, :, :], wk.rearrange("(c p) n -> p c n", p=128))
        kb.dma('pool', [], ['wvb'], wvb[:, :, :], wv.rearrange("(c p) n -> p c n", p=128))
        dve([], ['ones32'], lambda e: e.memset(ones32[:, :], 1.0))
        dve([], ['onesb'], lambda e: e.memset(onesb[:, :], 1.0))
        for hl in range(2):
            dve([], ['kmean%d' % hl], lambda e: e.memset(kmean[hl][:, :], 0.0))
            dve([], ['kmax2_%d' % hl], lambda e: e.memset(kmax2[hl][:, :], 0.0))
        nbk = [0]

        def bank():
            nbk[0] += 1
            return nbk[0] % 8

        for n in range(n_blocks):
            b = n % 2
            xb, xk = xTb[b], 'xTb%d' % b
            cols = slice(n * 256, (n + 1) * 256)
            kb.dma('pool', [], [xk], xb[:, :, :], xT[:, cols].rearrange("(c p) t -> p c t", p=128))
            kb.dma('sp', [], ['ct'], ct[:, :], cosd[:, cols])
            kb.dma('sp', [], ['st'], st[:, :], sind[:, cols])

            def rope_proj(wb, wkey, hl, outf, okey):
                bk = bank()
                for c in range(8):
                    pe([wkey, xk], ['pb%d' % bk],
                       lambda e, c=c: e.matmul(pb[bk][0:64, 0:256], wb[:, c, hl * 64:(hl + 1) * 64], xb[:, c, :],
                                               start=(c == 0), stop=(c == 7)))
                act(['pb%d' % bk], ['qs'], lambda e: e.activation(out=qs[:, :], in_=pb[bk][0:64, 0:256], func=AF.Copy))
                bk2 = bank()
                pe(['prot', 'qs'], ['pb%d' % bk2], lambda e: e.matmul(pb[bk2][0:64, 0:256], prot[:, :], qs[:, :], start=True, stop=True))
                dve(['qs', 'ct'], ['t1'], lambda e: e.tensor_tensor(out=t1[:, :], in0=qs[:, :], in1=ct[:, :], op=ALU.mult))
                dve(['pb%d' % bk2, 'st'], ['t2'], lambda e: e.tensor_tensor(out=t2[:, :], in0=pb[bk2][0:64, 0:256], in1=st[:, :], op=ALU.mult))
                dve(['t1', 't2'], [okey], lambda e: e.tensor_tensor(out=outf[:, :], in0=t1[:, :], in1=t2[:, :], op=ALU.add))

            for hl in range(2):
                rope_proj(wkb, 'wkb', hl, kf, 'kf')
                act(['kf'], ['kTm%d' % hl], lambda e: e.activation(out=kTm[hl][:, cols], in_=kf[:, :], func=AF.Copy))
                dve(['kf'], ['kmean%d' % hl], lambda e: e.reduce_sum(out=kmean[hl][:, n:n + 1], in_=kf[:, :], axis=AX.X))
                dve(['kf'], ['sq'], lambda e: e.tensor_tensor(out=sq[:, :], in0=kf[:, :], in1=kf[:, :], op=ALU.mult))
                bk = bank()
                pe(['ones32', 'sq'], ['pb%d' % bk], lambda e: e.matmul(pb[bk][:, 0:256], ones32[:, :], sq[:, :], start=True, stop=True))
                dve(['pb%d' % bk], ['kmx'], lambda e: e.reduce_max(out=kmx[:, :], in_=pb[bk][:, 0:256], axis=AX.X))
                dve(['kmx', 'kmax2_%d' % hl], ['kmax2_%d' % hl],
                    lambda e: e.tensor_tensor(out=kmax2[hl][:, :], in0=kmax2[hl][:, :], in1=kmx[:, :], op=ALU.max))
                rope_proj(wqb, 'wqb', hl, qf, 'qf')
                act(['qf'], ['qTm%d' % hl], lambda e: e.activation(out=qTm[hl][:, cols], in_=qf[:, :], func=AF.Copy, scale=0.125))
                dve(['qf'], ['sq'], lambda e: e.tensor_tensor(out=sq[:, :], in0=qf[:, :], in1=qf[:, :], op=ALU.mult))
                for tt in range(2):
                    ts_ = slice(tt * 128, (tt + 1) * 128)
                    bk = bank()
                    pk = 'pb%d' % bk
                    pe(['qf', 'kmean%d' % hl], [pk], lambda e: e.matmul(pb[bk][:, 0:32], qf[:, ts_], kmean[hl][:, :], start=True, stop=True))
                    pe(['sq', 'ones32'], [pk], lambda e: e.matmul(pb[bk][:, 32:33], sq[:, ts_], ones32[:, 0:1], start=True, stop=True))
                    if n == 0:
                        dve([], ['selt'], lambda e: e.memset(selt[:, 0:32], 0.0))
                    else:
                        dve([pk], ['gsb'], lambda e: e.tensor_copy(out=gsb[:, :], in_=pb[bk][:, 0:32]))
                        dve(['gsb'], ['gsb'], lambda e: e.memset(gsb[:, n:32], -1e30))
                        dve(['gsb'], ['m8'], lambda e: e.max(out=m8[:, :], in_=gsb[:, :]))
                        dve(['gsb', 'm8'], ['selt'], lambda e: e.tensor_scalar(out=selt[:, 0:32], in0=gsb[:, :], scalar1=m8[:, 2:3],
                                                                              scalar2=None, op0=ALU.is_ge))
                        dve(['selt'], ['selt'], lambda e: e.memset(selt[:, n:32], 0.0))
                    dve(['selt'], ['selt'], lambda e: e.memset(selt[:, n:n + 1], 1.0))
                    dve(['selt'], ['selt'], lambda e: e.tensor_scalar(out=selt[:, 0:32], in0=selt[:, 0:32], scalar1=-1.0, scalar2=30000.0,
                                                                     op0=ALU.add, op1=ALU.mult))
                    dve([pk, 'kmax2_%d' % hl], ['mt'], lambda e: e.tensor_tensor(out=mt[:, :], in0=pb[bk][:, 32:33], in1=kmax2[hl][:, :], op=ALU.mult))
                    act(['mt'], ['mt'], lambda e: e.activation(out=mt[:, :], in_=mt[:, :], func=AF.Sqrt))
                    dve(['mt', 'selt'], ['selt'], lambda e: e.tensor_scalar(out=selt[:, 32:33], in0=mt[:, :], scalar1=-0.125, scalar2=None, op0=ALU.mult))
                    bk2 = bank()
                    pe(['selt', 'ident'], ['pb%d' % bk2], lambda e: e.transpose(pb[bk2][0:33, 0:128], selt[:, :], ident[:, :]))
                    act(['pb%d' % bk2], ['biasT%d' % hl],
                        lambda e: e.activation(out=biasT[hl][:, n * 256 + tt * 128:n * 256 + (tt + 1) * 128], in_=pb[bk2][0:33, 0:128], func=AF.Copy))
            for tt in range(2):
                bk = bank()
                for c in range(8):
                    pe(['wvb', xk], ['pb%d' % bk],
                       lambda e, c=c: e.matmul(pb[bk][:, 0:128], xb[:, c, tt * 128:(tt + 1) * 128], wvb[:, c, :], start=(c == 0), stop=(c == 7)))
                act(['pb%d' % bk], ['vm'], lambda e: e.activation(out=vm[:, 2 * n + tt, :], in_=pb[bk][:, 0:128], func=AF.Copy))

        tiles = []
        g = 0
        for hl in range(2):
            for n in range(n_blocks):
                for kt in range(2 * n + 2):
                    tiles.append(dict(hl=hl, n=n, kt=kt, first=(kt == 0), last=(kt == 2 * n + 1),
                                      j=(kt - 2 * n if kt >= 2 * n else None), g=g, i=len(tiles)))
                g += 1
        nt = len(tiles)

        def M1(t):
            i = t['i']
            sb_ = i % 2
            qc = slice(t['n'] * 256, (t['n'] + 1) * 256)
            pe(['kTm%d' % t['hl'], 'qTm%d' % t['hl']], ['pb%d' % sb_],
               lambda e: e.matmul(pb[sb_][:, 0:256], kTm[t['hl']][:, t['kt'] * 128:(t['kt'] + 1) * 128], qTm[t['hl']][:, qc],
                                  start=True, stop=False))
            pe(['Eb', 'biasT%d' % t['hl']], ['pb%d' % sb_],
               lambda e: e.matmul(pb[sb_][:, 0:256], Eb[:, t['kt'] // 2, :], biasT[t['hl']][:, qc], start=False, stop=True))

        def M2(t):
            i = t['i']
            ak = 'att%d' % (i % 3)
            act(['pb%d' % (i % 2)], [ak], lambda e: e.activation(out=att[i % 3][:, :], in_=pb[i % 2][:, 0:256], func=AF.Exp))
            if t['j'] is not None:
                dve([ak, 'cmask'], [ak], lambda e: e.tensor_tensor(out=att[i % 3][:, :], in0=att[i % 3][:, :], in1=cmask[:, t['j'], :], op=ALU.mult))

        def M3(t):
            i = t['i']
            ak = 'att%d' % (i % 3)
            ob, db = 2 + t['g'] % 2, 4 + t['g'] % 2
            pe(['vm', ak], ['pb%d' % ob],
               lambda e: e.matmul(pb[ob][0:64, 0:256], vm[:, t['kt'], t['hl'] * 64:(t['hl'] + 1) * 64], att[i % 3][:, :],
                                  start=t['first'], stop=t['last']))
            pe(['onesb', ak], ['pb%d' % db],
               lambda e: e.matmul(pb[db][0:64, 0:256], onesb[:, :], att[i % 3][:, :], start=t['first'], stop=t['last']))
            if t['last']:
                yk = 'yo%d' % (t['g'] % 2)
                dve(['pb%d' % db], ['rec'], lambda e: e.reciprocal(out=rec[:, :], in_=pb[db][0:64, 0:256]))
                dve(['pb%d' % ob, 'rec'], [yk], lambda e: e.tensor_tensor(out=yo[t['g'] % 2][:, :], in0=pb[ob][0:64, 0:256], in1=rec[:, :], op=ALU.mult))
                kb.dma('sp', [yk], ['ybT'], ybT[t['hl'] * 64:(t['hl'] + 1) * 64, t['n'] * 256:(t['n'] + 1) * 256], yo[t['g'] % 2][:, :])

        for s in range(nt + 2):
            if s < nt:
                M1(tiles[s])
            if 0 <= s - 1 < nt:
                M2(tiles[s - 1])
            if 0 <= s - 2 < nt:
                M3(tiles[s - 2])
        kb.finish('sp')
    return nc


def moba_consts():
    half = 8
    inv_freq = (np.float32(500000.0) ** (-np.arange(half, dtype=np.float32) / np.float32(half))).astype(np.float32)
    ang = (np.arange(SEQ, dtype=np.float32)[:, None] * inv_freq[None, :]).astype(np.float32)
    cos = np.cos(ang).astype(np.float32).T
    sin = np.sin(ang).astype(np.float32).T
    cosf = np.ones((64, SEQ), np.float32)
    sinf = np.zeros((64, SEQ), np.float32)
    cosf[0:8] = cos
    cosf[8:16] = cos
    sinf[0:8] = sin
    sinf[8:16] = sin
    prot = np.zeros((64, 64), np.float32)
    for m in range(8):
        prot[m + 8, m] = -1.0
        prot[m, m + 8] = 1.0
    eb = np.zeros((33, 32, 128), np.float32)
    for n in range(32):
        eb[n, n, :] = 1.0
    eb[32, :, :] = 1.0
    s = np.arange(128)[:, None, None]
    j = np.arange(2)[None, :, None]
    t = np.arange(256)[None, None, :]
    cmask = (128 * j + s <= t).astype(np.float32)
    return {"cosf": cosf, "sinf": sinf, "prot": prot, "eb": eb, "cmask": np.ascontiguousarray(cmask),
            "ident": np.eye(128, dtype=np.float32)}


def moba_inputs(xT, w_in, hg, consts):
    cols = 1696 + np.arange(hg * 128, (hg + 1) * 128)
    m = {"xT": xT, "wq": np.ascontiguousarray(w_in[:, cols]), "wk": np.ascontiguousarray(w_in[:, 512 + cols]),
         "wv": np.ascontiguousarray(w_in[:, 1024 + cols])}
    m.update(consts)
    return m


_PROGS = {}


def _prog(name, fn):
    if name not in _PROGS:
        _PROGS[name] = fn()
    return _PROGS[name]


def _run(nc, in_maps):
    res = run_bass_kernel_spmd(nc, in_maps, core_ids=list(range(NCORES)))
    return res.results


def _post_launch(yT_b, xres, w_out, i, inputs):
    ident = np.eye(128, dtype=np.float32)
    b1T = np.ascontiguousarray(inputs['exp_b1'][i].reshape(32, 16, 128).transpose(2, 0, 1))
    shared = {
        "w_out": np.ascontiguousarray(w_out), "ln1g": inputs['ln1_g'][i], "ln1b": inputs['ln1_b'][i],
        "rw": inputs['router_w'][i], "rb": inputs['router_b'][i], "w1": inputs['exp_w1'][i], "b1T": b1T,
        "w2": inputs['exp_w2'][i], "b2": inputs['exp_b2'][i], "ln2g": inputs['ln2_g'][i], "ln2b": inputs['ln2_b'][i],
        "ident": ident, "pcst": post2_consts(),
    }
    maps = []
    for c in range(NCORES):
        b, s0 = c // 4, (c % 4) * 2048
        m = dict(shared)
        m["yT"] = np.ascontiguousarray(yT_b[b][:, s0:s0 + 2048])
        m["xr"] = np.ascontiguousarray(xres[b, s0:s0 + 2048, :])
        maps.append(m)
    res = _run(_prog("post2", build_post2), maps)
    out = np.empty((NB, SEQ, D), np.float32)
    for c in range(NCORES):
        b, s0 = c // 4, (c % 4) * 2048
        out[b, s0:s0 + 2048, :] = res[c]["out"]
    return out


def kernel(**inputs):
    inputs = {k: np.asarray(v) for k, v in inputs.items()}
    x = inputs['x'].astype(np.float32, copy=False)
    xT = [np.ascontiguousarray(x[b].T) for b in range(NB)]
    ab = {k: v for k, v in inputs.items() if k.startswith('ab_')}
    maps = [rwkv_inputs(xT[c // 4], ab, c % 4) for c in range(NCORES)]
    res = _run(_prog("rwkv", build_rwkv), maps)
    yT0 = [np.empty((D, SEQ), np.float32) for _ in range(NB)]
    for c in range(NCORES):
        b, hg = c // 4, c % 4
        yT0[b][hg * 128:(hg + 1) * 128, :] = res[c]["yaT"]
    mc = moba_consts()
    maps = [moba_inputs(xT[c // 4], ab['ab_w_in'][0], c % 4, mc) for c in range(NCORES)]
    res = _run(_prog("moba", build_moba), maps)
    for c in range(NCORES):
        b, hg = c // 4, c % 4
        yT0[b][512 + hg * 128:512 + (hg + 1) * 128, :] = res[c]["ybT"]
    x2 = _post_launch(yT0, x, ab['ab_w_out'][0], 0, inputs)
    xT2 = [np.ascontiguousarray(x2[b].T) for b in range(NB)]
    w_in = inputs['sb_w_in'][0]
    mask, negtri = sb_mask(), sb_negtri()
    maps = []
    for c in range(NCORES):
        b, hq = c // 4, c % 4
        cols = hq * 256 + np.arange(256)
        maps.append({"xT": xT2[b], "wq": np.ascontiguousarray(w_in[:, cols]), "wk": np.ascontiguousarray(w_in[:, 1024 + cols]),
                     "wv": np.ascontiguousarray(w_in[:, 2048 + cols]), "mask": mask, "negtri": negtri})
    res = _run(_prog("sb", build_sb), maps)
    yT1 = [np.empty((D, SEQ), np.float32) for _ in range(NB)]
    for c in range(NCORES):
        b, hq = c // 4, c % 4
        yT1[b][hq * 256:(hq + 1) * 256, :] = res[c]["yT"]
    out = _post_launch(yT1, x2, inputs['sb_w_out'][0], 1, inputs)
    return out


CAP = 512
I32 = mybir.dt.int32


def build_post2(n_exp=N_EXP):
    nc = bass.Bass("TRN2", target_bir_lowering=False)
    NTOK = 2048
    NT = NTOK // 128
    NSL = N_EXP * CAP
    yT = _din(nc, "yT", [D, NTOK])
    xr = _din(nc, "xr", [NTOK, D])
    w_out = _din(nc, "w_out", [D, D])
    ln1g = _din(nc, "ln1g", [D])
    ln1b = _din(nc, "ln1b", [D])
    rw = _din(nc, "rw", [D, N_EXP])
    rb = _din(nc, "rb", [N_EXP])
    w1 = _din(nc, "w1", [N_EXP, D, 2 * D])
    b1T = _din(nc, "b1T", [128, N_EXP, 16])
    w2 = _din(nc, "w2", [N_EXP, D, D])
    b2 = _din(nc, "b2", [N_EXP, D])
    ln2g = _din(nc, "ln2g", [D])
    ln2b = _din(nc, "ln2b", [D])
    ident_d = _din(nc, "ident", [128, 128])
    cst_d = _din(nc, "pcst", [128, 128 + N_EXP + 1])
    out = _dout(nc, "out", [NTOK, D])
    XG = nc.dram_tensor("XG_int", [NSL + 128, D], BF16, kind="Internal").ap()
    YG = nc.dram_tensor("YG_int", [NSL + 128, D], F32, kind="Internal").ap()
    AX1 = nc.dram_tensor("AX1_int", [NTOK, D], F32, kind="Internal").ap()

    with ExitStack() as es:
        kb = KB(nc, es)
        Tb = [kb.sb("Tb%d" % i, [128, D], F32) for i in range(2)]
        x1b = [kb.sb("x1b%d" % i, [128, D], BF16) for i in range(2)]
        ax1 = kb.sb("ax1", [128, D], F32)
        x1T32 = kb.sb("x1T32", [128, 8, 128], F32)
        woutb = kb.sb("woutb", [128, 8, D], BF16)
        yTb = [kb.sb("yTb%d" % i, [128, 8, 128], BF16) for i in range(2)]
        xrt = kb.sb("xrt", [128, D], F32)
        gB = kb.sb("gB", [128, D], F32)
        bB = kb.sb("bB", [128, D], F32)
        Mb = kb.sb("Mb", [128, NT, N_EXP], BF16)
        GS = kb.sb("GS", [128, NT * 4], I32)
        GA = kb.sb("GA", [128, NT, 4], F32)
        NSLOT = 6
        slots = [kb.sb("wslot%d" % i, [128, 8, 512], BF16) for i in range(NSLOT)]
        xg = kb.sb("xg", [128, 4, D], BF16)
        xgT = kb.sb("xgT", [128, 8, CAP], BF16)
        actT = kb.sb("actT", [128, 8, CAP], BF16)
        g32 = [kb.sb("g32_%d" % i, [128, 512], F32) for i in range(2)]
        s32 = [kb.sb("s32_%d" % i, [128, 512], F32) for i in range(2)]
        l32 = [kb.sb("l32_%d" % i, [128, 512], F32) for i in range(2)]
        ysl = [kb.sb("ysl%d" % i, [128, 512], F32) for i in range(3)]
        yk = [kb.sb("yk%d" % i, [128, D], F32) for i in range(2)]
        rw32 = kb.sb("rw32", [128, 8, N_EXP], F32)
        rbB = kb.sb("rbB", [128, N_EXP], F32)
        b1s = kb.sb("b1s", [128, N_EXP, 16], F32)
        b2t = kb.sb("b2t", [1, D], F32)
        ones32 = kb.sb("ones32", [1, 128], F32)
        onesb = kb.sb("onesb", [128, 128], BF16)
        ident = kb.sb("ident", [128, 128], F32)
        identb = kb.sb("identb", [128, 128], BF16)
        pcst = kb.sb("pcst", [128, 128 + N_EXP + 1], F32)
        trib = kb.sb("trib", [128, 128], BF16)
        stats = kb.sb("stats", [128, 2, 6], F32)
        mv = kb.sb("mv", [128, 2], F32)
        rs = kb.sb("rs", [128, 1], F32)
        lg = kb.sb("lg", [128, N_EXP], F32)
        m8 = kb.sb("m8", [128, 8], F32)
        msk = kb.sb("msk", [128, N_EXP], F32)
        val = kb.sb("val", [128, N_EXP], F32)
        rnk = kb.sb("rnk", [128, N_EXP], F32)
        prod = kb.sb("prod", [128, N_EXP], F32)
        gsf = kb.sb("gsf", [128, 4], F32)
        rkf = kb.sb("rkf", [128, 4], F32)
        ov = kb.sb("ov", [128, 4], F32)
        nov = kb.sb("nov", [128, 4], F32)
        nm = kb.sb("nm", [128, 1], F32)
        ex = kb.sb("ex", [128, 4], F32)
        den = kb.sb("den", [128, 1], F32)
        eps_t = kb.sb("eps_t", [128, 1], F32)
        kb.eps_t = eps_t
        pb = [kb.ps("pb%d" % i, [128, 512]) for i in range(6)]
        pt = [kb.ps("pb%d" % (6 + i), [128, 1024], BF16) for i in range(2)]

        def dve(r, w, fn):
            kb.op('dve', r, w, fn)

        def act(r, w, fn):
            kb.op('act', r, w, fn)

        def pe(r, w, fn):
            kb.op('pe', r, w, fn)

        def ind(reads, writes, **kw):
            writes = list(writes) + ['IND']
            deps = kb._deps(reads, writes)
            i = kb.dnext
            kb.dnext = (kb.dnext + 1) % len(kb.dsem)
            if kb.dcnt[i] > 0:
                deps[i] = max(deps.get(i, 0), kb.dcnt[i])
            kb._wait('pool', deps)
            inst = nc.gpsimd.indirect_dma_start(**kw)
            kb.dcnt[i] += 16
            inst.then_inc(kb.dsem[i], 16)
            kb._commit((i, kb.dcnt[i]), reads, writes)

        bc_reg = nc.gpsimd.to_reg(NSL + 127)
        dve([], ['eps'], lambda e: e.memset(eps_t[:, :], LN_EPS))
        dve([], ['ones32'], lambda e: e.memset(ones32[:, :], 1.0))
        dve([], ['onesb'], lambda e: e.memset(onesb[:, :], 1.0))
        dve([], ['xrt'], lambda e: e.memset(xrt[:, :], 0.0))
        kb.dma('sp', ['xrt'], ['YGz'], YG[NSL:NSL + 128, :], xrt[:, :])
        kb.dma('sp', [], ['ident'], ident[:, :], ident_d[:, :])
        kb.dma('sp', [], ['pcst'], pcst[:, :], cst_d[:, :])
        kb.dma('sp', [], ['rw32'], rw32[:, :, :], rw.rearrange("(c p) e -> p c e", p=128))
        kb.dma('sp', [], ['rbB'], rbB[:, :], rb.partition_broadcast(128))
        kb.dma('sp', [], ['b1s'], b1s[:, :, :], b1T[:, :, :])
        kb.dma('pool', [], ['woutb'], woutb[:, :, :], w_out.rearrange("(c p) n -> p c n", p=128))
        dve(['ident'], ['identb'], lambda e: e.tensor_copy(out=identb[:, :], in_=ident[:, :]))
        dve(['pcst'], ['trib'], lambda e: e.tensor_copy(out=trib[:, :], in_=pcst[:, 0:128]))
        CE = pcst[:, 128:128 + N_EXP]
        DCOL = pcst[:, 128 + N_EXP:128 + N_EXP + 1]

        blocks = [(e_, b_) for e_ in range(n_exp) for b_ in range(6)]
        nload = [0]

        def load_next_block():
            n = nload[0]
            if n >= len(blocks):
                return
            e_, b_ = blocks[n]
            s = n % NSLOT
            if b_ < 4:
                c0 = [0, 1024, 512, 1536][b_]
                src = w1[e_, :, c0:c0 + 512].rearrange("(c p) n -> p c n", p=128)
            else:
                c0 = (b_ - 4) * 512
                src = w2[e_, :, c0:c0 + 512].rearrange("(c p) n -> p c n", p=128)
            kb.dma('pool', [], ['slot%d' % s], slots[s][:, :, :], src)
            nload[0] += 1

        kb.dma('sp', [], ['gB'], gB[:, :], ln1g.partition_broadcast(128))
        kb.dma('sp', [], ['bB'], bB[:, :], ln1b.partition_broadcast(128))
        for i in range(NT):
            t0 = i * 128
            yb, ybk = yTb[i % 2], 'yTb%d' % (i % 2)
            T, tk = Tb[i % 2], 'Tb%d' % (i % 2)
            xb_, xbk = x1b[i % 2], 'x1b%d' % (i % 2)
            kb.dma('pool', [], [ybk], yb[:, :, :], yT[:, t0:t0 + 128].rearrange("(c p) t -> p c t", p=128))
            kb.dma('sp', [], ['xrt'], xrt[:, :], xr[t0:t0 + 128, :])
            for h in range(2):
                for c in range(8):
                    pe([ybk, 'woutb'], ['pb%d' % h],
                       lambda e, c=c, h=h: e.matmul(pb[h][:, :], yb[:, c, :], woutb[:, c, h * 512:(h + 1) * 512],
                                                    start=(c == 0), stop=(c == 7)))
            for h in range(2):
                dve(['xrt', 'pb%d' % h], [tk],
                    lambda e, h=h: e.scalar_tensor_tensor(out=T[:, h * 512:(h + 1) * 512], in0=xrt[:, h * 512:(h + 1) * 512],
                                                          scalar=ALPHA, in1=pb[h][:, :], op0=ALU.mult, op1=ALU.add))
            layer_norm_inplace(kb, T[:, :], tk, (gB, 'gB'), (bB, 'bB'), stats, mv, rs, '')
            act([tk], [xbk], lambda e: e.activation(out=xb_[:, :], in_=T[:, :], func=AF.Copy))
            act([tk], ['ax1'], lambda e: e.mul(ax1[:, :], T[:, :], ALPHA))
            kb.dma('sp', ['ax1'], ['AX1_%d' % i], AX1[t0:t0 + 128, :], ax1[:, :])
            for c in range(8):
                bk = 2 + c // 4
                pe([tk, 'ident'], ['pb%d' % bk],
                   lambda e, c=c, bk=bk: e.transpose(pb[bk][:, (c % 4) * 128:(c % 4 + 1) * 128], T[:, c * 128:(c + 1) * 128], ident[:, :]))
            for hb in range(2):
                bk = 2 + hb
                dve(['pb%d' % bk], ['x1T32'],
                    lambda e, hb=hb, bk=bk: e.tensor_copy(out=x1T32[:, hb * 4:(hb + 1) * 4, :],
                                                          in_=pb[bk][:, :].rearrange("p (c t) -> p c t", c=4)))
            for c in range(8):
                pe(['x1T32', 'rw32'], ['pb4'],
                   lambda e, c=c: e.matmul(pb[4][:, 0:N_EXP], x1T32[:, c, :], rw32[:, c, :], start=(c == 0), stop=(c == 7)))
            dve(['pb4', 'rbB'], ['lg'], lambda e: e.tensor_tensor(out=lg[:, :], in0=pb[4][:, 0:N_EXP], in1=rbB[:, :], op=ALU.add))
            dve(['lg'], ['m8'], lambda e: e.max(out=m8[:, :], in_=lg[:, :]))
            dve(['m8'], ['nm'], lambda e: e.tensor_scalar(out=nm[:, :], in0=m8[:, 0:1], scalar1=-1.0, scalar2=None, op0=ALU.mult))
            act(['m8', 'nm'], ['ex'], lambda e: e.activation(out=ex[:, :], in_=m8[:, 0:4], func=AF.Exp, bias=nm[:, 0:1], scale=1.0))
            dve(['ex'], ['den'], lambda e: e.reduce_sum(out=den[:, :], in_=ex[:, :], axis=AX.X))
            dve(['den'], ['den'], lambda e: e.reciprocal(out=den[:, :], in_=den[:, :]))
            dve(['ex', 'den'], ['ex'], lambda e: e.tensor_scalar(out=ex[:, :], in0=ex[:, :], scalar1=den[:, 0:1], scalar2=None, op0=ALU.mult))
            dve(['lg', 'm8'], ['msk'], lambda e: e.tensor_scalar(out=msk[:, :], in0=lg[:, :], scalar1=m8[:, 3:4], scalar2=None, op0=ALU.is_ge))
            dve(['msk'], ['Mb'], lambda e: e.tensor_copy(out=Mb[:, i, :], in_=msk[:, :]))
            for i2 in range(i):
                pe(['onesb', 'Mb'], ['pb5'], lambda e, i2=i2: e.matmul(pb[5][:, 0:N_EXP], onesb[:, :], Mb[:, i2, :], start=(i2 == 0), stop=False))
            pe(['trib', 'Mb'], ['pb5'], lambda e: e.matmul(pb[5][:, 0:N_EXP], trib[:, :], Mb[:, i, :], start=(i == 0), stop=True))
            dve(['pb5'], ['rnk'], lambda e: e.tensor_copy(out=rnk[:, :], in_=pb[5][:, 0:N_EXP]))
            dve(['rnk', 'pcst'], ['val'], lambda e: e.tensor_tensor(out=val[:, :], in0=rnk[:, :], in1=CE, op=ALU.add))
            for k in range(4):
                dve(['lg', 'm8', 'val'], ['prod'],
                    lambda e, k=k: e.scalar_tensor_tensor(out=prod[:, :], in0=lg[:, :], scalar=m8[:, k:k + 1], in1=val[:, :],
                                                          op0=ALU.is_equal, op1=ALU.mult))
                dve(['prod'], ['gsf'], lambda e, k=k: e.reduce_sum(out=gsf[:, k:k + 1], in_=prod[:, :], axis=AX.X))
                dve(['lg', 'm8', 'rnk'], ['prod'],
                    lambda e, k=k: e.scalar_tensor_tensor(out=prod[:, :], in0=lg[:, :], scalar=m8[:, k:k + 1], in1=rnk[:, :],
                                                          op0=ALU.is_equal, op1=ALU.mult))
                dve(['prod'], ['rkf'], lambda e, k=k: e.reduce_sum(out=rkf[:, k:k + 1], in_=prod[:, :], axis=AX.X))
            dve(['rkf'], ['ov'], lambda e: e.tensor_scalar(out=ov[:, :], in0=rkf[:, :], scalar1=float(CAP) - 0.5, scalar2=None, op0=ALU.is_ge))
            dve(['ov'], ['nov'], lambda e: e.tensor_scalar(out=nov[:, :], in0=ov[:, :], scalar1=-1.0, scalar2=1.0, op0=ALU.mult, op1=ALU.add))
            dve(['gsf', 'nov'], ['gsf'], lambda e: e.tensor_tensor(out=gsf[:, :], in0=gsf[:, :], in1=nov[:, :], op=ALU.mult))
            dve(['ov', 'pcst'], ['ov'], lambda e: e.tensor_scalar(out=ov[:, :], in0=ov[:, :], scalar1=DCOL, scalar2=None, op0=ALU.mult))
            dve(['gsf', 'ov'], ['gsf'], lambda e: e.tensor_tensor(out=gsf[:, :], in0=gsf[:, :], in1=ov[:, :], op=ALU.add))
            dve(['gsf'], ['GS'], lambda e: e.tensor_copy(out=GS[:, i * 4:(i + 1) * 4], in_=gsf[:, :]))
            dve(['ex', 'nov'], ['GA'], lambda e: e.tensor_tensor(out=GA[:, i, :], in0=ex[:, :], in1=nov[:, :], op=ALU.mult))
            for k in range(4):
                ind([xbk, 'GS'], ['XG%d' % i], out=XG[:, :], out_offset=bass.IndirectOffsetOnAxis(ap=GS[:, i * 4 + k:i * 4 + k + 1], axis=0),
                    in_=xb_[:, :], in_offset=None, bounds_check=bc_reg, oob_is_err=False)

        if _STAGE == 21:
            kb.finish('sp')
            return nc
        for _ in range(NSLOT):
            load_next_block()
        nuse = [0]
        pair = 0
        obn = 0
        for e_ in range(n_exp):
            kb.dma('sp', [], ['b2t'], b2t[:, :], b2[e_:e_ + 1, :])
            kb.dma('sp', ['XG%d' % q for q in range(NT)], ['xg'], xg[:, :, :], XG[e_ * CAP:(e_ + 1) * CAP, :].rearrange("(st p) f -> p st f", p=128))
            for cc in range(4):
                ptk = 'pb%d' % (6 + cc % 2)
                for c2 in range(2):
                    c = cc * 2 + c2
                    for st in range(4):
                        pe(['xg', 'identb'], [ptk],
                           lambda e, c=c, c2=c2, st=st, cc=cc: e.transpose(pt[cc % 2][:, c2 * 512 + st * 128:c2 * 512 + (st + 1) * 128],
                                                                          xg[:, st, c * 128:(c + 1) * 128], identb[:, :]))
                if cc % 2 == 0:
                    act([ptk], ['xgT'], lambda e, cc=cc: e.activation(out=xgT[:, cc * 2:cc * 2 + 2, :],
                                                                    in_=pt[cc % 2][:, :].rearrange("p (c t) -> p c t", c=2), func=AF.Copy))
                else:
                    dve([ptk], ['xgT'], lambda e, cc=cc: e.tensor_copy(out=xgT[:, cc * 2:cc * 2 + 2, :],
                                                                      in_=pt[cc % 2][:, :].rearrange("p (c t) -> p c t", c=2)))
            for fcb in range(2):
                sa = nuse[0] % NSLOT
                sl = (nuse[0] + 1) % NSLOT
                for f4 in range(4):
                    fc = fcb * 4 + f4
                    hg, hl, tb = pair % 2, 2 + pair % 2, pair % 2
                    pair += 1
                    for c in range(8):
                        pe(['slot%d' % sa, 'xgT'], ['pb%d' % hg],
                           lambda e, c=c: e.matmul(pb[hg][:, :], slots[sa][:, c, f4 * 128:(f4 + 1) * 128], xgT[:, c, :],
                                                   start=(c == 0), stop=(c == 7)))
                    for c in range(8):
                        pe(['slot%d' % sl, 'xgT'], ['pb%d' % hl],
                           lambda e, c=c: e.matmul(pb[hl][:, :], slots[sl][:, c, f4 * 128:(f4 + 1) * 128], xgT[:, c, :],
                                                   start=(c == 0), stop=(c == 7)))
                    G, S_, L = g32[tb], s32[tb], l32[tb]
                    gk, sk, lk = 'g32_%d' % tb, 's32_%d' % tb, 'l32_%d' % tb
                    dve(['pb%d' % hg, 'b1s'], [gk], lambda e: e.tensor_scalar(out=G[:, :], in0=pb[hg][:, :], scalar1=b1s[:, e_, fc:fc + 1],
                                                                             scalar2=7.0, op0=ALU.add, op1=ALU.min))
                    act([gk], [sk], lambda e: e.activation(out=S_[:, :], in_=G[:, :], func=AF.Sigmoid, scale=1.702))
                    dve(['pb%d' % hl, 'b1s'], [lk], lambda e: e.tensor_scalar(out=L[:, :], in0=pb[hl][:, :], scalar1=b1s[:, e_, 8 + fc:9 + fc],
                                                                             scalar2=-7.0, op0=ALU.add, op1=ALU.max))
                    dve([lk], [lk], lambda e: e.tensor_scalar(out=L[:, :], in0=L[:, :], scalar1=7.0, scalar2=1.0, op0=ALU.min, op1=ALU.add))
                    dve([gk, sk], [sk], lambda e: e.tensor_tensor(out=S_[:, :], in0=G[:, :], in1=S_[:, :], op=ALU.mult))
                    dve([sk, lk], ['actT'], lambda e: e.tensor_tensor(out=actT[:, fc, :], in0=S_[:, :], in1=L[:, :], op=ALU.mult))
                nuse[0] += 2
                load_next_block()
                load_next_block()
            for h in range(2):
                s2 = nuse[0] % NSLOT
                for st in range(4):
                    ob = 4 + obn % 2
                    yb_ = ysl[obn % 3]
                    ybk = 'ysl%d' % (obn % 3)
                    obn += 1
                    pe(['ones32', 'b2t'], ['pb%d' % ob],
                       lambda e: e.matmul(pb[ob][:, :], ones32[0:1, :], b2t[0:1, h * 512:(h + 1) * 512], start=True, stop=False))
                    for fc in range(8):
                        pe(['actT', 'slot%d' % s2], ['pb%d' % ob],
                           lambda e, fc=fc: e.matmul(pb[ob][:, :], actT[:, fc, st * 128:(st + 1) * 128], slots[s2][:, fc, :],
                                                     start=False, stop=(fc == 7)))
                    act(['pb%d' % ob], [ybk], lambda e: e.activation(out=yb_[:, :], in_=pb[ob][:, :], func=AF.Copy))
                    r0 = e_ * CAP + st * 128
                    kb.dma('sp', [ybk], ['YG%d' % e_], YG[r0:r0 + 128, h * 512:(h + 1) * 512], yb_[:, :])
                nuse[0] += 1
                load_next_block()

        if _STAGE == 22:
            kb.finish('sp')
            return nc
        kb.dma('sp', [], ['gB'], gB[:, :], ln2g.partition_broadcast(128))
        kb.dma('sp', [], ['bB'], bB[:, :], ln2b.partition_broadcast(128))
        nk = 0
        for i in range(NT):
            T, tk = Tb[i % 2], 'Tb%d' % (i % 2)
            kb.dma('sp', ['AX1_%d' % i], [tk], T[:, :], AX1[i * 128:(i + 1) * 128, :])
            for k in range(4):
                Y, ykk = yk[0], 'yk0'
                ind(['YGz', 'GS'] + ['YG%d' % q for q in range(n_exp)], [ykk], out=Y[:, :], out_offset=None, in_=YG[:, :],
                    in_offset=bass.IndirectOffsetOnAxis(ap=GS[:, i * 4 + k:i * 4 + k + 1], axis=0), bounds_check=bc_reg, oob_is_err=False)
                dve([ykk, 'GA', tk], [tk],
                    lambda e, k=k, Y=Y: e.scalar_tensor_tensor(out=T[:, :], in0=Y[:, :], scalar=GA[:, i, k:k + 1], in1=T[:, :],
                                                               op0=ALU.mult, op1=ALU.add))
            layer_norm_inplace(kb, T[:, :], tk, (gB, 'gB'), (bB, 'bB'), stats, mv, rs, '')
            kb.dma('sp', [tk], ['out%d' % i], out[i * 128:(i + 1) * 128, :], T[:, :])
        kb.finish('sp')
    return nc


def post2_consts():
    c = np.zeros((128, 128 + N_EXP + 1), np.float32)
    k = np.arange(128)[:, None]
    m = np.arange(128)[None, :]
    c[:, 0:128] = (k < m)
    c[:, 128:128 + N_EXP] = CAP * np.arange(N_EXP, dtype=np.float32)[None, :]
    c[:, 128 + N_EXP] = N_EXP * CAP + np.arange(128)
    return c
```

```python
import numpy as np
from contextlib import ExitStack
import concourse.bass as bass
import concourse.mybir as mybir
from concourse.bass_utils import run_bass_kernel_spmd

F32 = mybir.dt.float32
BF16 = mybir.dt.bfloat16
AF = mybir.ActivationFunctionType
ALU = mybir.AluOpType
AX = mybir.AxisListType

D = 1024
SEQ = 8192
NB = 2
NCORES = 8
ALPHA = float((2 * 2) ** 0.25)
LN_EPS = 1e-5
N_EXP = 32


class KB:
    def __init__(self, nc, es, n_dma_sems=24):
        self.nc = nc
        self.es = es
        self.E = {'pe': nc.tensor, 'dve': nc.vector, 'act': nc.scalar, 'pool': nc.gpsimd, 'sp': nc.sync}
        self.sem = {}
        self.cnt = {}
        for k in ['pe', 'dve', 'act', 'pool']:
            self.sem[k] = es.enter_context(nc.semaphore('s_' + k))
            self.cnt[k] = 0
        self.dsem = []
        for i in range(n_dma_sems):
            self.dsem.append(es.enter_context(nc.semaphore('d%d' % i)))
        self.dcnt = [0] * n_dma_sems
        self.dnext = 0
        self.waited = {k: {} for k in self.E}
        self.lastw = {}
        self.readers = {}
        self.ninst = 0

    def sb(self, name, shape, dt):
        return self.es.enter_context(self.nc.sbuf_tensor("sb_" + name, shape, dt))

    def ps(self, name, shape, dt=F32):
        return self.es.enter_context(self.nc.psum_tensor("ps_" + name, shape, dt))

    def _deps(self, reads, writes):
        deps = {}

        def add(t):
            if t is None:
                return
            s, v = t
            if deps.get(s, 0) < v:
                deps[s] = v
        for r in reads:
            add(self.lastw.get(r))
            if isinstance(r, str) and r.startswith('pb'):
                for t in self.readers.get(r, ()):
                    add(t)
        for w in writes:
            add(self.lastw.get(w))
            for t in self.readers.get(w, ()):
                add(t)
        return deps

    def _wait(self, eng, deps, skip=None):
        for s, v in deps.items():
            if skip is not None and s == skip:
                continue
            if self.waited[eng].get(s, 0) < v:
                semobj = self.sem[s] if isinstance(s, str) else self.dsem[s]
                self.E[eng].wait_ge(semobj, v)
                self.waited[eng][s] = v

    def _commit(self, tok, reads, writes):
        for r in reads:
            self.readers.setdefault(r, []).append(tok)
        for w in writes:
            self.lastw[w] = tok
            self.readers[w] = []

    def op(self, eng, reads, writes, emit):
        deps = self._deps(reads, writes)
        self._wait(eng, deps, skip='pe' if eng == 'pe' else None)
        inst = emit(self.E[eng])
        self.cnt[eng] += 1
        inst.then_inc(self.sem[eng], 1)
        self._commit((eng, self.cnt[eng]), reads, writes)
        self.ninst += 1
        return inst

    def dma(self, q, reads, writes, out, in_, **kw):
        deps = self._deps(reads, writes)
        i = self.dnext
        self.dnext = (self.dnext + 1) % len(self.dsem)
        if self.dcnt[i] > 0:
            deps[i] = max(deps.get(i, 0), self.dcnt[i])
        self._wait(q, deps)
        inst = self.E[q].dma_start(out=out, in_=in_, **kw)
        self.dcnt[i] += 16
        inst.then_inc(self.dsem[i], 16)
        self._commit((i, self.dcnt[i]), reads, writes)
        self.ninst += 1
        return inst

    def finish(self, eng='sp'):
        deps = {}
        for t in self.lastw.values():
            s, v = t
            if deps.get(s, 0) < v:
                deps[s] = v
        self._wait(eng, deps)


class _Stop(Exception):
    pass


import os as _os
_STAGE = int(_os.environ.get("KSTAGE", "0"))


def stage(k):
    if _STAGE == k:
        raise _Stop()


def _din(nc, name, shape, dt=F32):
    return nc.dram_tensor(name, list(shape), dt, kind="ExternalInput").ap()


def _dout(nc, name, shape, dt=F32):
    return nc.dram_tensor(name, list(shape), dt, kind="ExternalOutput").ap()


def layer_norm_inplace(kb, t, tkey, gB, bB, stats, mv, rs, sfx):
    kb.op('dve', [tkey], ['stats' + sfx], lambda e: e.bn_stats(out=stats[:, 0, :], in_=t[:, 0:512]))
    kb.op('dve', [tkey], ['stats' + sfx], lambda e: e.bn_stats(out=stats[:, 1, :], in_=t[:, 512:1024]))
    kb.op('dve', ['stats' + sfx], ['mv' + sfx],
          lambda e: e.bn_aggr(out=mv[:, :], in_=stats[:, :, :].rearrange("p a b -> p (a b)")))
    kb.op('act', ['mv' + sfx], ['rs' + sfx],
          lambda e: e.activation(out=rs[:, :], in_=mv[:, 1:2], func=AF.Sqrt, bias=kb.eps_t[:, 0:1], scale=1.0))
    kb.op('dve', ['rs' + sfx], ['rs' + sfx], lambda e: e.reciprocal(out=rs[:, :], in_=rs[:, :]))
    kb.op('dve', [tkey, 'mv' + sfx, 'rs' + sfx], [tkey],
          lambda e: e.tensor_scalar(out=t, in0=t, scalar1=mv[:, 0:1], scalar2=rs[:, 0:1],
                                    op0=ALU.subtract, op1=ALU.mult))
    kb.op('dve', [tkey, gB[1]], [tkey], lambda e: e.tensor_tensor(out=t, in0=t, in1=gB[0][:, :], op=ALU.mult))
    kb.op('dve', [tkey, bB[1]], [tkey], lambda e: e.tensor_tensor(out=t, in0=t, in1=bB[0][:, :], op=ALU.add))


def build_post(n_exp=N_EXP, npass=None):
    nc = bass.Bass("TRN2", target_bir_lowering=False)
    NTOK = 2048
    PT = 1024
    TT = PT // 128
    NPASS = npass or NTOK // PT
    yT = _din(nc, "yT", [D, NTOK])
    xr = _din(nc, "xr", [NTOK, D])
    w_out = _din(nc, "w_out", [D, D])
    ln1g = _din(nc, "ln1g", [D])
    ln1b = _din(nc, "ln1b", [D])
    rw = _din(nc, "rw", [D, N_EXP])
    rb = _din(nc, "rb", [N_EXP])
    w1 = _din(nc, "w1", [N_EXP, D, 2 * D])
    b1T = _din(nc, "b1T", [128, N_EXP, 16])
    w2 = _din(nc, "w2", [N_EXP, D, D])
    b2 = _din(nc, "b2", [N_EXP, D])
    ln2g = _din(nc, "ln2g", [D])
    ln2b = _din(nc, "ln2b", [D])
    ident_d = _din(nc, "ident", [128, 128])
    out = _dout(nc, "out", [NTOK, D])

    with ExitStack() as es:
        kb = KB(nc, es)
        yacc = kb.sb("yacc", [128, TT, D], F32)
        x1T = kb.sb("x1T", [128, 8, PT], BF16)
        gate = kb.sb("gate", [128, TT, N_EXP], F32)
        NSLOT = 6
        slots = [kb.sb("wslot%d" % i, [128, 8, 512], BF16) for i in range(NSLOT)]
        actT = kb.sb("actT", [128, 8, PT], BF16)
        g32 = [kb.sb("g32_%d" % i, [128, 512], F32) for i in range(2)]
        s32 = [kb.sb("s32_%d" % i, [128, 512], F32) for i in range(2)]
        l32 = [kb.sb("l32_%d" % i, [128, 512], F32) for i in range(2)]
        gB = kb.sb("gB", [128, D], F32)
        bB = kb.sb("bB", [128, D], F32)
        yTb = [kb.sb("yTb%d" % i, [128, 8, 128], BF16) for i in range(2)]
        xrt = kb.sb("xrt", [128, D], F32)
        x1T32 = kb.sb("x1T32", [128, 8, 128], F32)
        rw32 = kb.sb("rw32", [128, 8, N_EXP], F32)
        rbB = kb.sb("rbB", [128, N_EXP], F32)
        b1s = kb.sb("b1s", [128, N_EXP, 16], F32)
        b2t = kb.sb("b2t", [1, D], F32)
        ones32 = kb.sb("ones32", [1, 128], F32)
        ident = kb.sb("ident", [128, 128], F32)
        woutb = kb.sb("woutb", [128, 8, D], BF16)
        stats = kb.sb("stats", [128, 2, 6], F32)
        mv = kb.sb("mv", [128, 2], F32)
        rs = kb.sb("rs", [128, 1], F32)
        lg = kb.sb("lg", [128, N_EXP], F32)
        m8 = kb.sb("m8", [128, 8], F32)
        msk = kb.sb("msk", [128, N_EXP], F32)
        nm = kb.sb("nm", [128, 1], F32)
        ex = kb.sb("ex", [128, N_EXP], F32)
        den = kb.sb("den", [128, 1], F32)
        eps_t = kb.sb("eps_t", [128, 1], F32)
        kb.eps_t = eps_t
        pb = [kb.ps("pb%d" % i, [128, 512]) for i in range(8)]

        kb.op('dve', [], ['eps'], lambda e: e.memset(eps_t[:, :], LN_EPS))
        kb.op('dve', [], ['ones32'], lambda e: e.memset(ones32[:, :], 1.0))
        kb.dma('sp', [], ['ident'], ident[:, :], ident_d[:, :])
        kb.dma('sp', [], ['rw32'], rw32[:, :, :], rw.rearrange("(c p) e -> p c e", p=128))
        kb.dma('sp', [], ['rbB'], rbB[:, :], rb.partition_broadcast(128))
        kb.dma('sp', [], ['b1s'], b1s[:, :, :], b1T[:, :, :])
        kb.dma('pool', [], ['woutb'], woutb[:, :, :], w_out.rearrange("(c p) n -> p c n", p=128))

        blocks = []
        for p_ in range(NPASS):
            for e_ in range(n_exp):
                for b_ in range(6):
                    blocks.append((e_, b_))
        nload = [0]

        def load_next_block():
            n = nload[0]
            if n >= len(blocks):
                return
            e_, b_ = blocks[n]
            s = n % NSLOT
            if b_ < 4:
                c0 = [0, 1024, 512, 1536][b_]
                src = w1[e_, :, c0:c0 + 512].rearrange("(c p) n -> p c n", p=128)
            else:
                c0 = (b_ - 4) * 512
                src = w2[e_, :, c0:c0 + 512].rearrange("(c p) n -> p c n", p=128)
            kb.dma('pool', [], ['slot%d' % s], slots[s][:, :, :], src)
            nload[0] += 1

        for _ in range(NSLOT):
            load_next_block()
        nuse = [0]

        try:
            stage(1)
            for ps_ in range(NPASS):
                tok0 = ps_ * PT
                kb.dma('sp', ['eps'], ['gB'], gB[:, :], ln1g.partition_broadcast(128))
                kb.dma('sp', ['eps'], ['bB'], bB[:, :], ln1b.partition_broadcast(128))
                for i in range(TT):
                    t0 = tok0 + i * 128
                    yb = yTb[i % 2]
                    ybk = 'yTb%d' % (i % 2)
                    kb.dma('pool', [], [ybk], yb[:, :, :], yT[:, t0:t0 + 128].rearrange("(c p) t -> p c t", p=128))
                    kb.dma('sp', [], ['xrt'], xrt[:, :], xr[t0:t0 + 128, :])
                    for h in range(2):
                        for c in range(8):
                            kb.op('pe', [ybk, 'woutb'], ['pb%d' % h],
                                  lambda e, c=c, h=h: e.matmul(pb[h][:, :], yb[:, c, :], woutb[:, c, h * 512:(h + 1) * 512],
                                                               start=(c == 0), stop=(c == 7)))
                    yk = 'yacc%d' % i
                    for h in range(2):
                        kb.op('dve', ['xrt', 'pb%d' % h], [yk],
                              lambda e, h=h: e.scalar_tensor_tensor(out=yacc[:, i, h * 512:(h + 1) * 512],
                                                                    in0=xrt[:, h * 512:(h + 1) * 512], scalar=ALPHA,
                                                                    in1=pb[h][:, :], op0=ALU.mult, op1=ALU.add))
                    stage(2)
                    layer_norm_inplace(kb, yacc[:, i, :], yk, (gB, 'gB'), (bB, 'bB'), stats, mv, rs, '')
                    stage(3)
                    for c in range(8):
                        bk = 2 + c // 4
                        kb.op('pe', [yk, 'ident'], ['pb%d' % bk],
                              lambda e, c=c, bk=bk: e.transpose(pb[bk][:, (c % 4) * 128:(c % 4 + 1) * 128],
                                                                yacc[:, i, c * 128:(c + 1) * 128], ident[:, :]))
                    for hb in range(2):
                        bk = 2 + hb
                        kb.op('act', ['pb%d' % bk], ['x1T'],
                              lambda e, hb=hb, bk=bk: e.activation(
                                  out=x1T[:, hb * 4:(hb + 1) * 4, i * 128:(i + 1) * 128],
                                  in_=pb[bk][:, :].rearrange("p (c t) -> p c t", c=4), func=AF.Copy))
                        kb.op('dve', ['pb%d' % bk], ['x1T32'],
                              lambda e, hb=hb, bk=bk: e.tensor_copy(
                                  out=x1T32[:, hb * 4:(hb + 1) * 4, :],
                                  in_=pb[bk][:, :].rearrange("p (c t) -> p c t", c=4)))
                    stage(4)
                    kb.op('act', [yk], [yk], lambda e: e.mul(yacc[:, i, :], yacc[:, i, :], ALPHA))
                    stage(5)
                    for c in range(8):
                        kb.op('pe', ['x1T32', 'rw32'], ['pb4'],
                              lambda e, c=c: e.matmul(pb[4][:, 0:N_EXP], x1T32[:, c, :], rw32[:, c, :],
                                                      start=(c == 0), stop=(c == 7)))
                    kb.op('dve', ['pb4', 'rbB'], ['lg'],
                          lambda e: e.tensor_tensor(out=lg[:, :], in0=pb[4][:, 0:N_EXP], in1=rbB[:, :], op=ALU.add))
                    kb.op('dve', ['lg'], ['m8'], lambda e: e.max(out=m8[:, :], in_=lg[:, :]))
                    kb.op('dve', ['lg', 'm8'], ['msk'],
                          lambda e: e.tensor_scalar(out=msk[:, :], in0=lg[:, :], scalar1=m8[:, 3:4], scalar2=None,
                                                    op0=ALU.is_ge))
                    kb.op('dve', ['m8'], ['nm'],
                          lambda e: e.tensor_scalar(out=nm[:, :], in0=m8[:, 0:1], scalar1=-1.0, scalar2=None,
                                                    op0=ALU.mult))
                    kb.op('act', ['lg', 'nm'], ['ex'],
                          lambda e: e.activation(out=ex[:, :], in_=lg[:, :], func=AF.Exp, bias=nm[:, 0:1], scale=1.0))
                    kb.op('dve', ['ex', 'msk'], ['ex'],
                          lambda e: e.tensor_tensor(out=ex[:, :], in0=ex[:, :], in1=msk[:, :], op=ALU.mult))
                    kb.op('dve', ['ex'], ['den'], lambda e: e.reduce_sum(out=den[:, :], in_=ex[:, :], axis=AX.X))
                    kb.op('dve', ['den'], ['den'], lambda e: e.reciprocal(out=den[:, :], in_=den[:, :]))
                    kb.op('dve', ['ex', 'den'], ['gate'],
                          lambda e: e.tensor_scalar(out=gate[:, i, :], in0=ex[:, :], scalar1=den[:, 0:1], scalar2=None,
                                                    op0=ALU.mult))

                    stage(6)
                stage(7)
                pair = 0
                obn = 0
                for e_ in range(n_exp):
                    kb.dma('sp', [], ['b2t'], b2t[:, :], b2[e_:e_ + 1, :])
                    for fcb in range(2):
                        sa = nuse[0] % NSLOT
                        sl = (nuse[0] + 1) % NSLOT
                        for tg in range(PT // 512):
                            for f4 in range(4):
                                fc = fcb * 4 + f4
                                hg = pair % 2
                                hl = 2 + pair % 2
                                tb = pair % 2
                                pair += 1
                                for c in range(8):
                                    kb.op('pe', ['slot%d' % sa, 'x1T'], ['pb%d' % hg],
                                          lambda e, c=c, hg=hg, sa=sa, f4=f4, tg=tg: e.matmul(
                                              pb[hg][:, :], slots[sa][:, c, f4 * 128:(f4 + 1) * 128],
                                              x1T[:, c, tg * 512:(tg + 1) * 512], start=(c == 0), stop=(c == 7)))
                                for c in range(8):
                                    kb.op('pe', ['slot%d' % sl, 'x1T'], ['pb%d' % hl],
                                          lambda e, c=c, hl=hl, sl=sl, f4=f4, tg=tg: e.matmul(
                                              pb[hl][:, :], slots[sl][:, c, f4 * 128:(f4 + 1) * 128],
                                              x1T[:, c, tg * 512:(tg + 1) * 512], start=(c == 0), stop=(c == 7)))
                                G, S, L = g32[tb], s32[tb], l32[tb]
                                gk, sk, lk = 'g32_%d' % tb, 's32_%d' % tb, 'l32_%d' % tb
                                kb.op('dve', ['pb%d' % hg, 'b1s'], [gk],
                                      lambda e, G=G, hg=hg, fc=fc: e.tensor_scalar(
                                          out=G[:, :], in0=pb[hg][:, :], scalar1=b1s[:, e_, fc:fc + 1], scalar2=7.0,
                                          op0=ALU.add, op1=ALU.min))
                                kb.op('act', [gk], [sk],
                                      lambda e, G=G, S=S: e.activation(out=S[:, :], in_=G[:, :], func=AF.Sigmoid, scale=1.702))
                                kb.op('dve', ['pb%d' % hl, 'b1s'], [lk],
                                      lambda e, L=L, hl=hl, fc=fc: e.tensor_scalar(
                                          out=L[:, :], in0=pb[hl][:, :], scalar1=b1s[:, e_, 8 + fc:9 + fc], scalar2=-7.0,
                                          op0=ALU.add, op1=ALU.max))
                                kb.op('dve', [lk], [lk],
                                      lambda e, L=L: e.tensor_scalar(out=L[:, :], in0=L[:, :], scalar1=7.0, scalar2=1.0,
                                                                     op0=ALU.min, op1=ALU.add))
                                kb.op('dve', [gk, sk], [sk],
                                      lambda e, G=G, S=S: e.tensor_tensor(out=S[:, :], in0=G[:, :], in1=S[:, :], op=ALU.mult))
                                kb.op('dve', [sk, lk], ['actT'],
                                      lambda e, S=S, L=L, fc=fc, tg=tg: e.tensor_tensor(
                                          out=actT[:, fc, tg * 512:(tg + 1) * 512], in0=S[:, :], in1=L[:, :], op=ALU.mult))
                        nuse[0] += 2
                        load_next_block()
                        load_next_block()
                    stage(8)
                    for h in range(2):
                        s2 = nuse[0] % NSLOT
                        for tt in range(TT):
                            ob = 4 + obn % 3
                            obn += 1
                            kb.op('pe', ['ones32', 'b2t'], ['pb%d' % ob],
                                  lambda e, ob=ob, h=h: e.matmul(pb[ob][:, :], ones32[0:1, :], b2t[0:1, h * 512:(h + 1) * 512],
                                                                 start=True, stop=False))
                            for fc in range(8):
                                kb.op('pe', ['actT', 'slot%d' % s2], ['pb%d' % ob],
                                      lambda e, ob=ob, fc=fc, tt=tt, s2=s2: e.matmul(
                                          pb[ob][:, :], actT[:, fc, tt * 128:(tt + 1) * 128], slots[s2][:, fc, :],
                                          start=False, stop=(fc == 7)))
                            yk = 'yacc%d' % tt
                            kb.op('dve', ['pb%d' % ob, 'gate', yk], [yk],
                                  lambda e, ob=ob, tt=tt, h=h: e.scalar_tensor_tensor(
                                      out=yacc[:, tt, h * 512:(h + 1) * 512], in0=pb[ob][:, :],
                                      scalar=gate[:, tt, e_:e_ + 1], in1=yacc[:, tt, h * 512:(h + 1) * 512],
                                      op0=ALU.mult, op1=ALU.add))
                        nuse[0] += 1
                        load_next_block()

                stage(9)
                kb.dma('sp', [], ['gB'], gB[:, :], ln2g.partition_broadcast(128))
                kb.dma('sp', [], ['bB'], bB[:, :], ln2b.partition_broadcast(128))
                for i in range(TT):
                    yk = 'yacc%d' % i
                    layer_norm_inplace(kb, yacc[:, i, :], yk, (gB, 'gB'), (bB, 'bB'), stats, mv, rs, '')
                    kb.dma('sp', [yk], ['out'], out[tok0 + i * 128:tok0 + (i + 1) * 128, :], yacc[:, i, :])
        except _Stop:
            pass
        kb.finish('sp')
    return nc


def build_sb(n_pairs=2, n_qg=16):
    nc = bass.Bass("TRN2", target_bir_lowering=False)
    S = SEQ
    xT = _din(nc, "xT", [D, S])
    wq = _din(nc, "wq", [D, 256])
    wk = _din(nc, "wk", [D, 256])
    wv = _din(nc, "wv", [D, 256])
    maskd = _din(nc, "mask", [128, 4, 512])
    negtri_d = _din(nc, "negtri", [128, 128])
    yT = _dout(nc, "yT", [256, S])
    with ExitStack() as es:
        kb = KB(nc, es)
        qT = [kb.sb("qT%d" % i, [64, S], BF16) for i in range(2)]
        kT = [kb.sb("kT%d" % i, [64, S], BF16) for i in range(2)]
        v = kb.sb("v", [128, S // 128, 128], BF16)
        xTb = [kb.sb("xTb%d" % i, [128, 8, 512], BF16) for i in range(2)]
        wqb = kb.sb("wqb", [128, 8, 128], BF16)
        wkb = kb.sb("wkb", [128, 8, 128], BF16)
        wvb = kb.sb("wvb", [128, 8, 128], BF16)
        e_t = [kb.sb("e_t%d" % i, [128, 512], F32) for i in range(2)]
        sp_t = [kb.sb("sp_t%d" % i, [128, 512], F32) for i in range(3)]
        att_t = [kb.sb("att_t%d" % i, [128, 512], BF16) for i in range(2)]
        sacc = [kb.sb("sacc%d" % i, [128, 512], F32) for i in range(2)]
        mask = kb.sb("mask", [128, 4, 512], F32)
        negtri = kb.sb("negtri", [128, 128], F32)
        negones = kb.sb("negones", [128, 128], F32)
        osb = [kb.sb("osb%d" % i, [64, 512], F32) for i in range(2)]
        sacch = [kb.sb("sacch%d" % i, [128, 512], BF16) for i in range(2)]
        saccl = [kb.sb("saccl%d" % i, [128, 512], BF16) for i in range(2)]
        negonesb = kb.sb("negonesb", [128, 128], BF16)
        pb = [kb.ps("pb%d" % i, [128, 512]) for i in range(8)]

        kb.dma('sp', [], ['mask'], mask[:, :, :], maskd[:, :, :])
        kb.dma('sp', [], ['negtri'], negtri[:, :], negtri_d[:, :])
        kb.op('dve', [], ['negones'], lambda e: e.memset(negones[:, :], -1.0))
        kb.op('dve', [], ['negonesb'], lambda e: e.memset(negonesb[:, :], -1.0))

        for hp in range(n_pairs):
            c0 = hp * 128
            kb.dma('pool', [], ['wqb'], wqb[:, :, :], wq[:, c0:c0 + 128].rearrange("(c p) n -> p c n", p=128))
            kb.dma('pool', [], ['wkb'], wkb[:, :, :], wk[:, c0:c0 + 128].rearrange("(c p) n -> p c n", p=128))
            kb.dma('pool', [], ['wvb'], wvb[:, :, :], wv[:, c0:c0 + 128].rearrange("(c p) n -> p c n", p=128))
            for tg in range(S // 512):
                xb = xTb[tg % 2]
                xk = 'xTb%d' % (tg % 2)
                kb.dma('pool', [], [xk], xb[:, :, :],
                       xT[:, tg * 512:(tg + 1) * 512].rearrange("(c p) t -> p c t", p=128))
                for hl in range(2):
                    for which in range(2):
                        wb, wkey = (wqb, 'wqb') if which == 0 else (wkb, 'wkb')
                        bk = 6 + which
                        for c in range(8):
                            kb.op('pe', [wkey, xk], ['pb%d' % bk],
                                  lambda e, c=c: e.matmul(pb[bk][0:64, :], wb[:, c, hl * 64:(hl + 1) * 64], xb[:, c, :],
                                                          start=(c == 0), stop=(c == 7)))
                        if which == 0:
                            kb.op('act', ['pb%d' % bk], ['qT%d' % hl],
                                  lambda e: e.activation(out=qT[hl][:, tg * 512:(tg + 1) * 512], in_=pb[bk][0:64, :],
                                                         func=AF.Copy, scale=0.125))
                        else:
                            kb.op('dve', ['pb%d' % bk], ['kT%d' % hl],
                                  lambda e: e.tensor_copy(out=kT[hl][:, tg * 512:(tg + 1) * 512], in_=pb[bk][0:64, :]))
                for tt in range(4):
                    tile_i = tg * 4 + tt
                    bk = 4 + tt % 2
                    for c in range(8):
                        kb.op('pe', ['wvb', xk], ['pb%d' % bk],
                              lambda e, c=c: e.matmul(pb[bk][:, 0:128], xb[:, c, tt * 128:(tt + 1) * 128], wvb[:, c, :],
                                                      start=(c == 0), stop=(c == 7)))
                    if tt % 2 == 0:
                        kb.op('act', ['pb%d' % bk], ['v'],
                              lambda e: e.activation(out=v[:, tile_i, :], in_=pb[bk][:, 0:128], func=AF.Copy))
                    else:
                        kb.op('dve', ['pb%d' % bk], ['v'],
                              lambda e: e.tensor_copy(out=v[:, tile_i, :], in_=pb[bk][:, 0:128]))

            tiles = []
            g = 0
            for hl in range(2):
                for qg in range(n_qg):
                    for kt in range(4 * qg + 3, -1, -1):
                        tiles.append(dict(hl=hl, qg=qg, kt=kt, j=(kt - 4 * qg if kt >= 4 * qg else None),
                                          first=(kt == 4 * qg + 3), last=(kt == 0), g=g, i=len(tiles)))
                    g += 1
            n = len(tiles)

            def S1(t):
                i = t['i']
                kb.op('pe', ['kT%d' % t['hl'], 'qT%d' % t['hl']], ['pb%d' % (i % 2)],
                      lambda e: e.matmul(pb[i % 2][:, :], kT[t['hl']][:, t['kt'] * 128:(t['kt'] + 1) * 128],
                                         qT[t['hl']][:, t['qg'] * 512:(t['qg'] + 1) * 512], start=True, stop=True))

            def S2(t):
                i = t['i']
                ek, sk = 'e_t%d' % (i % 2), 'sp_t%d' % (i % 3)
                kb.op('act', ['pb%d' % (i % 2)], [ek],
                      lambda e: e.activation(out=e_t[i % 2][:, :], in_=pb[i % 2][:, :], func=AF.Exp))
                kb.op('act', [ek], [sk],
                      lambda e: e.activation(out=sp_t[i % 3][:, :], in_=e_t[i % 2][:, :], func=AF.Ln, bias=1.0, scale=1.0))
                if t['j'] is not None:
                    kb.op('dve', [sk, 'mask'], [sk],
                          lambda e: e.tensor_tensor(out=sp_t[i % 3][:, :], in0=sp_t[i % 3][:, :],
                                                    in1=mask[:, t['j'], :], op=ALU.mult))

            def S3(t):
                i = t['i']
                zb = 2 + i % 2
                sk = 'sp_t%d' % (i % 3)
                sa = t['g'] % 2
                kb.op('pe', ['kT%d' % t['hl'], 'qT%d' % t['hl']], ['pb%d' % zb],
                      lambda e: e.matmul(pb[zb][:, :], kT[t['hl']][:, t['kt'] * 128:(t['kt'] + 1) * 128],
                                         qT[t['hl']][:, t['qg'] * 512:(t['qg'] + 1) * 512], start=True, stop=False))
                kb.op('pe', ['negtri', sk], ['pb%d' % zb],
                      lambda e: e.matmul(pb[zb][:, :], negtri[:, :], sp_t[i % 3][:, :], start=False, stop=t['first']))
                if not t['first']:
                    kb.op('pe', ['negonesb', 'sacch%d' % sa], ['pb%d' % zb],
                          lambda e: e.matmul(pb[zb][:, :], negonesb[:, :], sacch[sa][:, :], start=False, stop=False))
                    kb.op('pe', ['negonesb', 'saccl%d' % sa], ['pb%d' % zb],
                          lambda e: e.matmul(pb[zb][:, :], negonesb[:, :], saccl[sa][:, :], start=False, stop=True))
                if not t['last']:
                    if t['first']:
                        kb.op('dve', [sk], ['sacc%d' % sa],
                              lambda e: e.tensor_copy(out=sacc[sa][:, :], in_=sp_t[i % 3][:, :]))
                    else:
                        kb.op('dve', [sk, 'sacc%d' % sa], ['sacc%d' % sa],
                              lambda e: e.tensor_tensor(out=sacc[sa][:, :], in0=sacc[sa][:, :], in1=sp_t[i % 3][:, :],
                                                        op=ALU.add))
                    kb.op('dve', ['sacc%d' % sa], ['sacch%d' % sa],
                          lambda e: e.tensor_copy(out=sacch[sa][:, :], in_=sacc[sa][:, :]))
                    kb.op('dve', ['sacc%d' % sa, 'sacch%d' % sa], ['saccl%d' % sa],
                          lambda e: e.tensor_tensor(out=saccl[sa][:, :], in0=sacc[sa][:, :], in1=sacch[sa][:, :], op=ALU.subtract))

            def S4(t):
                i = t['i']
                zb = 2 + i % 2
                ak = 'att_t%d' % (i % 2)
                kb.op('act', ['pb%d' % zb], [ak],
                      lambda e: e.activation(out=att_t[i % 2][:, :], in_=pb[zb][:, :], func=AF.Exp))
                if t['j'] is not None:
                    kb.op('dve', [ak, 'mask'], [ak],
                          lambda e: e.tensor_tensor(out=att_t[i % 2][:, :], in0=att_t[i % 2][:, :],
                                                    in1=mask[:, t['j'], :], op=ALU.mult))

            def S5(t):
                i = t['i']
                ob = 4 + t['g'] % 2
                ak = 'att_t%d' % (i % 2)
                kb.op('pe', ['v', ak], ['pb%d' % ob],
                      lambda e: e.matmul(pb[ob][0:64, :], v[:, t['kt'], t['hl'] * 64:(t['hl'] + 1) * 64], att_t[i % 2][:, :],
                                         start=t['first'], stop=t['last']))
                if t['last']:
                    ok = 'osb%d' % (t['g'] % 2)
                    kb.op('dve', ['pb%d' % ob], [ok],
                          lambda e: e.tensor_copy(out=osb[t['g'] % 2][:, :], in_=pb[ob][0:64, :]))
                    r0 = (hp * 2 + t['hl']) * 64
                    kb.dma('sp', [ok], ['yT'], yT[r0:r0 + 64, t['qg'] * 512:(t['qg'] + 1) * 512], osb[t['g'] % 2][:, :])

            for s in range(n + 4):
                if s < n:
                    S1(tiles[s])
                if 0 <= s - 1 < n:
                    S2(tiles[s - 1])
                if 0 <= s - 2 < n:
                    S3(tiles[s - 2])
                if 0 <= s - 3 < n:
                    S4(tiles[s - 3])
                if 0 <= s - 4 < n:
                    S5(tiles[s - 4])
        kb.finish('sp')
    return nc


def sb_mask():
    s = np.arange(128)[:, None, None]
    j = np.arange(4)[None, :, None]
    t = np.arange(512)[None, None, :]
    return np.ascontiguousarray((128 * j + s < t).astype(np.float32))


def sb_negtri():
    j = np.arange(128)[:, None]
    s = np.arange(128)[None, :]
    return np.ascontiguousarray(-(j >= s).astype(np.float32))


RW_EPS = 64e-5


def build_rwkv(n_groups=16):
    nc = bass.Bass("TRN2", target_bir_lowering=False)
    S = SEQ
    xT = _din(nc, "xT", [D, S])
    wr = _din(nc, "wr", [D, 128])
    wk = _din(nc, "wk", [D, 128])
    wv = _din(nc, "wv", [D, 128])
    wlo = _din(nc, "wlo", [D, 160])
    pch = _din(nc, "pch", [64, 2, 12])
    plo = _din(nc, "plo", [96, 4])
    w2h = _din(nc, "w2h", [32, 128])
    a2h = _din(nc, "a2h", [32, 128])
    g2h = _din(nc, "g2h", [96, 128])
    cst = _din(nc, "cst", [64, 6, 512])
    yaT = _dout(nc, "yaT", [128, S])
    with ExitStack() as es:
        kb = KB(nc, es)
        xTb = [kb.sb("xTb%d" % i, [128, 8, 512], BF16) for i in range(2)]
        wrb = kb.sb("wrb", [128, 8, 128], BF16)
        wkb = kb.sb("wkb", [128, 8, 128], BF16)
        wvb = kb.sb("wvb", [128, 8, 128], BF16)
        wlob = kb.sb("wlob", [128, 8, 160], BF16)
        pc = kb.sb("pc", [64, 2, 12], F32)
        pl = kb.sb("pl", [96, 4], F32)
        omk = kb.sb("omk", [64, 2], F32)
        w2s = kb.sb("w2s", [32, 128], F32)
        a2s = kb.sb("a2s", [32, 128], F32)
        g2s = kb.sb("g2s", [96, 128], F32)
        cs_ = kb.sb("cst", [64, 6, 512], F32)
        epst = kb.sb("epst", [64, 1], F32)
        praw = {}
        for sig in ['r', 'k', 'v']:
            for hl in range(2):
                for b in range(2):
                    praw[(sig, hl, b)] = kb.sb("praw_%s%d%d" % (sig, hl, b), [64, 513], F32)
        for sig, npart in [('w', 32), ('a', 32), ('g', 96)]:
            for b in range(2):
                praw[(sig, 0, b)] = kb.sb("praw_%s%d" % (sig, b), [npart, 513], F32)
        wlo_s = kb.sb("wlo_s", [32, 512], F32)
        alo_s = kb.sb("alo_s", [32, 512], F32)
        glo_s = kb.sb("glo_s", [96, 512], F32)
        tmp96 = kb.sb("tmp96", [96, 512], F32)
        names = ['R', 'K', 'V', 'A', 'G', 'LW', 'CW', 'E1', 'T1', 'T2', 'T3', 'BON', 'P0', 'P0T', 'Pa', 'PaT', 'Pb',
                 'PbT', 'TT', 'AakT', 'ArbT', 'ArkT', 'Btok', 'Ktok', 'Vtok', 'ysb']
        W = {nm: kb.sb("w_" + nm, [64, 512], F32) for nm in names}
        Xs = kb.sb("Xs", [64, 64], F32)
        Us = kb.sb("Us", [64, 64], F32)
        H = [[kb.sb("H%d%d" % (hl, i), [64, 64], F32) for i in range(2)] for hl in range(2)]
        osb = kb.sb("osb", [64, 512], F32)
        pb = [kb.ps("pb%d" % i, [128, 512]) for i in range(8)]

        def dve(r, w, fn):
            kb.op('dve', r, w, fn)

        def act(r, w, fn):
            kb.op('act', r, w, fn)

        def pe(r, w, fn):
            kb.op('pe', r, w, fn)

        kb.dma('sp', [], ['pc'], pc[:, :, :], pch[:, :, :])
        kb.dma('sp', [], ['pl'], pl[:, :], plo[:, :])
        kb.dma('sp', [], ['w2s'], w2s[:, :], w2h[:, :])
        kb.dma('sp', [], ['a2s'], a2s[:, :], a2h[:, :])
        kb.dma('sp', [], ['g2s'], g2s[:, :], g2h[:, :])
        kb.dma('sp', [], ['cst'], cs_[:, :, :], cst[:, :, :])
        kb.dma('pool', [], ['wrb'], wrb[:, :, :], wr.rearrange("(c p) n -> p c n", p=128))
        kb.dma('pool', [], ['wkb'], wkb[:, :, :], wk.rearrange("(c p) n -> p c n", p=128))
        kb.dma('pool', [], ['wvb'], wvb[:, :, :], wv.rearrange("(c p) n -> p c n", p=128))
        kb.dma('pool', [], ['wlob'], wlob[:, :, :], wlo.rearrange("(c p) n -> p c n", p=128))
        dve([], ['epst'], lambda e: e.memset(epst[:, :], RW_EPS))
        dve(['pc'], ['omk'], lambda e: e.tensor_scalar(out=omk[:, :], in0=pc[:, :, 6], scalar1=-1.0, scalar2=1.0,
                                                       op0=ALU.mult, op1=ALU.add))
        for hl in range(2):
            dve([], ['H%d0' % hl], lambda e: e.memset(H[hl][0][:, :], 0.0))
        hcur = [0, 0]
        MK0, MK1, MK2, IDR, ONE, ONEM = (cs_[:, i, :] for i in range(6))
        ident64 = cs_[:, 3, 0:64]
        ones64 = cs_[:, 4, 0:64]
        onesm64 = cs_[:, 5, 0:64]
        nbk = [0]

        def bank():
            nbk[0] += 1
            return nbk[0] % 2

        for tg in range(n_groups):
            b = tg % 2
            xb = xTb[b]
            xk = 'xTb%d' % b
            kb.dma('pool', [], [xk], xb[:, :, :], xT[:, tg * 512:(tg + 1) * 512].rearrange("(c p) t -> p c t", p=128))

            def proj(wb, wkey, c0, m, dst, dkey, prev):
                bk = bank()
                for c in range(8):
                    pe([wkey, xk], ['pb%d' % bk],
                       lambda e, c=c: e.matmul(pb[bk][0:m, :], wb[:, c, c0:c0 + m], xb[:, c, :], start=(c == 0), stop=(c == 7)))
                act(['pb%d' % bk], [dkey], lambda e: e.activation(out=dst[:, 1:513], in_=pb[bk][0:m, :], func=AF.Copy))
                if tg == 0:
                    dve([dkey], [dkey], lambda e: e.memset(dst[:, 0:1], 0.0))
                else:
                    dve([dkey, prev[1]], [dkey], lambda e: e.tensor_copy(out=dst[:, 0:1], in_=prev[0][:, 512:513]))

            def shift(src, skey, mu, out, okey, tmp, tkey):
                dve([skey], [tkey], lambda e: e.tensor_tensor(out=tmp, in0=src[:, 0:512], in1=src[:, 1:513], op=ALU.subtract))
                dve([skey, tkey, 'pc', 'pl'], [okey],
                    lambda e: e.scalar_tensor_tensor(out=out, in0=tmp, scalar=mu, in1=src[:, 1:513], op0=ALU.mult, op1=ALU.add))

            for hl in range(2):
                for si, (sig, wb, wkey) in enumerate([('r', wrb, 'wrb'), ('k', wkb, 'wkb'), ('v', wvb, 'wvb')]):
                    proj(wb, wkey, hl * 64, 64, praw[(sig, hl, b)], 'praw_%s%d%d' % (sig, hl, b),
                         (praw[(sig, hl, 1 - b)], 'praw_%s%d%d' % (sig, hl, 1 - b)))
            for sig, c0, m in [('w', 0, 32), ('a', 32, 32), ('g', 64, 96)]:
                proj(wlob, 'wlob', c0, m, praw[(sig, 0, b)], 'praw_%s%d' % (sig, b),
                     (praw[(sig, 0, 1 - b)], 'praw_%s%d' % (sig, 1 - b)))
            shift(praw[('w', 0, b)], 'praw_w%d' % b, pl[0:32, 0:1], wlo_s[:, :], 'wlo_s', tmp96[0:32, :], 'tmp96')
            shift(praw[('a', 0, b)], 'praw_a%d' % b, pl[0:32, 1:2], alo_s[:, :], 'alo_s', tmp96[0:32, :], 'tmp96')
            shift(praw[('g', 0, b)], 'praw_g%d' % b, pl[0:96, 2:3], glo_s[:, :], 'glo_s', tmp96[0:96, :], 'tmp96')
            act(['wlo_s'], ['wlo_s'], lambda e: e.activation(out=wlo_s[:, :], in_=wlo_s[:, :], func=AF.Tanh))
            act(['glo_s'], ['glo_s'], lambda e: e.activation(out=glo_s[:, :], in_=glo_s[:, :], func=AF.Sigmoid))

            for hl in range(2):
                P = lambda i: pc[:, hl, i:i + 1]
                R, K, V, A, G, LW, CW, E1, T1, T2, T3, BON = (W[n][:, :] for n in
                                                                ['R', 'K', 'V', 'A', 'G', 'LW', 'CW', 'E1', 'T1', 'T2', 'T3', 'BON'])
                for si, (sig, dst, dk) in enumerate([('r', R, 'R'), ('k', K, 'K'), ('v', V, 'V')]):
                    shift(praw[(sig, hl, b)], 'praw_%s%d%d' % (sig, hl, b), P(si), dst, dk, T3, 'T3')
                hc = slice(hl * 64, (hl + 1) * 64)
                bk = bank()
                pe(['w2s', 'wlo_s'], ['pb%d' % bk], lambda e: e.matmul(pb[bk][0:64, :], w2s[:, hc], wlo_s[:, :], start=True, stop=True))
                act(['pb%d' % bk, 'pc'], ['LW'], lambda e: e.activation(out=LW, in_=pb[bk][0:64, :], func=AF.Sigmoid, bias=P(3), scale=1.0))
                dve(['LW'], ['LW'], lambda e: e.tensor_scalar(out=LW, in0=LW, scalar1=-0.6065306597126334, scalar2=None, op0=ALU.mult))
                bk = bank()
                pe(['a2s', 'alo_s'], ['pb%d' % bk], lambda e: e.matmul(pb[bk][0:64, :], a2s[:, hc], alo_s[:, :], start=True, stop=True))
                act(['pb%d' % bk, 'pc'], ['A'], lambda e: e.activation(out=A, in_=pb[bk][0:64, :], func=AF.Sigmoid, bias=P(4), scale=1.0))
                bk = bank()
                pe(['g2s', 'glo_s'], ['pb%d' % bk], lambda e: e.matmul(pb[bk][0:64, :], g2s[:, hc], glo_s[:, :], start=True, stop=True))
                act(['pb%d' % bk], ['G'], lambda e: e.activation(out=G, in_=pb[bk][0:64, :], func=AF.Copy))
                dve(['K', 'pc'], ['T1'], lambda e: e.tensor_scalar(out=T1, in0=K, scalar1=P(5), scalar2=None, op0=ALU.mult))
                dve(['T1'], ['T2'], lambda e: e.tensor_tensor(out=T2, in0=T1, in1=T1, op=ALU.mult))
                bk = bank()
                pe(['cst', 'T2'], ['pb%d' % bk], lambda e: e.matmul(pb[bk][0:64, :], ones64, T2, start=True, stop=True))
                act(['pb%d' % bk], ['T2'], lambda e: e.activation(out=T2, in_=pb[bk][0:64, :], func=AF.Sqrt))
                dve(['T2'], ['T2'], lambda e: e.tensor_scalar(out=T2, in0=T2, scalar1=1e-12, scalar2=None, op0=ALU.max))
                dve(['T2'], ['T2'], lambda e: e.reciprocal(out=T2, in_=T2))
                dve(['T1', 'T2'], ['T1'], lambda e: e.tensor_tensor(out=T1, in0=T1, in1=T2, op=ALU.mult))
                dve(['A', 'pc', 'omk'], ['T2'], lambda e: e.tensor_scalar(out=T2, in0=A, scalar1=P(6), scalar2=omk[:, hl:hl + 1],
                                                                         op0=ALU.mult, op1=ALU.add))
                dve(['K', 'T2'], ['K'], lambda e: e.tensor_tensor(out=K, in0=K, in1=T2, op=ALU.mult))
                dve(['T1', 'A'], ['A'], lambda e: e.tensor_tensor(out=A, in0=T1, in1=A, op=ALU.mult))
                dve(['T1'], ['T1'], lambda e: e.tensor_scalar(out=T1, in0=T1, scalar1=-1.0, scalar2=None, op0=ALU.mult))
                dve(['R', 'K', 'pc'], ['T2'], lambda e: e.scalar_tensor_tensor(out=T2, in0=R, scalar=P(8), in1=K, op0=ALU.mult, op1=ALU.mult))
                bk = bank()
                pe(['cst', 'T2'], ['pb%d' % bk], lambda e: e.matmul(pb[bk][0:64, :], ones64, T2, start=True, stop=True))
                dve(['V', 'pb%d' % bk], ['BON'], lambda e: e.tensor_tensor(out=BON, in0=V, in1=pb[bk][0:64, :], op=ALU.mult))
                for c in range(8):
                    cs = slice(c * 64, (c + 1) * 64)
                    dve(['LW', 'cst'], ['CW'], lambda e, cs=cs: e.tensor_tensor_scan(out=CW[:, cs], data0=ONE[:, 0:64], data1=LW[:, cs],
                                                                                    initial=0.0, op0=ALU.mult, op1=ALU.add))
                act(['CW'], ['E1'], lambda e: e.activation(out=E1, in_=CW, func=AF.Exp))
                act(['CW'], ['T3'], lambda e: e.activation(out=T3, in_=CW, func=AF.Exp, scale=-1.0))
                dve(['R', 'E1'], ['R'], lambda e: e.tensor_tensor(out=R, in0=R, in1=E1, op=ALU.mult))
                dve(['K', 'T3'], ['K'], lambda e: e.tensor_tensor(out=K, in0=K, in1=T3, op=ALU.mult))
                dve(['A', 'T3'], ['A'], lambda e: e.tensor_tensor(out=A, in0=A, in1=T3, op=ALU.mult))
                dve(['CW', 'LW'], ['T3'], lambda e: e.tensor_tensor(out=T3, in0=CW, in1=LW, op=ALU.subtract))
                act(['T3'], ['T3'], lambda e: e.activation(out=T3, in_=T3, func=AF.Exp))
                dve(['T1', 'T3'], ['T1'], lambda e: e.tensor_tensor(out=T1, in0=T1, in1=T3, op=ALU.mult))
                CH = [slice(c * 64, (c + 1) * 64) for c in range(8)]

                def batched(bk, lk, L_, rk, R_, transpose=False):
                    for cs in CH:
                        if transpose:
                            pe([lk, 'cst'], ['pb%d' % bk], lambda e, cs=cs: e.transpose(pb[bk][0:64, cs], L_[:, cs], ident64))
                        else:
                            pe([lk, rk], ['pb%d' % bk],
                               lambda e, cs=cs: e.matmul(pb[bk][0:64, cs], L_[:, cs], R_[:, cs], start=True, stop=True))

                def evac_mask(bk, dst, dkey, m):
                    dve(['pb%d' % bk, 'cst'], [dkey], lambda e: e.tensor_tensor(out=dst, in0=pb[bk][0:64, :], in1=m, op=ALU.mult))

                P0, P0T, TT, AakT, ArbT, ArkT, Btok, Ktok, Vtok, ysb = (W[n][:, :] for n in
                                                                         ['P0', 'P0T', 'TT', 'AakT', 'ArbT', 'ArkT', 'Btok', 'Ktok', 'Vtok', 'ysb'])
                batched(2, 'A', A, 'T1', T1)
                evac_mask(2, P0T, 'P0T', MK0)
                batched(3, 'T1', T1, 'A', A)
                evac_mask(3, P0, 'P0', MK1)
                batched(2, 'K', K, 'T1', T1)
                evac_mask(2, AakT, 'AakT', MK0)
                batched(3, 'A', A, 'R', R)
                evac_mask(3, ArbT, 'ArbT', MK2)
                batched(2, 'K', K, 'R', R)
                evac_mask(2, ArkT, 'ArkT', MK2)
                batched(3, 'A', A, None, None, transpose=True)
                act(['pb3'], ['Btok'], lambda e: e.activation(out=Btok, in_=pb[3][0:64, :], func=AF.Copy))
                batched(2, 'K', K, None, None, transpose=True)
                act(['pb2'], ['Ktok'], lambda e: e.activation(out=Ktok, in_=pb[2][0:64, :], func=AF.Copy))
                batched(3, 'V', V, None, None, transpose=True)
                act(['pb3'], ['Vtok'], lambda e: e.activation(out=Vtok, in_=pb[3][0:64, :], func=AF.Copy))
                dve(['P0T', 'cst'], ['TT'], lambda e: e.tensor_tensor(out=TT, in0=P0T, in1=IDR, op=ALU.add))
                cur, curk, curT, curTk = P0, 'P0', P0T, 'P0T'
                for lvl in range(1, 6):
                    nn, nnT = ('Pa', 'PaT') if lvl % 2 == 1 else ('Pb', 'PbT')
                    Pn, PnT = W[nn][:, :], W[nnT][:, :]
                    batched(2, curTk, curT, curk, cur)
                    act(['pb2'], [nn], lambda e: e.activation(out=Pn, in_=pb[2][0:64, :], func=AF.Copy))
                    if lvl < 5:
                        batched(3, curk, cur, curTk, curT)
                        dve(['pb3'], [nnT], lambda e: e.tensor_copy(out=PnT, in_=pb[3][0:64, :]))
                    batched(2, nn, Pn, 'TT', TT)
                    dve(['pb2', 'TT'], ['TT'], lambda e: e.tensor_tensor(out=TT, in0=TT, in1=pb[2][0:64, :], op=ALU.add))
                    cur, curk, curT, curTk = Pn, nn, PnT, nnT
                sbk = 4 + hl
                ybk = 6 + hl
                for c in range(8):
                    cs = CH[c]
                    Hc = H[hl][hcur[hl]]
                    Hn = H[hl][1 - hcur[hl]]
                    hk, hnk = 'H%d%d' % (hl, hcur[hl]), 'H%d%d' % (hl, 1 - hcur[hl])
                    sk, yk = 'pb%d' % sbk, 'pb%d' % ybk
                    pe(['T1', hk], [sk], lambda e: e.matmul(pb[sbk][0:64, 0:64], T1[:, cs], Hc[:, :], start=True, stop=False))
                    pe(['AakT', 'Vtok'], [sk], lambda e: e.matmul(pb[sbk][0:64, 0:64], AakT[:, cs], Vtok[:, cs], start=False, stop=True))
                    act([sk], ['Xs'], lambda e: e.activation(out=Xs[:, :], in_=pb[sbk][0:64, 0:64], func=AF.Copy))
                    pe(['TT', 'Xs'], [sk], lambda e: e.matmul(pb[sbk][0:64, 64:128], TT[:, cs], Xs[:, :], start=True, stop=True))
                    dve([sk], ['Us'], lambda e: e.tensor_copy(out=Us[:, :], in_=pb[sbk][0:64, 64:128]))
                    pe([hk, 'R'], [yk], lambda e: e.matmul(pb[ybk][0:64, cs], Hc[:, :], R[:, cs], start=True, stop=False))
                    pe(['Us', 'ArbT'], [yk], lambda e: e.matmul(pb[ybk][0:64, cs], Us[:, :], ArbT[:, cs], start=False, stop=False))
                    pe(['Vtok', 'ArkT'], [yk], lambda e: e.matmul(pb[ybk][0:64, cs], Vtok[:, cs], ArkT[:, cs], start=False, stop=True))
                    pe(['cst', hk], [sk], lambda e: e.matmul(pb[sbk][0:64, 128:192], ident64, Hc[:, :], start=True, stop=False))
                    pe(['Btok', 'Us'], [sk], lambda e: e.matmul(pb[sbk][0:64, 128:192], Btok[:, cs], Us[:, :], start=False, stop=False))
                    pe(['Ktok', 'Vtok'], [sk], lambda e: e.matmul(pb[sbk][0:64, 128:192], Ktok[:, cs], Vtok[:, cs], start=False, stop=True))
                    dve([sk, 'E1'], [hnk], lambda e: e.tensor_scalar(out=Hn[:, :], in0=pb[sbk][0:64, 128:192],
                                                                    scalar1=E1[:, c * 64 + 63:c * 64 + 64], scalar2=None, op0=ALU.mult))
                    hcur[hl] = 1 - hcur[hl]
                yk = 'pb%d' % ybk
                act([yk], ['ysb'], lambda e: e.activation(out=ysb, in_=pb[ybk][0:64, :], func=AF.Copy))
                bk = bank()
                pe(['cst', 'ysb'], ['pb%d' % bk], lambda e: e.matmul(pb[bk][0:64, :], onesm64, ysb, start=True, stop=True))
                dve(['ysb', 'pb%d' % bk], ['ysb'], lambda e: e.tensor_tensor(out=ysb, in0=ysb, in1=pb[bk][0:64, :], op=ALU.subtract))
                dve(['ysb'], ['T2'], lambda e: e.tensor_tensor(out=T2, in0=ysb, in1=ysb, op=ALU.mult))
                bk = bank()
                pe(['cst', 'T2'], ['pb%d' % bk], lambda e: e.matmul(pb[bk][0:64, :], onesm64, T2, start=True, stop=True))
                act(['pb%d' % bk, 'epst'], ['T2'], lambda e: e.activation(out=T2, in_=pb[bk][0:64, :], func=AF.Sqrt, bias=epst[:, 0:1], scale=1.0))
                dve(['T2'], ['T2'], lambda e: e.reciprocal(out=T2, in_=T2))
                dve(['ysb', 'T2'], ['ysb'], lambda e: e.tensor_tensor(out=ysb, in0=ysb, in1=T2, op=ALU.mult))
                dve(['ysb', 'pc'], ['ysb'], lambda e: e.tensor_scalar(out=ysb, in0=ysb, scalar1=P(9), scalar2=P(10), op0=ALU.mult, op1=ALU.add))
                dve(['ysb', 'BON'], ['ysb'], lambda e: e.tensor_tensor(out=ysb, in0=ysb, in1=BON, op=ALU.add))
                dve(['ysb', 'G'], ['osb'], lambda e: e.tensor_tensor(out=osb[:, :], in0=ysb, in1=G, op=ALU.mult))
                kb.dma('sp', ['osb'], ['yaT'], yaT[hl * 64:(hl + 1) * 64, tg * 512:(tg + 1) * 512], osb[:, :])
        kb.finish('sp')
    return nc


def rwkv_consts():
    j = np.arange(64)[:, None]
    t = np.arange(64)[None, :]
    c = np.zeros((64, 6, 512), np.float32)
    for i in range(8):
        sl = slice(i * 64, (i + 1) * 64)
        c[:, 0, sl] = (j < t)
        c[:, 1, sl] = (t < j)
        c[:, 2, sl] = (j <= t)
        c[:, 3, sl] = (j == t)
    c[:, 4, :] = 1.0
    c[:, 5, :] = 1.0 / 64.0
    return c


def rwkv_inputs(xT, ab, hg):
    w_in = ab['ab_w_in'][0]
    mu = ab['ab_shift_mu'][0]
    cols = np.arange(hg * 128, (hg + 1) * 128)
    pch = np.zeros((64, 2, 12), np.float32)
    for hl in range(2):
        h = 2 * hg + hl
        c = np.arange(h * 64, (h + 1) * 64)
        pch[:, hl, 0] = mu[c]
        pch[:, hl, 1] = mu[512 + c]
        pch[:, hl, 2] = mu[1024 + c]
        pch[:, hl, 3] = ab['ab_w0'][0][c]
        pch[:, hl, 4] = ab['ab_a0'][0][c]
        pch[:, hl, 5] = ab['ab_k_k'][0][c]
        pch[:, hl, 6] = ab['ab_k_a'][0][c]
        pch[:, hl, 8] = ab['ab_r_k'][0][h]
        pch[:, hl, 9] = ab['ab_lnx_g'][0][c]
        pch[:, hl, 10] = ab['ab_lnx_b'][0][c]
    plo = np.zeros((96, 4), np.float32)
    plo[:32, 0] = mu[1536:1568]
    plo[:32, 1] = mu[1568:1600]
    plo[:96, 2] = mu[1600:1696]
    return {
        "xT": xT,
        "wr": np.ascontiguousarray(w_in[:, cols]),
        "wk": np.ascontiguousarray(w_in[:, 512 + cols]),
        "wv": np.ascontiguousarray(w_in[:, 1024 + cols]),
        "wlo": np.ascontiguousarray(w_in[:, 1536:1696]),
        "pch": pch, "plo": plo,
        "w2h": np.ascontiguousarray(ab['ab_w2'][0][:, cols]),
        "a2h": np.ascontiguousarray(ab['ab_a2'][0][:, cols]),
        "g2h": np.ascontiguousarray(ab['ab_g2'][0][:, cols]),
        "cst": rwkv_consts(),
    }


def build_moba(n_blocks=32):
    nc = bass.Bass("TRN2", target_bir_lowering=False)
    S = SEQ
    xT = _din(nc, "xT", [D, S])
    wq = _din(nc, "wq", [D, 128])
    wk = _din(nc, "wk", [D, 128])
    wv = _din(nc, "wv", [D, 128])
    cosd = _din(nc, "cosf", [64, S])
    sind = _din(nc, "sinf", [64, S])
    protd = _din(nc, "prot", [64, 64])
    ebd = _din(nc, "eb", [33, 32, 128])
    cmd = _din(nc, "cmask", [128, 2, 256])
    identd = _din(nc, "ident", [128, 128])
    ybT = _dout(nc, "ybT", [128, S])
    with ExitStack() as es:
        kb = KB(nc, es)
        qTm = [kb.sb("qTm%d" % i, [64, S], BF16) for i in range(2)]
        kTm = [kb.sb("kTm%d" % i, [64, S], BF16) for i in range(2)]
        vm = kb.sb("vm", [128, S // 128, 128], BF16)
        biasT = [kb.sb("biasT%d" % i, [33, S], BF16) for i in range(2)]
        xTb = [kb.sb("xTb%d" % i, [128, 8, 256], BF16) for i in range(2)]
        wqb = kb.sb("wqb", [128, 8, 128], BF16)
        wkb = kb.sb("wkb", [128, 8, 128], BF16)
        wvb = kb.sb("wvb", [128, 8, 128], BF16)
        Eb = kb.sb("Eb", [33, 32, 128], BF16)
        cmask = kb.sb("cmask", [128, 2, 256], F32)
        ident = kb.sb("ident", [128, 128], F32)
        prot = kb.sb("prot", [64, 64], F32)
        ones32 = kb.sb("ones32", [64, 128], F32)
        onesb = kb.sb("onesb", [128, 64], BF16)
        ct = kb.sb("ct", [64, 256], F32)
        st = kb.sb("st", [64, 256], F32)
        qs = kb.sb("qs", [64, 256], F32)
        t1 = kb.sb("t1", [64, 256], F32)
        t2 = kb.sb("t2", [64, 256], F32)
        qf = kb.sb("qf", [64, 256], F32)
        kf = kb.sb("kf", [64, 256], F32)
        sq = kb.sb("sq", [64, 256], F32)
        kmean = [kb.sb("kmean%d" % i, [64, 32], F32) for i in range(2)]
        kmax2 = [kb.sb("kmax2_%d" % i, [128, 1], F32) for i in range(2)]
        kmx = kb.sb("kmx", [128, 1], F32)
        gsb = kb.sb("gsb", [128, 32], F32)
        m8 = kb.sb("m8", [128, 8], F32)
        selt = kb.sb("selt", [128, 33], F32)
        mt = kb.sb("mt", [128, 1], F32)
        att = [kb.sb("att%d" % i, [128, 256], BF16) for i in range(3)]
        rec = kb.sb("rec", [64, 256], F32)
        yo = [kb.sb("yo%d" % i, [64, 256], F32) for i in range(2)]
        pb = [kb.ps("pb%d" % i, [128, 512]) for i in range(8)]

        def dve(r, w, fn):
            kb.op('dve', r, w, fn)

        def act(r, w, fn):
            kb.op('act', r, w, fn)

        def pe(r, w, fn):
            kb.op('pe', r, w, fn)

        kb.dma('sp', [], ['cmask'], cmask[:, :, :], cmd[:, :, :])
        kb.dma('sp', [], ['ident'], ident[:, :], identd[:, :])
        kb.dma('sp', [], ['prot'], prot[:, :], protd[:, :])
        kb.dma('pool', [], ['Eb'], Eb[:, :, :], ebd[:, :, :])
        kb.dma('pool', [], ['wqb'], wqb[:, :, :], wq.rearrange("(c p) n -> p c n", p=128))
        kb.dma('pool', [], ['wkb'], wkb[:, :, :], wk.rearrange("(c p) n -> p c n", p=128))
        kb.dma('pool', [], ['wvb'], wvb[:, :, :], wv.rearrange("(c p) n -> p c n", p=128))
        dve([], ['ones32'], lambda e: e.memset(ones32[:, :], 1.0))
        dve([], ['onesb'], lambda e: e.memset(onesb[:, :], 1.0))
        for hl in range(2):
            dve([], ['kmean%d' % hl], lambda e: e.memset(kmean[hl][:, :], 0.0))
            dve([], ['kmax2_%d' % hl], lambda e: e.memset(kmax2[hl][:, :], 0.0))
        nbk = [0]

        def bank():
            nbk[0] += 1
            return nbk[0] % 8

        for n in range(n_blocks):
            b = n % 2
            xb, xk = xTb[b], 'xTb%d' % b
            cols = slice(n * 256, (n + 1) * 256)
            kb.dma('pool', [], [xk], xb[:, :, :], xT[:, cols].rearrange("(c p) t -> p c t", p=128))
            kb.dma('sp', [], ['ct'], ct[:, :], cosd[:, cols])
            kb.dma('sp', [], ['st'], st[:, :], sind[:, cols])

            def rope_proj(wb, wkey, hl, outf, okey):
                bk = bank()
                for c in range(8):
                    pe([wkey, xk], ['pb%d' % bk],
                       lambda e, c=c: e.matmul(pb[bk][0:64, 0:256], wb[:, c, hl * 64:(hl + 1) * 64], xb[:, c, :],
                                               start=(c == 0), stop=(c == 7)))
                act(['pb%d' % bk], ['qs'], lambda e: e.activation(out=qs[:, :], in_=pb[bk][0:64, 0:256], func=AF.Copy))
                bk2 = bank()
                pe(['prot', 'qs'], ['pb%d' % bk2], lambda e: e.matmul(pb[bk2][0:64, 0:256], prot[:, :], qs[:, :], start=True, stop=True))
                dve(['qs', 'ct'], ['t1'], lambda e: e.tensor_tensor(out=t1[:, :], in0=qs[:, :], in1=ct[:, :], op=ALU.mult))
                dve(['pb%d' % bk2, 'st'], ['t2'], lambda e: e.tensor_tensor(out=t2[:, :], in0=pb[bk2][0:64, 0:256], in1=st[:, :], op=ALU.mult))
                dve(['t1', 't2'], [okey], lambda e: e.tensor_tensor(out=outf[:, :], in0=t1[:, :], in1=t2[:, :], op=ALU.add))

            for hl in range(2):
                rope_proj(wkb, 'wkb', hl, kf, 'kf')
                act(['kf'], ['kTm%d' % hl], lambda e: e.activation(out=kTm[hl][:, cols], in_=kf[:, :], func=AF.Copy))
                dve(['kf'], ['kmean%d' % hl], lambda e: e.reduce_sum(out=kmean[hl][:, n:n + 1], in_=kf[:, :], axis=AX.X))
                dve(['kf'], ['sq'], lambda e: e.tensor_tensor(out=sq[:, :], in0=kf[:, :], in1=kf[:, :], op=ALU.mult))
                bk = bank()
                pe(['ones32', 'sq'], ['pb%d' % bk], lambda e: e.matmul(pb[bk][:, 0:256], ones32[:, :], sq[:, :], start=True, stop=True))
                dve(['pb%d' % bk], ['kmx'], lambda e: e.reduce_max(out=kmx[:, :], in_=pb[bk][:, 0:256], axis=AX.X))
                dve(['kmx', 'kmax2_%d' % hl], ['kmax2_%d' % hl],
                    lambda e: e.tensor_tensor(out=kmax2[hl][:, :], in0=kmax2[hl][:, :], in1=kmx[:, :], op=ALU.max))
                rope_proj(wqb, 'wqb', hl, qf, 'qf')
                act(['qf'], ['qTm%d' % hl], lambda e: e.activation(out=qTm[hl][:, cols], in_=qf[:, :], func=AF.Copy, scale=0.125))
                dve(['qf'], ['sq'], lambda e: e.tensor_tensor(out=sq[:, :], in0=qf[:, :], in1=qf[:, :], op=ALU.mult))
                for tt in range(2):
                    ts_ = slice(tt * 128, (tt + 1) * 128)
                    bk = bank()
                    pk = 'pb%d' % bk
                    pe(['qf', 'kmean%d' % hl], [pk], lambda e: e.matmul(pb[bk][:, 0:32], qf[:, ts_], kmean[hl][:, :], start=True, stop=True))
                    pe(['sq', 'ones32'], [pk], lambda e: e.matmul(pb[bk][:, 32:33], sq[:, ts_], ones32[:, 0:1], start=True, stop=True))
                    if n == 0:
                        dve([], ['selt'], lambda e: e.memset(selt[:, 0:32], 0.0))
                    else:
                        dve([pk], ['gsb'], lambda e: e.tensor_copy(out=gsb[:, :], in_=pb[bk][:, 0:32]))
                        dve(['gsb'], ['gsb'], lambda e: e.memset(gsb[:, n:32], -1e30))
                        dve(['gsb'], ['m8'], lambda e: e.max(out=m8[:, :], in_=gsb[:, :]))
                        dve(['gsb', 'm8'], ['selt'], lambda e: e.tensor_scalar(out=selt[:, 0:32], in0=gsb[:, :], scalar1=m8[:, 2:3],
                                                                              scalar2=None, op0=ALU.is_ge))
                        dve(['selt'], ['selt'], lambda e: e.memset(selt[:, n:32], 0.0))
                    dve(['selt'], ['selt'], lambda e: e.memset(selt[:, n:n + 1], 1.0))
                    dve(['selt'], ['selt'], lambda e: e.tensor_scalar(out=selt[:, 0:32], in0=selt[:, 0:32], scalar1=-1.0, scalar2=30000.0,
                                                                     op0=ALU.add, op1=ALU.mult))
                    dve([pk, 'kmax2_%d' % hl], ['mt'], lambda e: e.tensor_tensor(out=mt[:, :], in0=pb[bk][:, 32:33], in1=kmax2[hl][:, :], op=ALU.mult))
                    act(['mt'], ['mt'], lambda e: e.activation(out=mt[:, :], in_=mt[:, :], func=AF.Sqrt))
                    dve(['mt', 'selt'], ['selt'], lambda e: e.tensor_scalar(out=selt[:, 32:33], in0=mt[:, :], scalar1=-0.125, scalar2=None, op0=ALU.mult))
                    bk2 = bank()
                    pe(['selt', 'ident'], ['pb%d' % bk2], lambda e: e.transpose(pb[bk2][0:33, 0:128], selt[:, :], ident[:, :]))
                    act(['pb%d' % bk2], ['biasT%d' % hl],
                        lambda e: e.activation(out=biasT[hl][:, n * 256 + tt * 128:n * 256 + (tt + 1) * 128], in_=pb[bk2][0:33, 0:128], func=AF.Copy))
            for tt in range(2):
                bk = bank()
                for c in range(8):
                    pe(['wvb', xk], ['pb%d' % bk],
                       lambda e, c=c: e.matmul(pb[bk][:, 0:128], xb[:, c, tt * 128:(tt + 1) * 128], wvb[:, c, :], start=(c == 0), stop=(c == 7)))
                act(['pb%d' % bk], ['vm'], lambda e: e.activation(out=vm[:, 2 * n + tt, :], in_=pb[bk][:, 0:128], func=AF.Copy))

        tiles = []
        g = 0
        for hl in range(2):
            for n in range(n_blocks):
                for kt in range(2 * n + 2):
                    tiles.append(dict(hl=hl, n=n, kt=kt, first=(kt == 0), last=(kt == 2 * n + 1),
                                      j=(kt - 2 * n if kt >= 2 * n else None), g=g, i=len(tiles)))
                g += 1
        nt = len(tiles)

        def M1(t):
            i = t['i']
            sb_ = i % 2
            qc = slice(t['n'] * 256, (t['n'] + 1) * 256)
            pe(['kTm%d' % t['hl'], 'qTm%d' % t['hl']], ['pb%d' % sb_],
               lambda e: e.matmul(pb[sb_][:, 0:256], kTm[t['hl']][:, t['kt'] * 128:(t['kt'] + 1) * 128], qTm[t['hl']][:, qc],
                                  start=True, stop=False))
            pe(['Eb', 'biasT%d' % t['hl']], ['pb%d' % sb_],
               lambda e: e.matmul(pb[sb_][:, 0:256], Eb[:, t['kt'] // 2, :], biasT[t['hl']][:, qc], start=False, stop=True))

        def M2(t):
            i = t['i']
            ak = 'att%d' % (i % 3)
            act(['pb%d' % (i % 2)], [ak], lambda e: e.activation(out=att[i % 3][:, :], in_=pb[i % 2][:, 0:256], func=AF.Exp))
            if t['j'] is not None:
                dve([ak, 'cmask'], [ak], lambda e: e.tensor_tensor(out=att[i % 3][:, :], in0=att[i % 3][:, :], in1=cmask[:, t['j'], :], op=ALU.mult))

        def M3(t):
            i = t['i']
            ak = 'att%d' % (i % 3)
            ob, db = 2 + t['g'] % 2, 4 + t['g'] % 2
            pe(['vm', ak], ['pb%d' % ob],
               lambda e: e.matmul(pb[ob][0:64, 0:256], vm[:, t['kt'], t['hl'] * 64:(t['hl'] + 1) * 64], att[i % 3][:, :],
                                  start=t['first'], stop=t['last']))
            pe(['onesb', ak], ['pb%d' % db],
               lambda e: e.matmul(pb[db][0:64, 0:256], onesb[:, :], att[i % 3][:, :], start=t['first'], stop=t['last']))
            if t['last']:
                yk = 'yo%d' % (t['g'] % 2)
                dve(['pb%d' % db], ['rec'], lambda e: e.reciprocal(out=rec[:, :], in_=pb[db][0:64, 0:256]))
                dve(['pb%d' % ob, 'rec'], [yk], lambda e: e.tensor_tensor(out=yo[t['g'] % 2][:, :], in0=pb[ob][0:64, 0:256], in1=rec[:, :], op=ALU.mult))
                kb.dma('sp', [yk], ['ybT'], ybT[t['hl'] * 64:(t['hl'] + 1) * 64, t['n'] * 256:(t['n'] + 1) * 256], yo[t['g'] % 2][:, :])

        for s in range(nt + 2):
            if s < nt:
                M1(tiles[s])
            if 0 <= s - 1 < nt:
                M2(tiles[s - 1])
            if 0 <= s - 2 < nt:
                M3(tiles[s - 2])
        kb.finish('sp')
    return nc


def moba_consts():
    half = 8
    inv_freq = (np.float32(500000.0) ** (-np.arange(half, dtype=np.float32) / np.float32(half))).astype(np.float32)
    ang = (np.arange(SEQ, dtype=np.float32)[:, None] * inv_freq[None, :]).astype(np.float32)
    cos = np.cos(ang).astype(np.float32).T
    sin = np.sin(ang).astype(np.float32).T
    cosf = np.ones((64, SEQ), np.float32)
    sinf = np.zeros((64, SEQ), np.float32)
    cosf[0:8] = cos
    cosf[8:16] = cos
    sinf[0:8] = sin
    sinf[8:16] = sin
    prot = np.zeros((64, 64), np.float32)
    for m in range(8):
        prot[m + 8, m] = -1.0
        prot[m, m + 8] = 1.0
    eb = np.zeros((33, 32, 128), np.float32)
    for n in range(32):
        eb[n, n, :] = 1.0
    eb[32, :, :] = 1.0
    s = np.arange(128)[:, None, None]
    j = np.arange(2)[None, :, None]
    t = np.arange(256)[None, None, :]
    cmask = (128 * j + s <= t).astype(np.float32)
    return {"cosf": cosf, "sinf": sinf, "prot": prot, "eb": eb, "cmask": np.ascontiguousarray(cmask),
            "ident": np.eye(128, dtype=np.float32)}


def moba_inputs(xT, w_in, hg, consts):
    cols = 1696 + np.arange(hg * 128, (hg + 1) * 128)
    m = {"xT": xT, "wq": np.ascontiguousarray(w_in[:, cols]), "wk": np.ascontiguousarray(w_in[:, 512 + cols]),
         "wv": np.ascontiguousarray(w_in[:, 1024 + cols])}
    m.update(consts)
    return m


_PROGS = {}


def _prog(name, fn):
    if name not in _PROGS:
        _PROGS[name] = fn()
    return _PROGS[name]


def _run(nc, in_maps):
    res = run_bass_kernel_spmd(nc, in_maps, core_ids=list(range(NCORES)))
    return res.results


def _post_launch(yT_b, xres, w_out, i, inputs):
    ident = np.eye(128, dtype=np.float32)
    b1T = np.ascontiguousarray(inputs['exp_b1'][i].reshape(32, 16, 128).transpose(2, 0, 1))
    shared = {
        "w_out": np.ascontiguousarray(w_out), "ln1g": inputs['ln1_g'][i], "ln1b": inputs['ln1_b'][i],
        "rw": inputs['router_w'][i], "rb": inputs['router_b'][i], "w1": inputs['exp_w1'][i], "b1T": b1T,
        "w2": inputs['exp_w2'][i], "b2": inputs['exp_b2'][i], "ln2g": inputs['ln2_g'][i], "ln2b": inputs['ln2_b'][i],
        "ident": ident, "pcst": post2_consts(),
    }
    maps = []
    for c in range(NCORES):
        b, s0 = c // 4, (c % 4) * 2048
        m = dict(shared)
        m["yT"] = np.ascontiguousarray(yT_b[b][:, s0:s0 + 2048])
        m["xr"] = np.ascontiguousarray(xres[b, s0:s0 + 2048, :])
        maps.append(m)
    res = _run(_prog("post2", build_post2), maps)
    out = np.empty((NB, SEQ, D), np.float32)
    for c in range(NCORES):
        b, s0 = c // 4, (c % 4) * 2048
        out[b, s0:s0 + 2048, :] = res[c]["out"]
    return out


def kernel(**inputs):
    inputs = {k: np.asarray(v) for k, v in inputs.items()}
    x = inputs['x'].astype(np.float32, copy=False)
    xT = [np.ascontiguousarray(x[b].T) for b in range(NB)]
    ab = {k: v for k, v in inputs.items() if k.startswith('ab_')}
    maps = [rwkv_inputs(xT[c // 4], ab, c % 4) for c in range(NCORES)]
    res = _run(_prog("rwkv", build_rwkv), maps)
    yT0 = [np.empty((D, SEQ), np.float32) for _ in range(NB)]
    for c in range(NCORES):
        b, hg = c // 4, c % 4
        yT0[b][hg * 128:(hg + 1) * 128, :] = res[c]["yaT"]
    mc = moba_consts()
    maps = [moba_inputs(xT[c // 4], ab['ab_w_in'][0], c % 4, mc) for c in range(NCORES)]
    res = _run(_prog("moba", build_moba), maps)
    for c in range(NCORES):
        b, hg = c // 4, c % 4
        yT0[b][512 + hg * 128:512 + (hg + 1) * 128, :] = res[c]["ybT"]
    x2 = _post_launch(yT0, x, ab['ab_w_out'][0], 0, inputs)
    xT2 = [np.ascontiguousarray(x2[b].T) for b in range(NB)]
    w_in = inputs['sb_w_in'][0]
    mask, negtri = sb_mask(), sb_negtri()
    maps = []
    for c in range(NCORES):
        b, hq = c // 4, c % 4
        cols = hq * 256 + np.arange(256)
        maps.append({"xT": xT2[b], "wq": np.ascontiguousarray(w_in[:, cols]), "wk": np.ascontiguousarray(w_in[:, 1024 + cols]),
                     "wv": np.ascontiguousarray(w_in[:, 2048 + cols]), "mask": mask, "negtri": negtri})
    res = _run(_prog("sb", build_sb), maps)
    yT1 = [np.empty((D, SEQ), np.float32) for _ in range(NB)]
    for c in range(NCORES):
        b, hq = c // 4, c % 4
        yT1[b][hq * 256:(hq + 1) * 256, :] = res[c]["yT"]
    out = _post_launch(yT1, x2, inputs['sb_w_out'][0], 1, inputs)
    return out


CAP = 512
I32 = mybir.dt.int32


def build_post2(n_exp=N_EXP):
    nc = bass.Bass("TRN2", target_bir_lowering=False)
    NTOK = 2048
    NT = NTOK // 128
    NSL = N_EXP * CAP
    yT = _din(nc, "yT", [D, NTOK])
    xr = _din(nc, "xr", [NTOK, D])
    w_out = _din(nc, "w_out", [D, D])
    ln1g = _din(nc, "ln1g", [D])
    ln1b = _din(nc, "ln1b", [D])
    rw = _din(nc, "rw", [D, N_EXP])
    rb = _din(nc, "rb", [N_EXP])
    w1 = _din(nc, "w1", [N_EXP, D, 2 * D])
    b1T = _din(nc, "b1T", [128, N_EXP, 16])
    w2 = _din(nc, "w2", [N_EXP, D, D])
    b2 = _din(nc, "b2", [N_EXP, D])
    ln2g = _din(nc, "ln2g", [D])
    ln2b = _din(nc, "ln2b", [D])
    ident_d = _din(nc, "ident", [128, 128])
    cst_d = _din(nc, "pcst", [128, 128 + N_EXP + 1])
    out = _dout(nc, "out", [NTOK, D])
    XG = nc.dram_tensor("XG_int", [NSL + 128, D], BF16, kind="Internal").ap()
    YG = nc.dram_tensor("YG_int", [NSL + 128, D], F32, kind="Internal").ap()
    AX1 = nc.dram_tensor("AX1_int", [NTOK, D], F32, kind="Internal").ap()

    with ExitStack() as es:
        kb = KB(nc, es)
        Tb = [kb.sb("Tb%d" % i, [128, D], F32) for i in range(2)]
        x1b = [kb.sb("x1b%d" % i, [128, D], BF16) for i in range(2)]
        ax1 = kb.sb("ax1", [128, D], F32)
        x1T32 = kb.sb("x1T32", [128, 8, 128], F32)
        woutb = kb.sb("woutb", [128, 8, D], BF16)
        yTb = [kb.sb("yTb%d" % i, [128, 8, 128], BF16) for i in range(2)]
        xrt = kb.sb("xrt", [128, D], F32)
        gB = kb.sb("gB", [128, D], F32)
        bB = kb.sb("bB", [128, D], F32)
        Mb = kb.sb("Mb", [128, NT, N_EXP], BF16)
        GS = kb.sb("GS", [128, NT * 4], I32)
        GA = kb.sb("GA", [128, NT, 4], F32)
        NSLOT = 6
        slots = [kb.sb("wslot%d" % i, [128, 8, 512], BF16) for i in range(NSLOT)]
        xg = kb.sb("xg", [128, 4, D], BF16)
        xgT = kb.sb("xgT", [128, 8, CAP], BF16)
        actT = kb.sb("actT", [128, 8, CAP], BF16)
        g32 = [kb.sb("g32_%d" % i, [128, 512], F32) for i in range(2)]
        s32 = [kb.sb("s32_%d" % i, [128, 512], F32) for i in range(2)]
        l32 = [kb.sb("l32_%d" % i, [128, 512], F32) for i in range(2)]
        ysl = [kb.sb("ysl%d" % i, [128, 512], F32) for i in range(3)]
        yk = [kb.sb("yk%d" % i, [128, D], F32) for i in range(2)]
        rw32 = kb.sb("rw32", [128, 8, N_EXP], F32)
        rbB = kb.sb("rbB", [128, N_EXP], F32)
        b1s = kb.sb("b1s", [128, N_EXP, 16], F32)
        b2t = kb.sb("b2t", [1, D], F32)
        ones32 = kb.sb("ones32", [1, 128], F32)
        onesb = kb.sb("onesb", [128, 128], BF16)
        ident = kb.sb("ident", [128, 128], F32)
        identb = kb.sb("identb", [128, 128], BF16)
        pcst = kb.sb("pcst", [128, 128 + N_EXP + 1], F32)
        trib = kb.sb("trib", [128, 128], BF16)
        stats = kb.sb("stats", [128, 2, 6], F32)
        mv = kb.sb("mv", [128, 2], F32)
        rs = kb.sb("rs", [128, 1], F32)
        lg = kb.sb("lg", [128, N_EXP], F32)
        m8 = kb.sb("m8", [128, 8], F32)
        msk = kb.sb("msk", [128, N_EXP], F32)
        val = kb.sb("val", [128, N_EXP], F32)
        rnk = kb.sb("rnk", [128, N_EXP], F32)
        prod = kb.sb("prod", [128, N_EXP], F32)
        gsf = kb.sb("gsf", [128, 4], F32)
        rkf = kb.sb("rkf", [128, 4], F32)
        ov = kb.sb("ov", [128, 4], F32)
        nov = kb.sb("nov", [128, 4], F32)
        nm = kb.sb("nm", [128, 1], F32)
        ex = kb.sb("ex", [128, 4], F32)
        den = kb.sb("den", [128, 1], F32)
        eps_t = kb.sb("eps_t", [128, 1], F32)
        kb.eps_t = eps_t
        pb = [kb.ps("pb%d" % i, [128, 512]) for i in range(6)]
        pt = [kb.ps("pb%d" % (6 + i), [128, 1024], BF16) for i in range(2)]

        def dve(r, w, fn):
            kb.op('dve', r, w, fn)

        def act(r, w, fn):
            kb.op('act', r, w, fn)

        def pe(r, w, fn):
            kb.op('pe', r, w, fn)

        def ind(reads, writes, **kw):
            writes = list(writes) + ['IND']
            deps = kb._deps(reads, writes)
            i = kb.dnext
            kb.dnext = (kb.dnext + 1) % len(kb.dsem)
            if kb.dcnt[i] > 0:
                deps[i] = max(deps.get(i, 0), kb.dcnt[i])
            kb._wait('pool', deps)
            inst = nc.gpsimd.indirect_dma_start(**kw)
            kb.dcnt[i] += 16
            inst.then_inc(kb.dsem[i], 16)
            kb._commit((i, kb.dcnt[i]), reads, writes)

        bc_reg = nc.gpsimd.to_reg(NSL + 127)
        dve([], ['eps'], lambda e: e.memset(eps_t[:, :], LN_EPS))
        dve([], ['ones32'], lambda e: e.memset(ones32[:, :], 1.0))
        dve([], ['onesb'], lambda e: e.memset(onesb[:, :], 1.0))
        dve([], ['xrt'], lambda e: e.memset(xrt[:, :], 0.0))
        kb.dma('sp', ['xrt'], ['YGz'], YG[NSL:NSL + 128, :], xrt[:, :])
        kb.dma('sp', [], ['ident'], ident[:, :], ident_d[:, :])
        kb.dma('sp', [], ['pcst'], pcst[:, :], cst_d[:, :])
        kb.dma('sp', [], ['rw32'], rw32[:, :, :], rw.rearrange("(c p) e -> p c e", p=128))
        kb.dma('sp', [], ['rbB'], rbB[:, :], rb.partition_broadcast(128))
        kb.dma('sp', [], ['b1s'], b1s[:, :, :], b1T[:, :, :])
        kb.dma('pool', [], ['woutb'], woutb[:, :, :], w_out.rearrange("(c p) n -> p c n", p=128))
        dve(['ident'], ['identb'], lambda e: e.tensor_copy(out=identb[:, :], in_=ident[:, :]))
        dve(['pcst'], ['trib'], lambda e: e.tensor_copy(out=trib[:, :], in_=pcst[:, 0:128]))
        CE = pcst[:, 128:128 + N_EXP]
        DCOL = pcst[:, 128 + N_EXP:128 + N_EXP + 1]

        blocks = [(e_, b_) for e_ in range(n_exp) for b_ in range(6)]
        nload = [0]

        def load_next_block():
            n = nload[0]
            if n >= len(blocks):
                return
            e_, b_ = blocks[n]
            s = n % NSLOT
            if b_ < 4:
                c0 = [0, 1024, 512, 1536][b_]
                src = w1[e_, :, c0:c0 + 512].rearrange("(c p) n -> p c n", p=128)
            else:
                c0 = (b_ - 4) * 512
                src = w2[e_, :, c0:c0 + 512].rearrange("(c p) n -> p c n", p=128)
            kb.dma('pool', [], ['slot%d' % s], slots[s][:, :, :], src)
            nload[0] += 1

        kb.dma('sp', [], ['gB'], gB[:, :], ln1g.partition_broadcast(128))
        kb.dma('sp', [], ['bB'], bB[:, :], ln1b.partition_broadcast(128))
        for i in range(NT):
            t0 = i * 128
            yb, ybk = yTb[i % 2], 'yTb%d' % (i % 2)
            T, tk = Tb[i % 2], 'Tb%d' % (i % 2)
            xb_, xbk = x1b[i % 2], 'x1b%d' % (i % 2)
            kb.dma('pool', [], [ybk], yb[:, :, :], yT[:, t0:t0 + 128].rearrange("(c p) t -> p c t", p=128))
            kb.dma('sp', [], ['xrt'], xrt[:, :], xr[t0:t0 + 128, :])
            for h in range(2):
                for c in range(8):
                    pe([ybk, 'woutb'], ['pb%d' % h],
                       lambda e, c=c, h=h: e.matmul(pb[h][:, :], yb[:, c, :], woutb[:, c, h * 512:(h + 1) * 512],
                                                    start=(c == 0), stop=(c == 7)))
            for h in range(2):
                dve(['xrt', 'pb%d' % h], [tk],
                    lambda e, h=h: e.scalar_tensor_tensor(out=T[:, h * 512:(h + 1) * 512], in0=xrt[:, h * 512:(h + 1) * 512],
                                                          scalar=ALPHA, in1=pb[h][:, :], op0=ALU.mult, op1=ALU.add))
            layer_norm_inplace(kb, T[:, :], tk, (gB, 'gB'), (bB, 'bB'), stats, mv, rs, '')
            act([tk], [xbk], lambda e: e.activation(out=xb_[:, :], in_=T[:, :], func=AF.Copy))
            act([tk], ['ax1'], lambda e: e.mul(ax1[:, :], T[:, :], ALPHA))
            kb.dma('sp', ['ax1'], ['AX1_%d' % i], AX1[t0:t0 + 128, :], ax1[:, :])
            for c in range(8):
                bk = 2 + c // 4
                pe([tk, 'ident'], ['pb%d' % bk],
                   lambda e, c=c, bk=bk: e.transpose(pb[bk][:, (c % 4) * 128:(c % 4 + 1) * 128], T[:, c * 128:(c + 1) * 128], ident[:, :]))
            for hb in range(2):
                bk = 2 + hb
                dve(['pb%d' % bk], ['x1T32'],
                    lambda e, hb=hb, bk=bk: e.tensor_copy(out=x1T32[:, hb * 4:(hb + 1) * 4, :],
                                                          in_=pb[bk][:, :].rearrange("p (c t) -> p c t", c=4)))
            for c in range(8):
                pe(['x1T32', 'rw32'], ['pb4'],
                   lambda e, c=c: e.matmul(pb[4][:, 0:N_EXP], x1T32[:, c, :], rw32[:, c, :], start=(c == 0), stop=(c == 7)))
            dve(['pb4', 'rbB'], ['lg'], lambda e: e.tensor_tensor(out=lg[:, :], in0=pb[4][:, 0:N_EXP], in1=rbB[:, :], op=ALU.add))
            dve(['lg'], ['m8'], lambda e: e.max(out=m8[:, :], in_=lg[:, :]))
            dve(['m8'], ['nm'], lambda e: e.tensor_scalar(out=nm[:, :], in0=m8[:, 0:1], scalar1=-1.0, scalar2=None, op0=ALU.mult))
            act(['m8', 'nm'], ['ex'], lambda e: e.activation(out=ex[:, :], in_=m8[:, 0:4], func=AF.Exp, bias=nm[:, 0:1], scale=1.0))
            dve(['ex'], ['den'], lambda e: e.reduce_sum(out=den[:, :], in_=ex[:, :], axis=AX.X))
            dve(['den'], ['den'], lambda e: e.reciprocal(out=den[:, :], in_=den[:, :]))
            dve(['ex', 'den'], ['ex'], lambda e: e.tensor_scalar(out=ex[:, :], in0=ex[:, :], scalar1=den[:, 0:1], scalar2=None, op0=ALU.mult))
            dve(['lg', 'm8'], ['msk'], lambda e: e.tensor_scalar(out=msk[:, :], in0=lg[:, :], scalar1=m8[:, 3:4], scalar2=None, op0=ALU.is_ge))
            dve(['msk'], ['Mb'], lambda e: e.tensor_copy(out=Mb[:, i, :], in_=msk[:, :]))
            for i2 in range(i):
                pe(['onesb', 'Mb'], ['pb5'], lambda e, i2=i2: e.matmul(pb[5][:, 0:N_EXP], onesb[:, :], Mb[:, i2, :], start=(i2 == 0), stop=False))
            pe(['trib', 'Mb'], ['pb5'], lambda e: e.matmul(pb[5][:, 0:N_EXP], trib[:, :], Mb[:, i, :], start=(i == 0), stop=True))
            dve(['pb5'], ['rnk'], lambda e: e.tensor_copy(out=rnk[:, :], in_=pb[5][:, 0:N_EXP]))
            dve(['rnk', 'pcst'], ['val'], lambda e: e.tensor_tensor(out=val[:, :], in0=rnk[:, :], in1=CE, op=ALU.add))
            for k in range(4):
                dve(['lg', 'm8', 'val'], ['prod'],
                    lambda e, k=k: e.scalar_tensor_tensor(out=prod[:, :], in0=lg[:, :], scalar=m8[:, k:k + 1], in1=val[:, :],
                                                          op0=ALU.is_equal, op1=ALU.mult))
                dve(['prod'], ['gsf'], lambda e, k=k: e.reduce_sum(out=gsf[:, k:k + 1], in_=prod[:, :], axis=AX.X))
                dve(['lg', 'm8', 'rnk'], ['prod'],
                    lambda e, k=k: e.scalar_tensor_tensor(out=prod[:, :], in0=lg[:, :], scalar=m8[:, k:k + 1], in1=rnk[:, :],
                                                          op0=ALU.is_equal, op1=ALU.mult))
                dve(['prod'], ['rkf'], lambda e, k=k: e.reduce_sum(out=rkf[:, k:k + 1], in_=prod[:, :], axis=AX.X))
            dve(['rkf'], ['ov'], lambda e: e.tensor_scalar(out=ov[:, :], in0=rkf[:, :], scalar1=float(CAP) - 0.5, scalar2=None, op0=ALU.is_ge))
            dve(['ov'], ['nov'], lambda e: e.tensor_scalar(out=nov[:, :], in0=ov[:, :], scalar1=-1.0, scalar2=1.0, op0=ALU.mult, op1=ALU.add))
            dve(['gsf', 'nov'], ['gsf'], lambda e: e.tensor_tensor(out=gsf[:, :], in0=gsf[:, :], in1=nov[:, :], op=ALU.mult))
            dve(['ov', 'pcst'], ['ov'], lambda e: e.tensor_scalar(out=ov[:, :], in0=ov[:, :], scalar1=DCOL, scalar2=None, op0=ALU.mult))
            dve(['gsf', 'ov'], ['gsf'], lambda e: e.tensor_tensor(out=gsf[:, :], in0=gsf[:, :], in1=ov[:, :], op=ALU.add))
            dve(['gsf'], ['GS'], lambda e: e.tensor_copy(out=GS[:, i * 4:(i + 1) * 4], in_=gsf[:, :]))
            dve(['ex', 'nov'], ['GA'], lambda e: e.tensor_tensor(out=GA[:, i, :], in0=ex[:, :], in1=nov[:, :], op=ALU.mult))
            for k in range(4):
                ind([xbk, 'GS'], ['XG%d' % i], out=XG[:, :], out_offset=bass.IndirectOffsetOnAxis(ap=GS[:, i * 4 + k:i * 4 + k + 1], axis=0),
                    in_=xb_[:, :], in_offset=None, bounds_check=bc_reg, oob_is_err=False)

        if _STAGE == 21:
            kb.finish('sp')
            return nc
        for _ in range(NSLOT):
            load_next_block()
        nuse = [0]
        pair = 0
        obn = 0
        for e_ in range(n_exp):
            kb.dma('sp', [], ['b2t'], b2t[:, :], b2[e_:e_ + 1, :])
            kb.dma('sp', ['XG%d' % q for q in range(NT)], ['xg'], xg[:, :, :], XG[e_ * CAP:(e_ + 1) * CAP, :].rearrange("(st p) f -> p st f", p=128))
            for cc in range(4):
                ptk = 'pb%d' % (6 + cc % 2)
                for c2 in range(2):
                    c = cc * 2 + c2
                    for st in range(4):
                        pe(['xg', 'identb'], [ptk],
                           lambda e, c=c, c2=c2, st=st, cc=cc: e.transpose(pt[cc % 2][:, c2 * 512 + st * 128:c2 * 512 + (st + 1) * 128],
                                                                          xg[:, st, c * 128:(c + 1) * 128], identb[:, :]))
                if cc % 2 == 0:
                    act([ptk], ['xgT'], lambda e, cc=cc: e.activation(out=xgT[:, cc * 2:cc * 2 + 2, :],
                                                                    in_=pt[cc % 2][:, :].rearrange("p (c t) -> p c t", c=2), func=AF.Copy))
                else:
                    dve([ptk], ['xgT'], lambda e, cc=cc: e.tensor_copy(out=xgT[:, cc * 2:cc * 2 + 2, :],
                                                                      in_=pt[cc % 2][:, :].rearrange("p (c t) -> p c t", c=2)))
            for fcb in range(2):
                sa = nuse[0] % NSLOT
                sl = (nuse[0] + 1) % NSLOT
                for f4 in range(4):
                    fc = fcb * 4 + f4
                    hg, hl, tb = pair % 2, 2 + pair % 2, pair % 2
                    pair += 1
                    for c in range(8):
                        pe(['slot%d' % sa, 'xgT'], ['pb%d' % hg],
                           lambda e, c=c: e.matmul(pb[hg][:, :], slots[sa][:, c, f4 * 128:(f4 + 1) * 128], xgT[:, c, :],
                                                   start=(c == 0), stop=(c == 7)))
                    for c in range(8):
                        pe(['slot%d' % sl, 'xgT'], ['pb%d' % hl],
                           lambda e, c=c: e.matmul(pb[hl][:, :], slots[sl][:, c, f4 * 128:(f4 + 1) * 128], xgT[:, c, :],
                                                   start=(c == 0), stop=(c == 7)))
                    G, S_, L = g32[tb], s32[tb], l32[tb]
                    gk, sk, lk = 'g32_%d' % tb, 's32_%d' % tb, 'l32_%d' % tb
                    dve(['pb%d' % hg, 'b1s'], [gk], lambda e: e.tensor_scalar(out=G[:, :], in0=pb[hg][:, :], scalar1=b1s[:, e_, fc:fc + 1],
                                                                             scalar2=7.0, op0=ALU.add, op1=ALU.min))
                    act([gk], [sk], lambda e: e.activation(out=S_[:, :], in_=G[:, :], func=AF.Sigmoid, scale=1.702))
                    dve(['pb%d' % hl, 'b1s'], [lk], lambda e: e.tensor_scalar(out=L[:, :], in0=pb[hl][:, :], scalar1=b1s[:, e_, 8 + fc:9 + fc],
                                                                             scalar2=-7.0, op0=ALU.add, op1=ALU.max))
                    dve([lk], [lk], lambda e: e.tensor_scalar(out=L[:, :], in0=L[:, :], scalar1=7.0, scalar2=1.0, op0=ALU.min, op1=ALU.add))
                    dve([gk, sk], [sk], lambda e: e.tensor_tensor(out=S_[:, :], in0=G[:, :], in1=S_[:, :], op=ALU.mult))
                    dve([sk, lk], ['actT'], lambda e: e.tensor_tensor(out=actT[:, fc, :], in0=S_[:, :], in1=L[:, :], op=ALU.mult))
                nuse[0] += 2
                load_next_block()
                load_next_block()
            for h in range(2):
                s2 = nuse[0] % NSLOT
                for st in range(4):
                    ob = 4 + obn % 2
                    yb_ = ysl[obn % 3]
                    ybk = 'ysl%d' % (obn % 3)
                    obn += 1
                    pe(['ones32', 'b2t'], ['pb%d' % ob],
                       lambda e: e.matmul(pb[ob][:, :], ones32[0:1, :], b2t[0:1, h * 512:(h + 1) * 512], start=True, stop=False))
                    for fc in range(8):
                        pe(['actT', 'slot%d' % s2], ['pb%d' % ob],
                           lambda e, fc=fc: e.matmul(pb[ob][:, :], actT[:, fc, st * 128:(st + 1) * 128], slots[s2][:, fc, :],
                                                     start=False, stop=(fc == 7)))
                    act(['pb%d' % ob], [ybk], lambda e: e.activation(out=yb_[:, :], in_=pb[ob][:, :], func=AF.Copy))
                    r0 = e_ * CAP + st * 128
                    kb.dma('sp', [ybk], ['YG%d' % e_], YG[r0:r0 + 128, h * 512:(h + 1) * 512], yb_[:, :])
                nuse[0] += 1
                load_next_block()

        if _STAGE == 22:
            kb.finish('sp')
            return nc
        kb.dma('sp', [], ['gB'], gB[:, :], ln2g.partition_broadcast(128))
        kb.dma('sp', [], ['bB'], bB[:, :], ln2b.partition_broadcast(128))
        nk = 0
        for i in range(NT):
            T, tk = Tb[i % 2], 'Tb%d' % (i % 2)
            kb.dma('sp', ['AX1_%d' % i], [tk], T[:, :], AX1[i * 128:(i + 1) * 128, :])
            for k in range(4):
                Y, ykk = yk[0], 'yk0'
                ind(['YGz', 'GS'] + ['YG%d' % q for q in range(n_exp)], [ykk], out=Y[:, :], out_offset=None, in_=YG[:, :],
                    in_offset=bass.IndirectOffsetOnAxis(ap=GS[:, i * 4 + k:i * 4 + k + 1], axis=0), bounds_check=bc_reg, oob_is_err=False)
                dve([ykk, 'GA', tk], [tk],
                    lambda e, k=k, Y=Y: e.scalar_tensor_tensor(out=T[:, :], in0=Y[:, :], scalar=GA[:, i, k:k + 1], in1=T[:, :],
                                                               op0=ALU.mult, op1=ALU.add))
            layer_norm_inplace(kb, T[:, :], tk, (gB, 'gB'), (bB, 'bB'), stats, mv, rs, '')
            kb.dma('sp', [tk], ['out%d' % i], out[i * 128:(i + 1) * 128, :], T[:, :])
        kb.finish('sp')
    return nc


def post2_consts():
    c = np.zeros((128, 128 + N_EXP + 1), np.float32)
    k = np.arange(128)[:, None]
    m = np.arange(128)[None, :]
    c[:, 0:128] = (k < m)
    c[:, 128:128 + N_EXP] = CAP * np.arange(N_EXP, dtype=np.float32)[None, :]
    c[:, 128 + N_EXP] = N_EXP * CAP + np.arange(128)
    return c
```
